# Optimizing a Trainium2 kernel written in Bass

```python
import math
import jax, jax.numpy as jnp
from jax import lax
import numpy as np

D_MODEL = 1024
BATCH = 16
SEQ = 4096
DEPTH = 4

HEAD_DIM = 64
ROT_DIM = HEAD_DIM // 4
ROPE_THETA = 500000.0
NORM_EPS = 1e-6
Q_BLOCK = 128

A_HEADS = 8
A_PATTERNS = ((128, 1), (512, 4), (2048, 16))
A_W = A_HEADS * HEAD_DIM
B_HEADS = 4
B_VDIM = 2 * HEAD_DIM
B_W = B_HEADS * B_VDIM
C_HEADS = 16
C_KV_HEADS = 4
IDX_HEADS = 8
IDX_DIM = 64
TOPK_MAX = 256
D_FF = 4 * D_MODEL

N_EVEN = (DEPTH + 1) // 2
N_ODD = DEPTH // 2
EVEN_IN = 3 * A_W + 3 * B_W
EVEN_SPLITS = [A_W, 2 * A_W, 3 * A_W, 3 * A_W + B_W, 3 * A_W + 2 * B_W]
C_QW = C_HEADS * HEAD_DIM
C_KVW = C_KV_HEADS * HEAD_DIM
ODD_IN = C_QW + 2 * C_KVW + IDX_HEADS * IDX_DIM + IDX_DIM + IDX_HEADS
ODD_SPLITS = [C_QW, C_QW + C_KVW, C_QW + 2 * C_KVW,
              C_QW + 2 * C_KVW + IDX_HEADS * IDX_DIM,
              C_QW + 2 * C_KVW + IDX_HEADS * IDX_DIM + IDX_DIM]

kernel_name = "hybrid_dilated_diff_dsa_trunk"


def _rmsnorm(x, g):
    xf = x.astype(jnp.float32)
    y = xf * lax.rsqrt(jnp.mean(xf * xf, axis=-1, keepdims=True) + NORM_EPS)
    return (y * g.astype(jnp.float32)).astype(x.dtype)


def _rope_tables(seq_len):
    pos = jnp.arange(seq_len, dtype=jnp.float32)
    inv_freq = jnp.power(ROPE_THETA, -jnp.arange(0, ROT_DIM, 2, dtype=jnp.float32) / ROT_DIM)
    ang = pos[:, None] * inv_freq[None, :]
    return jnp.cos(ang), jnp.sin(ang)


def _partial_rope(x, cos, sin):
    half = ROT_DIM // 2
    c = cos[:, None, :].astype(x.dtype)
    s = sin[:, None, :].astype(x.dtype)
    x1 = x[..., :half]
    x2 = x[..., half:ROT_DIM]
    return jnp.concatenate([x1 * c - x2 * s, x2 * c + x1 * s, x[..., ROT_DIM:]], axis=-1)


def _dilated_branch(q, k, v, window, dil):
    B, S, H, Dh = q.shape
    ls = S // dil
    blk = window // dil
    nb = -(-ls // blk)
    lp = nb * blk

    def to_blocks(t):
        t = t.reshape(B, ls, dil, H, Dh).transpose(0, 2, 1, 3, 4)
        t = jnp.pad(t, ((0, 0), (0, 0), (0, lp - ls), (0, 0), (0, 0)))
        return t.reshape(B, dil, nb, blk, H, Dh)

    def with_prev(t):
        prev = jnp.pad(t[:, :, :-1], ((0, 0), (0, 0), (1, 0), (0, 0), (0, 0), (0, 0)))
        return jnp.concatenate([prev, t], axis=3)

    qb = to_blocks(q)
    kc = with_prev(to_blocks(k))
    vc = with_prev(to_blocks(v))
    s = jnp.einsum('brnqhd,brnkhd->brnhqk', qb, kc).astype(jnp.float32)
    qi = jnp.arange(nb)[:, None, None] * blk + jnp.arange(blk)[None, :, None]
    kj = (jnp.arange(nb)[:, None, None] - 1) * blk + jnp.arange(2 * blk)[None, None, :]
    rel = qi - kj
    mask = (rel >= 0) & (rel <= blk) & (kj >= 0)
    s = jnp.where(mask[None, None, :, None], s, -jnp.inf)
    m = jnp.max(s, axis=-1)
    p = jnp.exp(s - m[..., None])
    l = jnp.sum(p, axis=-1)
    num = jnp.einsum('brnhqk,brnkhd->brnqhd', p, vc.astype(jnp.float32))

    def stat_back(t):
        t = t.transpose(0, 1, 2, 4, 3).reshape(B, dil, lp, H)[:, :, :ls]
        return t.transpose(0, 2, 1, 3).reshape(B, S, H)

    num = num.reshape(B, dil, lp, H, Dh)[:, :, :ls].transpose(0, 2, 1, 3, 4).reshape(B, S, H, Dh)
    return stat_back(m), stat_back(l), num


def _dilated_attention(q, k, v):
    stats = [_dilated_branch(q, k, v, w, d) for (w, d) in A_PATTERNS]
    m_all = jnp.stack([st[0] for st in stats])
    l_all = jnp.stack([st[1] for st in stats])
    n_all = jnp.stack([st[2] for st in stats])
    w = jnp.exp(m_all - jnp.max(m_all, axis=0))
    return jnp.sum(w[..., None] * n_all, axis=0) / jnp.sum(w * l_all, axis=0)[..., None]


def _diff_attention(q, k, v, lam):
    B, S = q.shape[:2]
    nq = S // Q_BLOCK
    qblocks = q.reshape(B, nq, Q_BLOCK, B_HEADS, 2, HEAD_DIM).swapaxes(0, 1)
    kpos = jnp.arange(S)
    vf = v.astype(jnp.float32)

    def block(args):
        qb, n = args
        s = jnp.einsum('bqhcd,bkhcd->bhcqk', qb, k).astype(jnp.float32)
        qpos = n * Q_BLOCK + jnp.arange(Q_BLOCK)
        mask = kpos[None, :] <= qpos[:, None]
        a = jax.nn.softmax(jnp.where(mask, s, -jnp.inf), axis=-1)
        diff = a[:, :, 0] - lam * a[:, :, 1]
        return jnp.einsum('bhqk,bkhd->bqhd', diff, vf)

    out = lax.map(block, (qblocks, jnp.arange(nq)))
    return out.swapaxes(0, 1).reshape(B, S, B_HEADS, B_VDIM)


def _dsa_attention(q, k, v, qi, ki, wi):
    B, S = q.shape[:2]
    nq = S // Q_BLOCK
    topk = min(TOPK_MAX, S // 4)
    grp = C_HEADS // C_KV_HEADS
    kpos = jnp.arange(S)
    kif = ki.astype(jnp.float32)
    vf = v.astype(jnp.float32)

    def blocks(t):
        return t.reshape(B, nq, Q_BLOCK, *t.shape[2:]).swapaxes(0, 1)

    def block(args):
        qb, qib, wib, n = args
        qpos = n * Q_BLOCK + jnp.arange(Q_BLOCK)
        causal = kpos[None, :] <= qpos[:, None]
        rel = jax.nn.relu(jnp.einsum('bqhd,bkd->bqhk', qib.astype(jnp.float32), kif))
        score = jnp.einsum('bqhk,bqh->bqk', rel, wib.astype(jnp.float32))
        score = jnp.where(causal[None], score, -jnp.inf)
        _, idx = lax.top_k(score, topk)
        valid = idx <= qpos[None, :, None]
        k_sel = jax.vmap(lambda kb_, ib_: kb_[ib_])(k, idx)
        v_sel = jax.vmap(lambda vb_, ib_: vb_[ib_])(vf, idx)
        qg = qb.reshape(B, Q_BLOCK, C_KV_HEADS, grp, HEAD_DIM)
        s = jnp.einsum('bqgjd,bqkgd->bqgjk', qg, k_sel).astype(jnp.float32)
        s = jnp.where(valid[:, :, None, None, :], s, -jnp.inf)
        p = jax.nn.softmax(s, axis=-1)
        o = jnp.einsum('bqgjk,bqkgd->bqgjd', p, v_sel)
        return o.reshape(B, Q_BLOCK, C_QW)

    out = lax.map(block, (blocks(q), blocks(qi), blocks(wi), jnp.arange(nq)))
    return out.swapaxes(0, 1).reshape(B, S, C_QW)


def setup_inputs(seed: int = 0) -> dict:
    key = jax.random.key(seed)
    ks = jax.random.split(key, 16)
    f32 = jnp.float32
    nrm = lambda k, shape, s: jax.random.normal(k, shape, f32) * s
    return {
        "x": nrm(ks[0], (BATCH, SEQ, D_MODEL), 1.0),
        "norm_mix": 1.0 + nrm(ks[1], (DEPTH, D_MODEL), 0.02),
        "norm_ffn": 1.0 + nrm(ks[2], (DEPTH, D_MODEL), 0.02),
        "w_in_even": nrm(ks[3], (N_EVEN, D_MODEL, EVEN_IN), D_MODEL ** -0.5),
        "w_out_even": nrm(ks[4], (N_EVEN, A_W + B_W, D_MODEL), (A_W + B_W) ** -0.5),
        "lambda_q1": nrm(ks[5], (N_EVEN, HEAD_DIM), 0.1),
        "lambda_k1": nrm(ks[6], (N_EVEN, HEAD_DIM), 0.1),
        "lambda_q2": nrm(ks[7], (N_EVEN, HEAD_DIM), 0.1),
        "lambda_k2": nrm(ks[8], (N_EVEN, HEAD_DIM), 0.1),
        "diff_subln": 1.0 + nrm(ks[9], (N_EVEN, B_VDIM), 0.02),
        "w_in_odd": nrm(ks[10], (N_ODD, D_MODEL, ODD_IN), D_MODEL ** -0.5),
        "w_out_odd": nrm(ks[11], (N_ODD, C_QW, D_MODEL), C_QW ** -0.5),
        "w_ffn_up": nrm(ks[12], (DEPTH, D_MODEL, D_FF), D_MODEL ** -0.5),
        "w_ffn_down": nrm(ks[13], (DEPTH, D_FF, D_MODEL), D_FF ** -0.5),
        "norm_final": 1.0 + nrm(ks[14], (D_MODEL,), 0.02),
    }


def reference(x, norm_mix, norm_ffn, w_in_even, w_out_even, lambda_q1, lambda_k1,
              lambda_q2, lambda_k2, diff_subln, w_in_odd, w_out_odd, w_ffn_up,
              w_ffn_down, norm_final):
    B, S, _ = x.shape
    cos, sin = _rope_tables(S)
    scale = HEAD_DIM ** -0.5
    for layer in range(DEPTH):
        h = _rmsnorm(x, norm_mix[layer])
        if layer % 2 == 0:
            e = layer // 2
            proj = h @ w_in_even[e]
            qa, ka, va, qb, kb, vb = jnp.split(proj, EVEN_SPLITS, axis=-1)
            qa = _partial_rope(qa.reshape(B, S, A_HEADS, HEAD_DIM), cos, sin) * scale
            ka = _partial_rope(ka.reshape(B, S, A_HEADS, HEAD_DIM), cos, sin)
            va = va.reshape(B, S, A_HEADS, HEAD_DIM)
            out_a = _dilated_attention(qa, ka, va).astype(x.dtype).reshape(B, S, A_W)
            qb = (_partial_rope(qb.reshape(B, S, 2 * B_HEADS, HEAD_DIM), cos, sin) * scale
                  ).reshape(B, S, B_HEADS, 2, HEAD_DIM)
            kb = _partial_rope(kb.reshape(B, S, 2 * B_HEADS, HEAD_DIM), cos, sin
                               ).reshape(B, S, B_HEADS, 2, HEAD_DIM)
            vb = vb.reshape(B, S, B_HEADS, B_VDIM)
            lam_init = 0.8 - 0.6 * math.exp(-0.3 * layer)
            lam = (jnp.exp(jnp.sum((lambda_q1[e] * lambda_k1[e]).astype(jnp.float32)))
                   - jnp.exp(jnp.sum((lambda_q2[e] * lambda_k2[e]).astype(jnp.float32)))
                   + lam_init)
            ob = _rmsnorm(_diff_attention(qb, kb, vb, lam), diff_subln[e]) * (1.0 - lam_init)
            out_b = ob.astype(x.dtype).reshape(B, S, B_W)
            mixed = jnp.concatenate([out_a, out_b], axis=-1) @ w_out_even[e]
        else:
            o = layer // 2
            proj = h @ w_in_odd[o]
            q, k, v, qi, ki, wi = jnp.split(proj, ODD_SPLITS, axis=-1)
            q = _partial_rope(q.reshape(B, S, C_HEADS, HEAD_DIM), cos, sin) * scale
            k = _partial_rope(k.reshape(B, S, C_KV_HEADS, HEAD_DIM), cos, sin)
            v = v.reshape(B, S, C_KV_HEADS, HEAD_DIM)
            qi = _partial_rope(qi.reshape(B, S, IDX_HEADS, IDX_DIM), cos, sin) * (IDX_DIM ** -0.5)
            ki = _partial_rope(ki.reshape(B, S, 1, IDX_DIM), cos, sin)[:, :, 0]
            wi = wi * (IDX_HEADS ** -0.5)
            mixed = _dsa_attention(q, k, v, qi, ki, wi).astype(x.dtype) @ w_out_odd[o]
        x = x + mixed
        h = _rmsnorm(x, norm_ffn[layer])
        x = x + jnp.square(jax.nn.relu(h @ w_ffn_up[layer])) @ w_ffn_down[layer]
    return _rmsnorm(x, norm_final)
```

```python
import math
import os
from contextlib import ExitStack

import numpy as np
import concourse.bass as bass
import concourse.mybir as mybir
from concourse.bass_utils import run_bass_kernel_spmd

F32 = mybir.dt.float32
BF16 = mybir.dt.bfloat16
AF = mybir.ActivationFunctionType
ALU = mybir.AluOpType
AX = mybir.AxisListType

D = 1024
DFF = 4096
EPS = 1e-6
TOPK = 256
NEG = -1.0e30
EPOCH = 10000


class Cfg:
    def __init__(self, S=4096, NSEQ=2, DEPTH=4, NCORES=8, debug=False):
        self.debug = debug
        self.S = S
        self.NSEQ = NSEQ
        self.DEPTH = DEPTH
        self.NCORES = NCORES
        self.NTOK = S * NSEQ
        self.KT = S // 128
        self.NQB = S // 512
        self.NE = (DEPTH + 1) // 2
        self.NO = DEPTH // 2


class Res:
    __slots__ = ("name", "w", "rs", "sem")

    def __init__(self, name):
        self.name = name
        self.w = None
        self.rs = {}
        self.sem = None


class DSem:
    __slots__ = ("h", "val")

    def __init__(self, h):
        self.h = h
        self.val = 0


class Prog:
    CE = ("pe", "act", "dve", "pool")

    def __init__(self, nc, stack):
        self.nc = nc
        self.stack = stack
        self.eng = {"pe": nc.tensor, "act": nc.scalar, "dve": nc.vector, "pool": nc.gpsimd, "sp": nc.sync}
        self.cnt = {e: 0 for e in self.CE}
        self.known = {e: {} for e in self.eng}
        self.snaps = {e: [] for e in self.CE}
        self.dirty = {e: True for e in self.CE}
        self.semh = {}
        self.nsem = 0
        self.nwait = 0
        self.nins = 0
        self.dfree = []
        self.dall = []
        self.downers = []

    def _new_sem(self, name):
        self.nsem += 1
        return self.stack.enter_context(self.nc.semaphore(name))

    def _esem(self, e, idx):
        key = (e, idx)
        if key not in self.semh:
            self.semh[key] = self._new_sem("s_%s_%d" % (e, idx))
        return self.semh[key]

    def _need(self, e, ev, raw):
        if ev is None:
            return
        key, val = ev
        kn = self.known[e]
        if isinstance(key, str):
            if key == e and e == "pe":
                return
            if kn.get(key, 0) >= val:
                return
            idx, v = (val - 1) // EPOCH, (val - 1) % EPOCH + 1
            self.eng[e].wait_ge(self._esem(key, idx), v)
            kn[key] = val
            sn = self.snaps[key]
            lo, hi = 0, len(sn)
            while lo < hi:
                mid = (lo + hi) // 2
                if sn[mid][0] <= val:
                    lo = mid + 1
                else:
                    hi = mid
            if lo > 0:
                for ce, kv in zip(self.CE, sn[lo - 1][1]):
                    if ce != e and kn.get(ce, 0) < kv:
                        kn[ce] = kv
        else:
            if kn.get(key, 0) >= val:
                return
            self.eng[e].wait_ge(key.h, val)
            kn[key] = val
        if e in self.dirty:
            self.dirty[e] = True
        self.nwait += 1

    def _deps(self, e, reads, writes):
        for r in reads:
            self._need(e, r.w, True)
        for w in writes:
            self._need(e, w.w, False)
            for k, v in w.rs.items():
                self._need(e, (k, v), False)

    def _commit(self, ev, reads, writes):
        k, v = ev
        for r in reads:
            if r.rs.get(k, 0) < v:
                r.rs[k] = v
        for w in writes:
            w.w = ev
            w.rs = {}

    def op(self, e, fn, reads=(), writes=()):
        self._deps(e, reads, writes)
        ins = fn()
        self.cnt[e] += 1
        n = self.cnt[e]
        idx = (n - 1) // EPOCH
        ins.then_inc(self._esem(e, idx), 1)
        if self.dirty[e]:
            kn = self.known[e]
            self.snaps[e].append((n, tuple(kn.get(ce, 0) for ce in self.CE)))
            self.dirty[e] = False
        self._commit((e, n), reads, writes)
        self.nins += 1
        return ins

    def dma(self, q, out, in_, owner, reads=(), writes=(), **kw):
        if owner.sem is None:
            if self.dfree:
                owner.sem = self.dfree.pop(0)
            else:
                owner.sem = DSem(self._new_sem("d%d" % len(self.dall)))
                self.dall.append(owner.sem)
            self.downers.append(owner)
        ds = owner.sem
        if ds.val > 0:
            self._need(q, (ds, ds.val), True)
        self._deps(q, reads, writes)
        ins = self.eng[q].dma_start(out=out, in_=in_, **kw)
        ds.val += 16
        ins.then_inc(ds.h, 16)
        self._commit((ds, ds.val), reads, writes)
        self.nins += 1
        return ins

    def barrier(self, release=True):
        evs = [(e, n) for e, n in self.cnt.items() if n > 0]
        for ds in self.dall:
            if ds.val > 0:
                evs.append((ds, ds.val))
        for e in self.eng:
            for ev in evs:
                self._need(e, ev, True)
        if release:
            for o in self.downers:
                self.dfree.append(o.sem)
                o.sem = None
            self.downers = []


def _rope_table(S):
    pos = np.arange(S, dtype=np.float32)
    inv = np.power(np.float32(500000.0), -np.arange(0, 16, 2, dtype=np.float32) / np.float32(16)).astype(np.float32)
    ang = (pos[:, None] * inv[None, :]).astype(np.float32)
    cos = np.cos(ang.astype(np.float64)).astype(np.float32)
    sin = np.sin(ang.astype(np.float64)).astype(np.float32)
    return np.concatenate([np.tile(cos, (1, 32)), np.tile(sin, (1, 32))], axis=1).astype(np.float32)


def _mask_a():
    k = np.arange(128)[:, None]
    j = np.arange(2944)[None, :]
    d = j - 384 - k
    m = ((d >= 0) & (d <= 128)).astype(np.float32)
    m += ((d >= 0) & (d <= 512) & (d % 4 == 0)).astype(np.float32)
    m += ((d >= 0) & (d <= 2048) & (d % 16 == 0)).astype(np.float32)
    return m


def _consts(cfg):
    k = np.arange(128)[:, None]
    u = np.arange(128)[None, :]
    c = {}
    c["c_rope"] = _rope_table(cfg.S)
    c["c_maska"] = _mask_a()
    small = np.zeros((128, 416), np.float32)
    small[:, 0:128] = np.eye(128, dtype=np.float32)
    small[:, 128:256] = (u >= k).astype(np.float32)
    small[:, 256:384] = np.where(u > k, NEG, 0.0)
    small[:, 384:416] = (0.5 ** np.arange(32, dtype=np.float64)).astype(np.float32)[None, :]
    c["c_small"] = small
    return c


class Builder:
    def __init__(self, cfg):
        self.cfg = cfg
        self.nc = bass.Bass("TRN2", target_bir_lowering=False)
        self.uid = 0

    def sb(self, st, shape, dt, name=None):
        self.uid += 1
        nm = "%s_%d" % (name or "t", self.uid)
        t = st.enter_context(self.nc.sbuf_tensor(nm, list(shape), dt))
        return t, Res(nm)

    def dram(self, name, shape, dt, kind=None):
        if kind is None:
            return self.nc.dram_tensor(name, list(shape), dt).ap()
        return self.nc.dram_tensor(name, list(shape), dt, kind=kind).ap()

    def build(self):
        cfg, nc = self.cfg, self.nc
        NT = cfg.NTOK
        NE, NO, DP = cfg.NE, max(cfg.NO, 1), cfg.DEPTH
        I = "ExternalInput"
        self.x_in = self.dram("x", [NT, D], F32, I)
        self.g_mix = self.dram("g_mix", [DP, 128, D], F32, I)
        self.g_ffn = self.dram("g_ffn", [DP, 128, D], F32, I)
        self.g_fin = self.dram("g_fin", [128, D], F32, I)
        self.lamv = self.dram("lamv", [NE, 128, 256], F32, I)
        self.subln = self.dram("subln", [NE, 128, 1], F32, I)
        self.w_ie = self.dram("w_ie", [NE, D, 3072], F32, I)
        self.w_oe = self.dram("w_oe", [NE, D, D], F32, I)
        self.w_io = self.dram("w_io", [NO, D, 2120], F32, I)
        self.w_oo = self.dram("w_oo", [NO, D, D], F32, I)
        self.w_up = self.dram("w_up", [DP, D, DFF], F32, I)
        self.w_dn = self.dram("w_dn", [DP, DFF, D], F32, I)
        self.c_rope = self.dram("c_rope", [cfg.S, 512], F32, I)
        self.c_maska = self.dram("c_maska", [128, 2944], F32, I)
        self.c_small = self.dram("c_small", [128, 416], F32, I)
        self.y_out = self.dram("y", [NT, D], F32, "ExternalOutput")
        self.b_ie = self.dram("b_ie", [NE, D, 3072], BF16)
        self.b_oe = self.dram("b_oe", [NE, D, D], BF16)
        self.b_io = self.dram("b_io", [NO, D, 2120], BF16)
        self.b_oo = self.dram("b_oo", [NO, D, D], BF16)
        self.b_up = self.dram("b_up", [DP, D, DFF], BF16)
        self.b_dn = self.dram("b_dn", [DP, DFF, D], BF16)
        dk = "ExternalOutput" if cfg.debug else None
        self.xs = self.dram("xs", [NT, D], F32, dk)
        self.qkT = self.dram("qkT", [2048, NT], BF16, dk)
        self.vv = self.dram("vv", [NT, 1024], BF16, dk)
        self.wv = self.dram("wv", [NT, 8], F32, dk)
        self.aoT = self.dram("aoT", [1024, NT], BF16, dk)

        with ExitStack() as st:
            P = self.P = Prog(nc, st)
            self.PS = []
            for i in range(6):
                t = st.enter_context(nc.psum_tensor("ps%d" % i, [128, 512], F32))
                self.PS.append((t, Res("ps%d" % i)))
            self.PT = st.enter_context(nc.psum_tensor("pt", [128, 2048], BF16))
            self.rPT = [Res("pta"), Res("ptb")]
            st.enter_context(nc.Block())
            self.ident, self.r_ident = self.sb(st, [128, 128], BF16, "ident")
            self.tri, self.r_tri = self.sb(st, [128, 128], BF16, "tri")
            self.negm, self.r_negm = self.sb(st, [128, 128], F32, "negm")
            self.onesb, self.r_onesb = self.sb(st, [128, 128], BF16, "onesb")
            self.onesf, self.r_onesf = self.sb(st, [128, 128], F32, "onesf")
            self.epst, self.r_eps = self.sb(st, [128, 1], F32, "eps")
            self.pw, self.r_pw = self.sb(st, [128, 32], F32, "pw")
            self.setup_consts()
            self.convert_weights()
            P.barrier()
            for L in range(cfg.DEPTH):
                self.phase1(L)
                P.barrier()
                if L % 2 == 0:
                    self.phase_even(L)
                else:
                    self.phase_odd(L)
                P.barrier()
                self.phase3(L)
                P.barrier()
            print("program: nins=%d nwait=%d nsem=%d" % (P.nins, P.nwait, P.nsem))
        return nc

    def setup_consts(self):
        nc, P = self.nc, self.P
        with ExitStack() as st:
            sm, r_sm = self.sb(st, [128, 416], F32, "csm")
            P.dma("sp", sm[:], self.c_small[:, :], r_sm, writes=[r_sm])
            P.op("dve", lambda: nc.vector.tensor_copy(self.ident[:], sm[:, 0:128]), [r_sm], [self.r_ident])
            P.op("dve", lambda: nc.vector.tensor_copy(self.tri[:], sm[:, 128:256]), [r_sm], [self.r_tri])
            P.op("dve", lambda: nc.vector.tensor_copy(self.negm[:], sm[:, 256:384]), [r_sm], [self.r_negm])
            P.op("dve", lambda: nc.vector.tensor_copy(self.pw[:], sm[:, 384:416]), [r_sm], [self.r_pw])
            P.op("pool", lambda: nc.gpsimd.memset(self.onesb[:], 1.0), [], [self.r_onesb])
            P.op("pool", lambda: nc.gpsimd.memset(self.onesf[:], 1.0), [], [self.r_onesf])
            P.op("pool", lambda: nc.gpsimd.memset(self.epst[:], EPS), [], [self.r_eps])
            P.barrier()

    def convert_weights(self):
        nc, P, cfg = self.nc, self.P, self.cfg
        jobs = []
        for e in range(cfg.NE):
            jobs.append((self.w_ie[e], self.b_ie[e], D, 3072))
            jobs.append((self.w_oe[e], self.b_oe[e], D, D))
        for o in range(cfg.NO):
            jobs.append((self.w_io[o], self.b_io[o], D, 2120))
            jobs.append((self.w_oo[o], self.b_oo[o], D, D))
        for L in range(cfg.DEPTH):
            jobs.append((self.w_up[L], self.b_up[L], D, DFF))
            jobs.append((self.w_dn[L], self.b_dn[L], DFF, D))
        with ExitStack() as st:
            NB = 3
            stg = [self.sb(st, [128, 2048], F32, "wst") for _ in range(NB)]
            obf = [self.sb(st, [128, 2048], BF16, "wob") for _ in range(NB)]
            i = 0
            for (src, dst, R, C) in jobs:
                for r0 in range(0, R, 128):
                    for c0 in range(0, C, 2048):
                        w = min(2048, C - c0)
                        s, rs = stg[i % NB]
                        o, ro = obf[i % NB]
                        P.dma("sp", s[:, 0:w], src[r0:r0 + 128, c0:c0 + w], rs, writes=[rs])
                        eng = ("dve", "pool", "act")[i % 3]
                        if eng == "dve":
                            P.op("dve", lambda: nc.vector.tensor_copy(o[:, 0:w], s[:, 0:w]), [rs], [ro])
                        elif eng == "pool":
                            P.op("pool", lambda: nc.gpsimd.tensor_copy(o[:, 0:w], s[:, 0:w]), [rs], [ro])
                        else:
                            P.op("act", lambda: nc.scalar.copy(o[:, 0:w], s[:, 0:w]), [rs], [ro])
                        P.dma("sp", dst[r0:r0 + 128, c0:c0 + w], o[:, 0:w], ro, reads=[ro])
                        i += 1
            P.barrier()

    def rmsnorm(self, xt, r_x, gB, r_g, h, r_h, stt, r_st, junk, r_junk):
        nc, P = self.nc, self.P
        P.op("act", lambda: nc.scalar.activation(junk, xt, AF.Square, accum_out=stt[:, 0:1]), [r_x], [r_junk, r_st])
        P.op("act", lambda: nc.scalar.activation(stt[:, 1:2], stt[:, 0:1], AF.Sqrt, bias=self.epst[:, 0:1], scale=1.0 / D),
             [r_st, self.r_eps], [r_st])
        P.op("dve", lambda: nc.vector.reciprocal(stt[:, 2:3], stt[:, 1:2]), [r_st], [r_st])
        P.op("dve", lambda: nc.vector.scalar_tensor_tensor(h, xt, stt[:, 2:3], gB, ALU.mult, ALU.mult), [r_x, r_st, r_g], [r_h])

    def transpose_h(self, h, r_h, hT_dst, r_hT):
        nc, P = self.nc, self.P
        for c in range(8):
            P.op("pe", lambda: nc.tensor.transpose(self.PT[:, c * 128:(c + 1) * 128], h[:, c * 128:(c + 1) * 128], self.ident[:]),
                 [r_h, self.r_ident], [self.rPT[0]])
        P.op("act", lambda: nc.scalar.copy(hT_dst, self.PT[:, 0:1024].rearrange("p (c t) -> p c t", c=8)), [self.rPT[0]], [r_hT])

    def phase1(self, L):
        nc, P, cfg = self.nc, self.P, self.cfg
        even = (L % 2 == 0)
        e = L // 2
        F = 3072 if even else 2120
        wsrc = self.b_ie[e] if even else self.b_io[e]
        xsrc = self.x_in if L == 0 else self.xs
        if even:
            spans = [(0, 16), (1536, 16)]
            fm = [(0, 1024, 0), (1536, 2560, 1024)]
            nfull, half = 16, False
        else:
            spans = [(0, 20), (1536, 9)]
            fm = [(0, 1280, 0), (1536, 2112, 1280)]
            nfull, half = 14, True
        with ExitStack() as st:
            W, r_W = self.sb(st, [128, 8, F], BF16, "W")
            gB, r_g = self.sb(st, [128, D], F32, "gB")
            xt = [self.sb(st, [128, D], F32, "xt") for _ in range(2)]
            rc = [self.sb(st, [128, 512], F32, "rc") for _ in range(2)]
            junk, r_junk = self.sb(st, [128, D], BF16, "junk")
            h = [self.sb(st, [128, D], BF16, "h") for _ in range(2)]
            hT = [self.sb(st, [128, 8, 128], BF16, "hT") for _ in range(2)]
            proj = [self.sb(st, [128, F], F32, "proj") for _ in range(2)]
            tmp = [self.sb(st, [128, 160], F32, "rt") for _ in range(4)]
            pb = [self.sb(st, [128, 2048], BF16, "pb") for _ in range(2)]
            qst = [self.sb(st, [128, 16, 512], BF16, "qst") for _ in range(2)]
            vst = [self.sb(st, [128, 4, 1024], BF16, "vst") for _ in range(2)]
            wst = [self.sb(st, [128, 4, 8], F32, "wst") for _ in range(2)]
            stt = [self.sb(st, [128, 4], F32, "stt") for _ in range(2)]

            for c in range(8):
                P.dma("sp", W[:, c, :], wsrc[c * 128:(c + 1) * 128, :], r_W, writes=[r_W])
            P.dma("sp", gB[:], self.g_mix[L], r_g, writes=[r_g])
            ntile = cfg.NTOK // 128

            def load(ti):
                t0 = ti * 128
                x_, rx = xt[ti % 2]
                P.dma("sp", x_[:], xsrc[t0:t0 + 128, :], rx, writes=[rx])
                r_, rr = rc[ti % 2]
                p0 = t0 % cfg.S
                P.dma("sp", r_[:], self.c_rope[p0:p0 + 128, :], rr, writes=[rr])

            load(0)
            for ti in range(ntile):
                if ti + 1 < ntile:
                    load(ti + 1)
                g, j = ti // 4, ti % 4
                x_, rx = xt[ti % 2]
                r_, rr = rc[ti % 2]
                h_, rh = h[ti % 2]
                hT_, rhT = hT[ti % 2]
                pj, rpj = proj[ti % 2]
                pb_, rpb = pb[ti % 2]
                q_, rq = qst[g % 2]
                v_, rv = vst[g % 2]
                w_, rw = wst[g % 2]
                s_, rs = stt[ti % 2]
                self.rmsnorm(x_[:], rx, gB[:], r_g, h_[:], rh, s_, rs, junk[:], r_junk)
                self.transpose_h(h_, rh, hT_[:], rhT)
                nch = (F + 511) // 512
                for ch in range(nch):
                    f0 = ch * 512
                    fw = min(512, F - f0)
                    ps, rps = self.PS[ch % 6]
                    for c in range(8):
                        P.op("pe", lambda: nc.tensor.matmul(ps[:, 0:fw], hT_[:, c, :], W[:, c, f0:f0 + fw], start=(c == 0), stop=(c == 7)),
                             [rhT, r_W], [rps])
                    P.op("act", lambda: nc.scalar.copy(pj[:, f0:f0 + fw], ps[:, 0:fw]), [rps], [rpj])
                for (c0, nh) in spans:
                    v3 = pj[:, c0:c0 + 64 * nh].rearrange("p (h d) -> p h d", d=64)
                    x1, x2 = v3[:, :, 0:8], v3[:, :, 8:16]
                    cs = r_[:, 0:8 * nh].rearrange("p (h d) -> p h d", d=8)
                    sn = r_[:, 256:256 + 8 * nh].rearrange("p (h d) -> p h d", d=8)
                    tt = [t[0][:, 0:8 * nh].rearrange("p (h d) -> p h d", d=8) for t in tmp]
                    rt = [t[1] for t in tmp]
                    P.op("dve", lambda: nc.vector.tensor_tensor(tt[0], x1, cs, ALU.mult), [rpj, rr], [rt[0]])
                    P.op("dve", lambda: nc.vector.tensor_tensor(tt[1], x2, sn, ALU.mult), [rpj, rr], [rt[1]])
                    P.op("dve", lambda: nc.vector.tensor_tensor(tt[2], x2, cs, ALU.mult), [rpj, rr], [rt[2]])
                    P.op("dve", lambda: nc.vector.tensor_tensor(tt[3], x1, sn, ALU.mult), [rpj, rr], [rt[3]])
                    P.op("dve", lambda: nc.vector.tensor_tensor(x1, tt[0], tt[1], ALU.subtract), [rt[0], rt[1]], [rpj])
                    P.op("dve", lambda: nc.vector.tensor_tensor(x2, tt[2], tt[3], ALU.add), [rt[2], rt[3]], [rpj])
                for (a, b, d0) in fm:
                    P.op("act", lambda: nc.scalar.copy(pb_[:, d0:d0 + (b - a)], pj[:, a:b]), [rpj], [rpb])
                for blk in range(nfull):
                    P.op("pe", lambda: nc.tensor.transpose(self.PT[:, blk * 128:(blk + 1) * 128], pb_[:, blk * 128:(blk + 1) * 128], self.ident[:]),
                         [rpb, self.r_ident], [self.rPT[0], self.rPT[1]])
                if half:
                    P.op("pe", lambda: nc.tensor.transpose(self.PT[0:64, nfull * 128:(nfull + 1) * 128], pb_[:, nfull * 128:nfull * 128 + 64], self.ident[:]),
                         [rpb, self.r_ident], [self.rPT[0], self.rPT[1]])
                P.op("dve", lambda: nc.vector.tensor_copy(q_[:, 0:nfull, j * 128:(j + 1) * 128],
                                                          self.PT[:, 0:nfull * 128].rearrange("p (b t) -> p b t", b=nfull)),
                     [self.rPT[0], self.rPT[1]], [rq])
                if half:
                    P.op("dve", lambda: nc.vector.tensor_copy(q_[0:64, nfull, j * 128:(j + 1) * 128], self.PT[0:64, nfull * 128:(nfull + 1) * 128]),
                         [self.rPT[0], self.rPT[1]], [rq])
                if even:
                    P.op("pool", lambda: nc.gpsimd.tensor_copy(v_[:, j, 0:512], pj[:, 1024:1536]), [rpj], [rv])
                    P.op("pool", lambda: nc.gpsimd.tensor_copy(v_[:, j, 512:1024], pj[:, 2560:3072]), [rpj], [rv])
                else:
                    P.op("pool", lambda: nc.gpsimd.tensor_copy(v_[:, j, 0:256], pj[:, 1280:1536]), [rpj], [rv])
                    P.op("pool", lambda: nc.gpsimd.tensor_copy(w_[:, j, :], pj[:, 2112:2120]), [rpj], [rw])
                if j == 3:
                    t0 = g * 512
                    P.dma("sp", self.qkT[0:nfull * 128, t0:t0 + 512].rearrange("(b p) t -> p b t", p=128), q_[:, 0:nfull, :], rq, reads=[rq])
                    if half:
                        P.dma("sp", self.qkT[nfull * 128:nfull * 128 + 64, t0:t0 + 512], q_[0:64, nfull, :], rq, reads=[rq])
                    vw = 1024 if even else 256
                    P.dma("sp", self.vv[t0:t0 + 512, 0:vw].rearrange("(j p) f -> p j f", p=128), v_[:, :, 0:vw], rv, reads=[rv])
                    if not even:
                        P.dma("sp", self.wv[t0:t0 + 512, :].rearrange("(j p) f -> p j f", p=128), w_[:], rw, reads=[rw])

    def pipe_begin(self, la):
        self._pq = []
        self._la = int(os.environ.get('KLA', la))

    def pipe_push(self, front, back):
        front()
        self._pq.append(back)
        if len(self._pq) > self._la:
            self._pq.pop(0)()

    def pipe_flush(self):
        while self._pq:
            self._pq.pop(0)()

    def attn_unit(self, kT_ap, qT_ap, c0, psS, pT, mask_ap, mask_eng, pv_list, post=None):
        nc, P = self.nc, self.P
        (ps, rps), (p_, rp) = psS, pT
        (kap, rk), (qap, rq) = kT_ap, qT_ap

        def front():
            P.op("pe", lambda: nc.tensor.matmul(ps[:, c0:512], kap, qap, start=True, stop=True), [rk, rq], [rps])
            P.op("act", lambda: nc.scalar.activation(p_[:, c0:512], ps[:, c0:512], AF.Exp, scale=0.125), [rps], [rp])
            if mask_ap is not None:
                (dst, map_, rm) = mask_ap
                if mask_eng == "pool":
                    P.op("pool", lambda: nc.gpsimd.tensor_tensor(dst, dst, map_, ALU.mult), [rp, rm], [rp])
                else:
                    P.op("dve", lambda: nc.vector.tensor_tensor(dst, dst, map_, ALU.mult), [rp, rm], [rp])

        def back():
            for (oap, ro, lap, rl, st_, sp_) in pv_list:
                P.op("pe", lambda: nc.tensor.matmul(oap, lap, p_[:, c0:512], start=st_, stop=sp_), [rp, rl], [ro])
            if post is not None:
                post()

        self.pipe_push(front, back)

    def finalize_pair(self, psE, psO, L_, R_, Rs_, ao, r_ao):
        nc, P = self.nc, self.P
        (pe_, rpe), (po_, rpo) = psE, psO
        (L, rL), (R, rR), (Rs, rRs) = L_, R_, Rs_
        P.op("dve", lambda: nc.vector.tensor_copy(L[0:64, :], po_[0:64, :]), [rpo], [rL])
        P.op("dve", lambda: nc.vector.tensor_copy(L[64:128, :], pe_[64:128, :]), [rpe], [rL])
        P.op("dve", lambda: nc.vector.reciprocal(R[:], L[:]), [rL], [rR])
        P.op("dve", lambda: nc.vector.tensor_copy(Rs[0:64, :], R[64:128, :]), [rR], [rRs])
        P.op("dve", lambda: nc.vector.tensor_copy(Rs[64:128, :], R[0:64, :]), [rR], [rRs])
        P.op("dve", lambda: nc.vector.tensor_tensor(ao[0:64, :], pe_[0:64, :], Rs[0:64, :], ALU.mult), [rpe, rRs], [r_ao])
        P.op("dve", lambda: nc.vector.tensor_tensor(ao[64:128, :], po_[64:128, :], Rs[64:128, :], ALU.mult), [rpo, rRs], [r_ao])

    def phase_even(self, L):
        nc, P, cfg = self.nc, self.P, self.cfg
        e = L // 2
        S, KT, NQB = cfg.S, cfg.KT, cfg.NQB
        lam_init = 0.8 - 0.6 * math.exp(-0.3 * L)
        with ExitStack() as st:
            maskA, r_mA = self.sb(st, [128, 2944], BF16, "maskA")
            qT = [self.sb(st, [128, 2, S], BF16, "qT") for _ in range(2)]
            kT = [self.sb(st, [128, S], BF16, "kT") for _ in range(2)]
            Vr = [self.sb(st, [128, KT, 128], BF16, "Vr") for _ in range(2)]
            Vx = [self.sb(st, [128, KT, 2, 192], BF16, "Vx") for _ in range(2)]
            pT = [self.sb(st, [128, 512], BF16, "pT") for _ in range(4)]
            Lb = self.sb(st, [128, 512], F32, "Lb")
            Rb = self.sb(st, [128, 512], F32, "Rb")
            Rsb = self.sb(st, [128, 512], F32, "Rsb")
            y0 = self.sb(st, [128, 512], F32, "y0")
            y1 = self.sb(st, [128, 512], F32, "y1")
            ao = [self.sb(st, [128, 512], BF16, "ao") for _ in range(4)]
            lam_t, r_lam = self.sb(st, [128, 256], F32, "lam")
            lst, r_lst = self.sb(st, [128, 8], F32, "lst")
            ljunk, r_lj = self.sb(st, [128, 64], F32, "ljunk")
            with ExitStack() as st2:
                mstg, r_ms = self.sb(st2, [128, 2944], F32, "mstg")
                P.dma("sp", mstg[:], self.c_maska[:, :], r_ms, writes=[r_ms])
                P.op("dve", lambda: nc.vector.tensor_copy(maskA[:], mstg[:]), [r_ms], [r_mA])
                P.barrier()
            for b in range(2):
                P.op("pool", lambda: nc.gpsimd.memset(Vx[b][0][:], 1.0), [], [Vx[b][1]])
                P.op("pool", lambda: nc.gpsimd.memset(qT[b][0][:], 0.0), [], [qT[b][1]])
            P.dma("sp", lam_t[:], self.lamv[e], r_lam, writes=[r_lam])
            P.dma("sp", lst[:, 6:7], self.subln[e], r_lst, writes=[r_lst])
            P.op("dve", lambda: nc.vector.tensor_tensor(ljunk[:], lam_t[:, 0:64], lam_t[:, 64:128], ALU.mult), [r_lam], [r_lj])
            P.op("dve", lambda: nc.vector.reduce_sum(lst[:, 0:1], ljunk[:], AX.X), [r_lj], [r_lst])
            P.op("dve", lambda: nc.vector.tensor_tensor(ljunk[:], lam_t[:, 128:192], lam_t[:, 192:256], ALU.mult), [r_lam, r_lst], [r_lj])
            P.op("dve", lambda: nc.vector.reduce_sum(lst[:, 1:2], ljunk[:], AX.X), [r_lj], [r_lst])
            P.op("act", lambda: nc.scalar.activation(lst[:, 2:4], lst[:, 0:2], AF.Exp), [r_lst], [r_lst])
            P.op("dve", lambda: nc.vector.scalar_tensor_tensor(lst[:, 4:5], lst[:, 3:4], -lam_init, lst[:, 2:3], ALU.add, ALU.subtract),
                 [r_lst], [r_lst])
            P.op("dve", lambda: nc.vector.tensor_scalar(lst[:, 5:6], lst[:, 6:7], 1.0 - lam_init, None, ALU.mult), [r_lst], [r_lst])

            it = 0
            pti = 0
            aoi = 0
            gi = 0
            y0b = [y0, self.sb(st, [128, 512], F32, "y0b")]
            self.pipe_begin(2)

            def load_group(gidx):
                if gidx >= cfg.NSEQ * 8:
                    return
                s_, r8 = gidx // 8, gidx % 8
                T0_ = s_ * S
                b_ = gidx % 2
                q__, rq_ = qT[b_]
                k__, rk_ = kT[b_]
                vr_, rvr_ = Vr[b_]
                vx_, rvx_ = Vx[b_]
                if r8 < 4:
                    hp_ = r8
                    for e_ in range(2):
                        P.dma("sp", q__[e_ * 64:(e_ + 1) * 64, e_, :], self.qkT[hp_ * 128 + e_ * 64:hp_ * 128 + (e_ + 1) * 64, T0_:T0_ + S], rq_, writes=[rq_])
                    P.dma("sp", k__[:], self.qkT[512 + hp_ * 128:512 + (hp_ + 1) * 128, T0_:T0_ + S], rk_, writes=[rk_])
                    P.dma("sp", vr_[:], self.vv[T0_:T0_ + S, hp_ * 128:(hp_ + 1) * 128].rearrange("(k p) f -> p k f", p=128), rvr_, writes=[rvr_])
                    for eh_ in range(2):
                        P.op("pool", lambda: nc.gpsimd.tensor_copy(vx_[:, :, eh_, 64:128], vr_[:, :, eh_ * 64:(eh_ + 1) * 64]), [rvr_], [rvx_])
                else:
                    hb_ = r8 - 4
                    for e_ in range(2):
                        P.dma("sp", q__[e_ * 64:(e_ + 1) * 64, e_, :], self.qkT[1024 + hb_ * 128 + e_ * 64:1024 + hb_ * 128 + (e_ + 1) * 64, T0_:T0_ + S], rq_, writes=[rq_])
                    P.dma("sp", k__[:], self.qkT[1536 + hb_ * 128:1536 + (hb_ + 1) * 128, T0_:T0_ + S], rk_, writes=[rk_])
                    P.dma("sp", vr_[:], self.vv[T0_:T0_ + S, 512 + hb_ * 128:512 + (hb_ + 1) * 128].rearrange("(k p) f -> p k f", p=128), rvr_, writes=[rvr_])
            for s in range(cfg.NSEQ):
                T0 = s * S
                for hp in range(4):
                    b = it % 2
                    it += 1
                    q_, rq = qT[b]
                    k_, rk = kT[b]
                    vr, rvr = Vr[b]
                    vx, rvx = Vx[b]
                    if it == 1:
                        load_group(0)
                    self.pipe_flush()
                    load_group(it)
                    for qb in range(NQB):
                        Q0 = qb * 512
                        kts = list(range(max(0, 4 * qb - 16), 4 * qb + 4))
                        accE = self.PS[2 + 2 * (gi % 2)]
                        accO = self.PS[3 + 2 * (gi % 2)]
                        gi += 1
                        a_, ra = ao[aoi % 4]
                        aoi += 1

                        def fin(accE=accE, accO=accO, a_=a_, ra=ra, hp=hp, Q0=Q0, T0=T0):
                            self.finalize_pair(accE, accO, Lb, Rb, Rsb, a_, ra)
                            P.dma("sp", self.aoT[hp * 128:(hp + 1) * 128, T0 + Q0:T0 + Q0 + 512], a_[:], ra, reads=[ra])

                        for eh in range(2):
                            po = accE if eh == 0 else accO
                            pr = slice(eh * 64, eh * 64 + 64)
                            for kt in kts:
                                i = kt - 4 * qb
                                c0 = 128 * i if i > 0 else 0
                                j0 = Q0 - 128 * kt + 384
                                p_ = pT[pti % 4]
                                psS = self.PS[pti % 2]
                                meng = "pool" if (pti % 5 == 4 and os.environ.get("KPOOL", "1") == "1") else "dve"
                                pti += 1
                                lhs = vx[:, kt, eh, 64:192] if eh == 0 else vx[:, kt, eh, 0:128]
                                last = (eh == 1 and kt == kts[-1])
                                self.attn_unit((k_[:, kt * 128:(kt + 1) * 128], rk), (q_[:, eh, Q0 + c0:Q0 + 512], rq), c0, psS, p_,
                                               (p_[0][:, c0:512], maskA[:, j0 + c0:j0 + 512], r_mA), meng,
                                               [(po[0][:, c0:512], po[1], lhs, rvx, kt == kts[0], kt == kts[-1])],
                                               post=(fin if last else None))
                for hb in range(4):
                    b = it % 2
                    it += 1
                    q_, rq = qT[b]
                    k_, rk = kT[b]
                    vr, rvr = Vr[b]
                    self.pipe_flush()
                    load_group(it)
                    for qb in range(NQB):
                        Q0 = qb * 512
                        kts = list(range(0, 4 * qb + 4))
                        (Y0, rY0) = y0b[gi % 2]
                        gi += 1
                        a_, ra = ao[aoi % 4]
                        aoi += 1

                        def fin0(Y0=Y0, rY0=rY0):
                            (R, rR) = Rb
                            P.op("dve", lambda: nc.vector.reciprocal(R[:], self.PS[3][0][:]), [self.PS[3][1]], [rR])
                            P.op("dve", lambda: nc.vector.tensor_tensor(Y0[:], self.PS[2][0][:], R[:], ALU.mult), [self.PS[2][1], rR], [rY0])

                        def fin1(Y0=Y0, rY0=rY0, a_=a_, ra=ra, hb=hb, Q0=Q0, T0=T0):
                            (R, rR), (Rs, rRs), (L_, rL) = Rb, Rsb, Lb
                            (Y1, rY1) = y1
                            P.op("dve", lambda: nc.vector.reciprocal(Rs[:], self.PS[5][0][:]), [self.PS[5][1]], [rRs])
                            P.op("dve", lambda: nc.vector.tensor_tensor(Y1[:], self.PS[4][0][:], Rs[:], ALU.mult), [self.PS[4][1], rRs], [rY1])
                            P.op("dve", lambda: nc.vector.scalar_tensor_tensor(Y0[:], Y1[:], lst[:, 4:5], Y0[:], ALU.mult, ALU.add), [rY0, rY1, r_lst], [rY0])
                            P.op("act", lambda: nc.scalar.activation(Y1[:], Y0[:], AF.Square), [rY0], [rY1])
                            pn = self.PS[0]
                            P.op("pe", lambda: nc.tensor.matmul(pn[0][:], self.onesf[:], Y1[:], start=True, stop=True), [rY1, self.r_onesf], [pn[1]])
                            P.op("act", lambda: nc.scalar.activation(L_[:], pn[0][:], AF.Sqrt, bias=self.epst[:, 0:1], scale=1.0 / 128.0),
                                 [pn[1], self.r_eps], [rL])
                            P.op("dve", lambda: nc.vector.reciprocal(Rs[:], L_[:]), [rL], [rRs])
                            P.op("dve", lambda: nc.vector.scalar_tensor_tensor(a_[:], Y0[:], lst[:, 5:6], Rs[:], ALU.mult, ALU.mult), [rY0, rRs, r_lst], [ra])
                            P.dma("sp", self.aoT[512 + hb * 128:512 + (hb + 1) * 128, T0 + Q0:T0 + Q0 + 512], a_[:], ra, reads=[ra])

                        for cc in range(2):
                            po = self.PS[2 + 2 * cc]
                            pl = self.PS[3 + 2 * cc]
                            pr = slice(cc * 64, cc * 64 + 64)
                            for kt in kts:
                                i = kt - 4 * qb
                                c0 = 128 * i if i > 0 else 0
                                p_ = pT[pti % 4]
                                psS = self.PS[pti % 2]
                                pti += 1
                                m = None
                                if i >= 0:
                                    m = (p_[0][:, c0:c0 + 128], self.tri[:], self.r_tri)
                                post = None
                                if kt == kts[-1]:
                                    post = fin0 if cc == 0 else fin1
                                self.attn_unit((k_[:, kt * 128:(kt + 1) * 128], rk), (q_[:, cc, Q0 + c0:Q0 + 512], rq), c0, psS, p_, m, "dve",
                                               [(po[0][:, c0:512], po[1], vr[:, kt, :], rvr, kt == 0, kt == kts[-1]),
                                                (pl[0][:, c0:512], pl[1], self.onesb[:], self.r_onesb, kt == 0, kt == kts[-1])],
                                               post=post)
            self.pipe_flush()

    def phase_odd(self, L):
        nc, P, cfg = self.nc, self.P, self.cfg
        S, KT, NQB = cfg.S, cfg.KT, cfg.NQB
        NIT = 16
        with ExitStack() as st:
            kT2, r_k = self.sb(st, [128, 2, S], BF16, "kT2")
            kiT, r_ki = self.sb(st, [128, S], BF16, "kiT")
            Vx, r_vx = self.sb(st, [128, KT, 4, 192], BF16, "Vx")
            qT = [self.sb(st, [128, 16, 512], BF16, "qT") for _ in range(1)]
            qiT = [self.sb(st, [128, 4, 512], BF16, "qiT") for _ in range(1)]
            wi = [self.sb(st, [128, 4, 8], F32, "wi") for _ in range(2)]
            Ib, r_I = self.sb(st, [128, S], F32, "Ib")
            rl = [self.sb(st, [128, 512], F32, "rl") for _ in range(3)]
            mq, r_mq = self.sb(st, [128, S], BF16, "mq")
            cj, r_cj = mq, r_mq
            mT, r_mT = self.sb(st, [128, KT, 512], BF16, "mT")
            bs, r_bs = self.sb(st, [128, 16], F32, "bs")
            hs, r_hs = self.sb(st, [128, 32], F32, "hs")
            pT = [self.sb(st, [128, 512], BF16, "pT") for _ in range(4)]
            Lb = self.sb(st, [128, 512], F32, "Lb")
            Rb = self.sb(st, [128, 512], F32, "Rb")
            Rsb = self.sb(st, [128, 512], F32, "Rsb")
            ao = [self.sb(st, [128, 512], BF16, "ao") for _ in range(4)]
            P.op("pool", lambda: nc.gpsimd.memset(Vx[:], 1.0), [], [r_vx])
            P.op("pool", lambda: nc.gpsimd.memset(qT[0][0][:], 0.0), [], [qT[0][1]])
            pti = 0
            aoi = 0
            rli = 0
            gi = 0
            self.pipe_begin(2)
            for s in range(cfg.NSEQ):
                T0 = s * S
                for c2 in range(2):
                    P.dma("sp", kT2[:, c2, :], self.qkT[1024 + c2 * 128:1024 + (c2 + 1) * 128, T0:T0 + S], r_k, writes=[r_k])
                for hh in range(2):
                    P.dma("sp", kiT[hh * 64:(hh + 1) * 64, :], self.qkT[1792:1856, T0:T0 + S], r_ki, writes=[r_ki])
                KH = KT // 2
                for vh in range(2):
                    vstg = mq[:, :].rearrange("p (k f) -> p k f", f=256)
                    P.dma("sp", vstg, self.vv[T0 + vh * KH * 128:T0 + (vh + 1) * KH * 128, 0:256].rearrange("(k p) f -> p k f", p=128), r_mq, writes=[r_mq])
                    for g in range(4):
                        P.op("pool", lambda: nc.gpsimd.tensor_copy(Vx[:, vh * KH:(vh + 1) * KH, g, 64:128], vstg[:, :, g * 64:(g + 1) * 64]), [r_mq], [r_vx])
                for qb in range(NQB):
                    Q0 = qb * 512
                    q_, rq = qT[0]
                    qi_, rqi = qiT[0]
                    w_, rw = wi[qb % 2]
                    for g4 in range(4):
                        ph = (g4 % 2) * 64
                        P.dma("sp", q_[ph:ph + 64, g4 * 4:(g4 + 1) * 4, :],
                              self.qkT[g4 * 256:(g4 + 1) * 256, T0 + Q0:T0 + Q0 + 512].rearrange("(h p) t -> p h t", p=64), rq, writes=[rq])
                    P.dma("sp", qi_[:], self.qkT[1280:1792, T0 + Q0:T0 + Q0 + 512].rearrange("(c p) t -> p c t", p=128), rqi, writes=[rqi])
                    P.dma("sp", w_[:], self.wv[T0 + Q0:T0 + Q0 + 512, :].rearrange("(j p) f -> p j f", p=128), rw, writes=[rw])
                    for j in range(4):
                        qt = 4 * qb + j
                        n = (qt + 1) * 128
                        for kb in range((n + 511) // 512):
                            k0 = kb * 512
                            kw = min(512, n - k0)
                            for hi in range(8):
                                pr = slice((hi % 2) * 64, (hi % 2) * 64 + 64)
                                ps, rps = self.PS[rli % 2]
                                r_, rr = rl[rli % 3]
                                rli += 1
                                P.op("pe", lambda: nc.tensor.matmul(ps[:, 0:kw], qi_[pr, hi // 2, j * 128:(j + 1) * 128], kiT[pr, k0:k0 + kw], start=True, stop=True),
                                     [rqi, r_ki], [rps])
                                P.op("act", lambda: nc.scalar.activation(r_[:, 0:kw], ps[:, 0:kw], AF.Relu), [rps], [rr])
                                if hi == 0:
                                    P.op("dve", lambda: nc.vector.tensor_scalar(Ib[:, k0:k0 + kw], r_[:, 0:kw], w_[:, j, 0:1], None, ALU.mult), [rr, rw], [r_I])
                                else:
                                    P.op("dve", lambda: nc.vector.scalar_tensor_tensor(Ib[:, k0:k0 + kw], r_[:, 0:kw], w_[:, j, hi:hi + 1], Ib[:, k0:k0 + kw], ALU.mult, ALU.add),
                                         [rr, rw, r_I], [r_I])
                        P.op("dve", lambda: nc.vector.tensor_reduce(bs[:, 1:2], Ib[:, 0:n], AX.X, ALU.max), [r_I], [r_bs])
                        P.op("dve", lambda: nc.vector.tensor_reduce(bs[:, 0:1], Ib[:, 0:n], AX.X, ALU.min), [r_I], [r_bs])
                        P.op("dve", lambda: nc.vector.tensor_scalar(bs[:, 0:1], bs[:, 0:1], -1.0, None, ALU.add), [r_bs], [r_bs])
                        P.op("dve", lambda: nc.vector.tensor_tensor(Ib[:, qt * 128:n], Ib[:, qt * 128:n], self.negm[:], ALU.add), [r_I, self.r_negm], [r_I])
                        if qt >= 2:
                            P.op("dve", lambda: nc.vector.tensor_scalar(bs[:, 2:3], bs[:, 1:2], bs[:, 0:1], 0.5, ALU.subtract, ALU.mult), [r_bs], [r_bs])
                            P.op("dve", lambda: nc.vector.tensor_scalar(hs[:, 0:NIT + 1], self.pw[:, 0:NIT + 1], bs[:, 2:3], None, ALU.mult), [r_bs, self.r_pw], [r_hs])
                            P.op("dve", lambda: nc.vector.tensor_tensor(bs[:, 3:4], bs[:, 0:1], bs[:, 2:3], ALU.add), [r_bs], [r_bs])
                            for it_ in range(NIT):
                                P.op("dve", lambda: nc.vector.tensor_scalar(mq[:, 0:n], Ib[:, 0:n], bs[:, 3:4], None, ALU.is_ge, ALU.add, accum_out=bs[:, 4:5]),
                                     [r_I, r_bs], [r_mq, r_bs])
                                P.op("dve", lambda: nc.vector.tensor_scalar(bs[:, 5:6], bs[:, 4:5], float(TOPK), 0.5, ALU.is_ge, ALU.subtract), [r_bs], [r_bs])
                                P.op("dve", lambda: nc.vector.scalar_tensor_tensor(bs[:, 3:4], bs[:, 5:6], hs[:, it_:it_ + 1], bs[:, 3:4], ALU.mult, ALU.add), [r_bs, r_hs], [r_bs])
                            P.op("dve", lambda: nc.vector.tensor_tensor(bs[:, 0:1], bs[:, 3:4], hs[:, NIT:NIT + 1], ALU.subtract), [r_bs, r_hs], [r_bs])
                        P.op("dve", lambda: nc.vector.tensor_scalar(mq[:, 0:n], Ib[:, 0:n], bs[:, 0:1], None, ALU.is_ge), [r_I, r_bs], [r_mq])
                        for k1 in range(0, qt + 1, 16):
                            nb = min(16, qt + 1 - k1)
                            for bb in range(nb):
                                kt = k1 + bb
                                P.op("pe", lambda: nc.tensor.transpose(self.PT[:, bb * 128:(bb + 1) * 128], mq[:, kt * 128:(kt + 1) * 128], self.ident[:]),
                                     [r_mq, self.r_ident], [self.rPT[0], self.rPT[1]])
                            P.op("act", lambda: nc.scalar.copy(mT[:, k1:k1 + nb, j * 128:(j + 1) * 128],
                                                               self.PT[:, 0:nb * 128].rearrange("p (b t) -> p b t", b=nb)),
                                 [self.rPT[0], self.rPT[1]], [r_mT])
                    kts = list(range(0, 4 * qb + 4))
                    for hp in range(8):
                        accE = self.PS[2 + 2 * (gi % 2)]
                        accO = self.PS[3 + 2 * (gi % 2)]
                        gi += 1
                        a_, ra = ao[aoi % 4]
                        aoi += 1

                        def fin(accE=accE, accO=accO, a_=a_, ra=ra, hp=hp, Q0=Q0, T0=T0):
                            self.finalize_pair(accE, accO, Lb, Rb, Rsb, a_, ra)
                            P.dma("sp", self.aoT[hp * 128:(hp + 1) * 128, T0 + Q0:T0 + Q0 + 512], a_[:], ra, reads=[ra])

                        for eh in range(2):
                            hd = 2 * hp + eh
                            g = hd // 4
                            po = accE if eh == 0 else accO
                            pr = slice(eh * 64, eh * 64 + 64)
                            for kt in kts:
                                i = kt - 4 * qb
                                c0 = 128 * i if i > 0 else 0
                                p_ = pT[pti % 4]
                                psS = self.PS[pti % 2]
                                meng = "pool" if (pti % 3 == 2) else "dve"
                                pti += 1
                                lhs = Vx[:, kt, g, 64:192] if eh == 0 else Vx[:, kt, g, 0:128]
                                last = (eh == 1 and kt == kts[-1])
                                self.attn_unit((kT2[:, g // 2, kt * 128:(kt + 1) * 128], r_k), (q_[:, hd, c0:512], rq), c0, psS, p_,
                                               (p_[0][:, c0:512], mT[:, kt, c0:512], r_mT), meng,
                                               [(po[0][:, c0:512], po[1], lhs, r_vx, kt == 0, kt == kts[-1])],
                                               post=(fin if last else None))
                    self.pipe_flush()

    def phase3(self, L):
        nc, P, cfg = self.nc, self.P, self.cfg
        even = (L % 2 == 0)
        e = L // 2
        wo = self.b_oe[e] if even else self.b_oo[e]
        xsrc = self.x_in if L == 0 else self.xs
        last = (L == cfg.DEPTH - 1)
        dst = self.y_out if last else self.xs
        NG = cfg.NTOK // 512
        with ExitStack() as st:
            Wd, r_Wd = self.sb(st, [128, 32, D], BF16, "Wd")
            gB, r_g = self.sb(st, [128, D], F32, "gB")
            gF, r_gF = self.sb(st, [128, D], F32, "gF") if last else (None, None)
            NS = 3
            slots = [self.sb(st, [128, 4096], BF16, "ws") for _ in range(NS)]
            aoS = [self.sb(st, [128, 8, 512], BF16, "aoS") for _ in range(1)]
            xt = [self.sb(st, [128, 4, D], F32, "xt") for _ in range(2)]
            h2, r_h2 = self.sb(st, [128, D], BF16, "h2")
            junk, r_junk = self.sb(st, [128, D], BF16, "junk")
            h2T, r_h2T = self.sb(st, [128, 8, 512], BF16, "h2T")
            aT, r_aT = self.sb(st, [128, 32, 512], BF16, "aT")
            sq = [self.sb(st, [128, 512], F32, "sq") for _ in range(2)]
            stt = [self.sb(st, [128, 4], F32, "stt") for _ in range(2)]
            for c8 in range(4):
                P.dma("sp", Wd[:, c8 * 8:(c8 + 1) * 8, :], self.b_dn[L][c8 * 1024:(c8 + 1) * 1024, :].rearrange("(c p) f -> p c f", p=128), r_Wd, writes=[r_Wd])
            P.dma("sp", gB[:], self.g_ffn[L], r_g, writes=[r_g])
            if last:
                P.dma("sp", gF[:], self.g_fin[:, :], r_gF, writes=[r_gF])
            si = [0]
            sti = [0]
            h2x, r_h2x = self.sb(st, [128, 4, D], BF16, "h2x")
            h2Tb = [(h2T, r_h2T), self.sb(st, [128, 8, 512], BF16, "h2Tb")]

            def load_a(g):
                t0 = g * 512
                a_, ra = aoS[0]
                P.dma("sp", a_[:], self.aoT[:, t0:t0 + 512].rearrange("(c p) t -> p c t", p=128), ra, writes=[ra])

            def load_x(g):
                t0 = g * 512
                x_, rx = xt[g % 2]
                P.dma("sp", x_[:], xsrc[t0:t0 + 512, :].rearrange("(j p) f -> p j f", p=128), rx, writes=[rx])

            def F1(g):
                a_, ra = aoS[0]
                x_, rx = xt[g % 2]
                wslots = []
                for hc in range(2):
                    w_, rw = slots[si[0] % NS]
                    si[0] += 1
                    P.dma("sp", w_[:].rearrange("p (c f) -> p c f", c=4), wo[hc * 512:(hc + 1) * 512, :].rearrange("(c p) f -> p c f", p=128), rw, writes=[rw])
                    wslots.append((w_, rw))
                for j in range(4):
                    for half in range(2):
                        ps, rps = self.PS[half]
                        for c in range(8):
                            w_, rw = wslots[c // 4]
                            wv_ = w_[:].rearrange("p (c f) -> p c f", c=4)
                            P.op("pe", lambda: nc.tensor.matmul(ps[:], a_[:, c, j * 128:(j + 1) * 128], wv_[:, c % 4, half * 512:(half + 1) * 512], start=(c == 0), stop=(c == 7)),
                                 [ra, rw], [rps])
                        P.op("dve", lambda: nc.vector.tensor_tensor(x_[:, j, half * 512:(half + 1) * 512], ps[:], x_[:, j, half * 512:(half + 1) * 512], ALU.add),
                             [rps, rx], [rx])
                    s_, rs = stt[sti[0] % 2]
                    sti[0] += 1
                    self.rmsnorm(x_[:, j, :], rx, gB[:], r_g, h2x[:, j, :], r_h2x, s_, rs, junk[:], r_junk)

            def F2(g):
                hT_, rhT = h2Tb[g % 2]
                for j in range(4):
                    for c in range(8):
                        P.op("pe", lambda: nc.tensor.transpose(self.PT[:, c * 128:(c + 1) * 128], h2x[:, j, c * 128:(c + 1) * 128], self.ident[:]),
                             [r_h2x, self.r_ident], [self.rPT[0]])
                    P.op("act", lambda: nc.scalar.copy(hT_[:, :, j * 128:(j + 1) * 128], self.PT[:, 0:1024].rearrange("p (c t) -> p c t", c=8)),
                         [self.rPT[0]], [rhT])

            def U(g):
                hT_, rhT = h2Tb[g % 2]
                for qc in range(8):
                    w_, rw = slots[si[0] % NS]
                    si[0] += 1
                    wv_ = w_[:].rearrange("p (c f) -> p c f", c=8)
                    P.dma("sp", wv_, self.b_up[L][:, qc * 512:(qc + 1) * 512].rearrange("(c p) f -> p c f", p=128), rw, writes=[rw])
                    for f4 in range(4):
                        ffc = qc * 4 + f4
                        ps, rps = self.PS[2 + ffc % 2]
                        s_, rsq = sq[ffc % 2]
                        for c in range(8):
                            P.op("pe", lambda: nc.tensor.matmul(ps[:], wv_[:, c, f4 * 128:(f4 + 1) * 128], hT_[:, c, :], start=(c == 0), stop=(c == 7)),
                                 [rw, rhT], [rps])
                        P.op("act", lambda: nc.scalar.activation(s_[:], ps[:], AF.Square), [rps], [rsq])
                        P.op("dve", lambda: nc.vector.scalar_tensor_tensor(aT[:, ffc, :], ps[:], 0.0, s_[:], ALU.is_gt, ALU.mult), [rps, rsq], [r_aT])

            def Dn(g, jh_list):
                x_, rx = xt[g % 2]
                for (j, half) in jh_list:
                    ps, rps = self.PS[4 + half]
                    for ffc in range(32):
                        P.op("pe", lambda: nc.tensor.matmul(ps[:], aT[:, ffc, j * 128:(j + 1) * 128], Wd[:, ffc, half * 512:(half + 1) * 512], start=(ffc == 0), stop=(ffc == 31)),
                             [r_aT, r_Wd], [rps])
                    P.op("dve", lambda: nc.vector.tensor_tensor(x_[:, j, half * 512:(half + 1) * 512], ps[:], x_[:, j, half * 512:(half + 1) * 512], ALU.add),
                         [rps, rx], [rx])
                    if last and half == 1:
                        s_, rs = stt[sti[0] % 2]
                        sti[0] += 1
                        P.op("act", lambda: nc.scalar.activation(junk[:], x_[:, j, :], AF.Square, accum_out=s_[:, 0:1]), [rx], [r_junk, rs])
                        P.op("act", lambda: nc.scalar.activation(s_[:, 1:2], s_[:, 0:1], AF.Sqrt, bias=self.epst[:, 0:1], scale=1.0 / D), [rs, self.r_eps], [rs])
                        P.op("dve", lambda: nc.vector.reciprocal(s_[:, 2:3], s_[:, 1:2]), [rs], [rs])
                        P.op("dve", lambda: nc.vector.scalar_tensor_tensor(x_[:, j, :], x_[:, j, :], s_[:, 2:3], gF[:], ALU.mult, ALU.mult), [rx, rs, r_gF], [rx])

            jh = [(j, half) for j in range(4) for half in range(2)]
            load_a(0)
            load_x(0)
            F1(0)
            F2(0)
            for g in range(NG):
                t0 = g * 512
                x_, rx = xt[g % 2]
                if g + 1 < NG:
                    load_x(g + 1)
                    load_a(g + 1)
                U(g)
                if g + 1 < NG:
                    F1(g + 1)
                Dn(g, jh[0:2])
                if g + 1 < NG:
                    F2(g + 1)
                Dn(g, jh[2:8])
                P.dma("sp", dst[t0:t0 + 512, :].rearrange("(j p) f -> p j f", p=128), x_[:], rx, reads=[rx])


def _in_maps(cfg, x, norm_mix, norm_ffn, w_in_even, w_out_even, lambda_q1, lambda_k1, lambda_q2, lambda_k2,
             diff_subln, w_in_odd, w_out_odd, w_ffn_up, w_ffn_down, norm_final):
    f = lambda a: np.ascontiguousarray(np.asarray(a, dtype=np.float32))
    NE, NO = cfg.NE, max(cfg.NO, 1)
    bc = lambda a: np.ascontiguousarray(np.broadcast_to(f(a)[:, None, :], (a.shape[0], 128, a.shape[1])))
    lam = np.concatenate([f(lambda_q1)[:NE], f(lambda_k1)[:NE], f(lambda_q2)[:NE], f(lambda_k2)[:NE]], axis=1)
    w_io = f(w_in_odd)[:NO] if cfg.NO > 0 else np.zeros((1, D, 2120), np.float32)
    w_oo = f(w_out_odd)[:NO] if cfg.NO > 0 else np.zeros((1, D, D), np.float32)
    shared = {
        "g_mix": bc(np.asarray(norm_mix)[:cfg.DEPTH]),
        "g_ffn": bc(np.asarray(norm_ffn)[:cfg.DEPTH]),
        "g_fin": np.ascontiguousarray(np.broadcast_to(f(norm_final)[None, :], (128, D))),
        "lamv": np.ascontiguousarray(np.broadcast_to(lam[:, None, :], (NE, 128, 256))),
        "subln": np.ascontiguousarray(f(diff_subln)[:NE][:, :, None]),
        "w_ie": f(w_in_even)[:NE], "w_oe": f(w_out_even)[:NE],
        "w_io": w_io, "w_oo": w_oo,
        "w_up": f(w_ffn_up)[:cfg.DEPTH], "w_dn": f(w_ffn_down)[:cfg.DEPTH],
    }
    shared.update(_consts(cfg))
    xf = f(x).reshape(-1, D)
    maps = []
    for c in range(cfg.NCORES):
        m = dict(shared)
        m["x"] = np.ascontiguousarray(xf[c * cfg.NTOK:(c + 1) * cfg.NTOK])
        maps.append(m)
    return maps


def run_cfg(cfg, **inputs):
    nc = Builder(cfg).build()
    maps = _in_maps(cfg, **inputs)
    res = run_bass_kernel_spmd(nc, maps, core_ids=list(range(cfg.NCORES)))
    out = np.concatenate([np.asarray(r["y"]) for r in res.results], axis=0)
    if cfg.debug:
        return out, res.results
    return out


def kernel(**inputs):
    cfg = Cfg(S=4096, NSEQ=2, DEPTH=4, NCORES=8)
    out = run_cfg(cfg, **inputs)
    return out.reshape(16, 4096, D).astype(np.float32)
```

```python
import math
import os
from contextlib import ExitStack

import numpy as np
import concourse.bass as bass
import concourse.mybir as mybir
from concourse.bass_utils import run_bass_kernel_spmd

F32 = mybir.dt.float32
BF16 = mybir.dt.bfloat16
AF = mybir.ActivationFunctionType
ALU = mybir.AluOpType
AX = mybir.AxisListType

D = 1024
DFF = 4096
EPS = 1e-6
TOPK = 256
NEG = -1.0e30
NEGM = -30000.0
EPOCH = 10000


class Cfg:
    def __init__(self, S=4096, NSEQ=2, DEPTH=4, NCORES=8, debug=False):
        self.debug = debug
        self.S = S
        self.NSEQ = NSEQ
        self.DEPTH = DEPTH
        self.NCORES = NCORES
        self.NTOK = S * NSEQ
        self.KT = S // 128
        self.NQB = S // 512
        self.NE = (DEPTH + 1) // 2
        self.NO = DEPTH // 2


class Res:
    __slots__ = ("name", "w", "rs", "sem")

    def __init__(self, name):
        self.name = name
        self.w = None
        self.rs = {}
        self.sem = None


class DSem:
    __slots__ = ("h", "val")

    def __init__(self, h):
        self.h = h
        self.val = 0


class Prog:
    CE = ("pe", "act", "dve", "pool")

    def __init__(self, nc, stack):
        self.nc = nc
        self.stack = stack
        self.eng = {"pe": nc.tensor, "act": nc.scalar, "dve": nc.vector, "pool": nc.gpsimd, "sp": nc.sync}
        self.cnt = {e: 0 for e in self.CE}
        self.known = {e: {} for e in self.eng}
        self.snaps = {e: [] for e in self.CE}
        self.dirty = {e: True for e in self.CE}
        self.semh = {}
        self.nsem = 0
        self.nwait = 0
        self.nins = 0
        self.dfree = []
        self.dall = []
        self.downers = []

    def _new_sem(self, name):
        self.nsem += 1
        return self.stack.enter_context(self.nc.semaphore(name))

    def _esem(self, e, idx):
        key = (e, idx)
        if key not in self.semh:
            self.semh[key] = self._new_sem("s_%s_%d" % (e, idx))
        return self.semh[key]

    def _need(self, e, ev, raw):
        if ev is None:
            return
        key, val = ev
        kn = self.known[e]
        if isinstance(key, str):
            if key == e and e == "pe":
                return
            if kn.get(key, 0) >= val:
                return
            idx, v = (val - 1) // EPOCH, (val - 1) % EPOCH + 1
            self.eng[e].wait_ge(self._esem(key, idx), v)
            kn[key] = val
            sn = self.snaps[key]
            lo, hi = 0, len(sn)
            while lo < hi:
                mid = (lo + hi) // 2
                if sn[mid][0] <= val:
                    lo = mid + 1
                else:
                    hi = mid
            if lo > 0:
                for ce, kv in zip(self.CE, sn[lo - 1][1]):
                    if ce != e and kn.get(ce, 0) < kv:
                        kn[ce] = kv
        else:
            if kn.get(key, 0) >= val:
                return
            self.eng[e].wait_ge(key.h, val)
            kn[key] = val
        if e in self.dirty:
            self.dirty[e] = True
        self.nwait += 1

    def _deps(self, e, reads, writes):
        for r in reads:
            self._need(e, r.w, True)
        for w in writes:
            self._need(e, w.w, False)
            for k, v in w.rs.items():
                self._need(e, (k, v), False)

    def _commit(self, ev, reads, writes):
        k, v = ev
        for r in reads:
            if r.rs.get(k, 0) < v:
                r.rs[k] = v
        for w in writes:
            w.w = ev
            w.rs = {}

    def op(self, e, fn, reads=(), writes=()):
        self._deps(e, reads, writes)
        ins = fn()
        self.cnt[e] += 1
        n = self.cnt[e]
        idx = (n - 1) // EPOCH
        ins.then_inc(self._esem(e, idx), 1)
        if self.dirty[e]:
            kn = self.known[e]
            self.snaps[e].append((n, tuple(kn.get(ce, 0) for ce in self.CE)))
            self.dirty[e] = False
        self._commit((e, n), reads, writes)
        self.nins += 1
        return ins

    def dma(self, q, out, in_, owner, reads=(), writes=(), **kw):
        if owner.sem is None:
            if self.dfree:
                owner.sem = self.dfree.pop(0)
            else:
                owner.sem = DSem(self._new_sem("d%d" % len(self.dall)))
                self.dall.append(owner.sem)
            self.downers.append(owner)
        ds = owner.sem
        if ds.val > 0:
            self._need(q, (ds, ds.val), True)
        self._deps(q, reads, writes)
        ins = self.eng[q].dma_start(out=out, in_=in_, **kw)
        ds.val += 16
        ins.then_inc(ds.h, 16)
        self._commit((ds, ds.val), reads, writes)
        self.nins += 1
        return ins

    def barrier(self, release=True):
        evs = [(e, n) for e, n in self.cnt.items() if n > 0]
        for ds in self.dall:
            if ds.val > 0:
                evs.append((ds, ds.val))
        for e in self.eng:
            for ev in evs:
                self._need(e, ev, True)
        if release:
            for o in self.downers:
                self.dfree.append(o.sem)
                o.sem = None
            self.downers = []


def _rope_table(S):
    pos = np.arange(S, dtype=np.float32)
    inv = np.power(np.float32(500000.0), -np.arange(0, 16, 2, dtype=np.float32) / np.float32(16)).astype(np.float32)
    ang = (pos[:, None] * inv[None, :]).astype(np.float32)
    cos = np.cos(ang.astype(np.float64)).astype(np.float32)
    sin = np.sin(ang.astype(np.float64)).astype(np.float32)
    return np.concatenate([np.tile(cos, (1, 32)), np.tile(sin, (1, 32))], axis=1).astype(np.float32)


def _mask_a():
    k = np.arange(128)[:, None]
    j = np.arange(2944)[None, :]
    d = j - 384 - k
    m = ((d >= 0) & (d <= 128)).astype(np.float32)
    m += ((d >= 0) & (d <= 512) & (d % 4 == 0)).astype(np.float32)
    m += ((d >= 0) & (d <= 2048) & (d % 16 == 0)).astype(np.float32)
    return m


def _consts(cfg):
    k = np.arange(128)[:, None]
    u = np.arange(128)[None, :]
    c = {}
    c["c_rope"] = _rope_table(cfg.S)
    c["c_maska"] = _mask_a()
    small = np.zeros((128, 416), np.float32)
    small[:, 0:128] = np.eye(128, dtype=np.float32)
    small[:, 128:256] = (u >= k).astype(np.float32)
    small[:, 256:384] = np.where(u > k, NEG, 0.0)
    small[:, 384:416] = (0.5 ** np.arange(32, dtype=np.float64)).astype(np.float32)[None, :]
    c["c_small"] = small
    return c


class Builder:
    def __init__(self, cfg):
        self.cfg = cfg
        self.nc = bass.Bass("TRN2", target_bir_lowering=False)
        self.uid = 0

    def sb(self, st, shape, dt, name=None):
        self.uid += 1
        nm = "%s_%d" % (name or "t", self.uid)
        t = st.enter_context(self.nc.sbuf_tensor(nm, list(shape), dt))
        return t, Res(nm)

    def dram(self, name, shape, dt, kind=None):
        if kind is None:
            return self.nc.dram_tensor(name, list(shape), dt).ap()
        return self.nc.dram_tensor(name, list(shape), dt, kind=kind).ap()

    def build(self):
        cfg, nc = self.cfg, self.nc
        NT = cfg.NTOK
        NE, NO, DP = cfg.NE, max(cfg.NO, 1), cfg.DEPTH
        I = "ExternalInput"
        self.x_in = self.dram("x", [NT, D], F32, I)
        self.g_mix = self.dram("g_mix", [DP, 128, D], F32, I)
        self.g_ffn = self.dram("g_ffn", [DP, 128, D], F32, I)
        self.g_fin = self.dram("g_fin", [128, D], F32, I)
        self.lamv = self.dram("lamv", [NE, 128, 256], F32, I)
        self.subln = self.dram("subln", [NE, 128, 1], F32, I)
        self.w_ie = self.dram("w_ie", [NE, D, 3072], F32, I)
        self.w_oe = self.dram("w_oe", [NE, D, D], F32, I)
        self.w_io = self.dram("w_io", [NO, D, 2120], F32, I)
        self.w_oo = self.dram("w_oo", [NO, D, D], F32, I)
        self.w_up = self.dram("w_up", [DP, D, DFF], F32, I)
        self.w_dn = self.dram("w_dn", [DP, DFF, D], F32, I)
        self.c_rope = self.dram("c_rope", [cfg.S, 512], F32, I)
        self.c_maska = self.dram("c_maska", [128, 2944], F32, I)
        self.c_small = self.dram("c_small", [128, 416], F32, I)
        self.y_out = self.dram("y", [NT, D], F32, "ExternalOutput")
        self.b_ie = self.dram("b_ie", [NE, D, 3072], BF16)
        self.b_oe = self.dram("b_oe", [NE, D, D], BF16)
        self.b_io = self.dram("b_io", [NO, D, 2120], BF16)
        self.b_oo = self.dram("b_oo", [NO, D, D], BF16)
        self.b_up = self.dram("b_up", [DP, D, DFF], BF16)
        self.b_dn = self.dram("b_dn", [DP, DFF, D], BF16)
        dk = "ExternalOutput" if cfg.debug else None
        self.xs = self.dram("xs", [NT, D], F32, dk)
        self.qkT = self.dram("qkT", [2048, NT], BF16, dk)
        self.vv = self.dram("vv", [NT, 1024], BF16, dk)
        self.wv = self.dram("wv", [NT, 8], F32, dk)
        self.aoT = self.dram("aoT", [1024, NT], BF16, dk)

        with ExitStack() as st:
            P = self.P = Prog(nc, st)
            self.PS = []
            for i in range(6):
                t = st.enter_context(nc.psum_tensor("ps%d" % i, [128, 512], F32))
                self.PS.append((t, Res("ps%d" % i)))
            self.PT = st.enter_context(nc.psum_tensor("pt", [128, 2048], BF16))
            self.rPT = [Res("pta"), Res("ptb")]
            st.enter_context(nc.Block())
            self.ident, self.r_ident = self.sb(st, [128, 128], BF16, "ident")
            self.tri, self.r_tri = self.sb(st, [128, 128], BF16, "tri")
            self.negm, self.r_negm = self.sb(st, [128, 128], F32, "negm")
            self.onesb, self.r_onesb = self.sb(st, [128, 128], BF16, "onesb")
            self.onesf, self.r_onesf = self.sb(st, [128, 128], F32, "onesf")
            self.epst, self.r_eps = self.sb(st, [128, 1], F32, "eps")
            self.pw, self.r_pw = self.sb(st, [128, 32], F32, "pw")
            self.setup_consts()
            self.convert_weights()
            P.barrier()
            for L in range(cfg.DEPTH):
                self.phase1(L)
                P.barrier()
                if L % 2 == 0:
                    self.phase_even(L)
                else:
                    self.phase_odd(L)
                P.barrier()
                self.phase3(L)
                P.barrier()
            print("program: nins=%d nwait=%d nsem=%d" % (P.nins, P.nwait, P.nsem))
        return nc

    def setup_consts(self):
        nc, P = self.nc, self.P
        with ExitStack() as st:
            sm, r_sm = self.sb(st, [128, 416], F32, "csm")
            P.dma("sp", sm[:], self.c_small[:, :], r_sm, writes=[r_sm])
            P.op("dve", lambda: nc.vector.tensor_copy(self.ident[:], sm[:, 0:128]), [r_sm], [self.r_ident])
            P.op("dve", lambda: nc.vector.tensor_copy(self.tri[:], sm[:, 128:256]), [r_sm], [self.r_tri])
            P.op("dve", lambda: nc.vector.tensor_copy(self.negm[:], sm[:, 256:384]), [r_sm], [self.r_negm])
            P.op("dve", lambda: nc.vector.tensor_copy(self.pw[:], sm[:, 384:416]), [r_sm], [self.r_pw])
            P.op("pool", lambda: nc.gpsimd.memset(self.onesb[:], 1.0), [], [self.r_onesb])
            P.op("pool", lambda: nc.gpsimd.memset(self.onesf[:], 1.0), [], [self.r_onesf])
            P.op("pool", lambda: nc.gpsimd.memset(self.epst[:], EPS), [], [self.r_eps])
            P.barrier()

    def convert_weights(self):
        nc, P, cfg = self.nc, self.P, self.cfg
        jobs = []
        for e in range(cfg.NE):
            jobs.append((self.w_ie[e], self.b_ie[e], D, 3072))
            jobs.append((self.w_oe[e], self.b_oe[e], D, D))
        for o in range(cfg.NO):
            jobs.append((self.w_io[o], self.b_io[o], D, 2120))
            jobs.append((self.w_oo[o], self.b_oo[o], D, D))
        for L in range(cfg.DEPTH):
            jobs.append((self.w_up[L], self.b_up[L], D, DFF))
            jobs.append((self.w_dn[L], self.b_dn[L], DFF, D))
        with ExitStack() as st:
            NB = 6
            stg = [self.sb(st, [128, 2048], F32, "wst") for _ in range(NB)]
            obf = [self.sb(st, [128, 2048], BF16, "wob") for _ in range(NB)]
            i = 0
            for (src, dst, R, C) in jobs:
                for r0 in range(0, R, 128):
                    for c0 in range(0, C, 2048):
                        w = min(2048, C - c0)
                        s, rs = stg[i % NB]
                        o, ro = obf[i % NB]
                        P.dma("sp", s[:, 0:w], src[r0:r0 + 128, c0:c0 + w], rs, writes=[rs])
                        eng = ("dve", "pool", "act")[i % 3]
                        if eng == "dve":
                            P.op("dve", lambda: nc.vector.tensor_copy(o[:, 0:w], s[:, 0:w]), [rs], [ro])
                        elif eng == "pool":
                            P.op("pool", lambda: nc.gpsimd.tensor_copy(o[:, 0:w], s[:, 0:w]), [rs], [ro])
                        else:
                            P.op("act", lambda: nc.scalar.copy(o[:, 0:w], s[:, 0:w]), [rs], [ro])
                        P.dma("sp", dst[r0:r0 + 128, c0:c0 + w], o[:, 0:w], ro, reads=[ro])
                        i += 1
            P.barrier()

    def rmsnorm(self, xt, r_x, gB, r_g, h, r_h, stt, r_st, junk, r_junk):
        nc, P = self.nc, self.P
        P.op("act", lambda: nc.scalar.activation(junk, xt, AF.Square, accum_out=stt[:, 0:1]), [r_x], [r_junk, r_st])
        P.op("act", lambda: nc.scalar.activation(stt[:, 1:2], stt[:, 0:1], AF.Sqrt, bias=self.epst[:, 0:1], scale=1.0 / D),
             [r_st, self.r_eps], [r_st])
        P.op("dve", lambda: nc.vector.reciprocal(stt[:, 2:3], stt[:, 1:2]), [r_st], [r_st])
        P.op("dve", lambda: nc.vector.scalar_tensor_tensor(h, xt, stt[:, 2:3], gB, ALU.mult, ALU.mult), [r_x, r_st, r_g], [r_h])

    def transpose_h(self, h, r_h, hT_dst, r_hT):
        nc, P = self.nc, self.P
        for c in range(8):
            P.op("pe", lambda: nc.tensor.transpose(self.PT[:, c * 128:(c + 1) * 128], h[:, c * 128:(c + 1) * 128], self.ident[:]),
                 [r_h, self.r_ident], [self.rPT[0]])
        P.op("act", lambda: nc.scalar.copy(hT_dst, self.PT[:, 0:1024].rearrange("p (c t) -> p c t", c=8)), [self.rPT[0]], [r_hT])

    def phase1(self, L):
        nc, P, cfg = self.nc, self.P, self.cfg
        even = (L % 2 == 0)
        e = L // 2
        F = 3072 if even else 2120
        wsrc = self.b_ie[e] if even else self.b_io[e]
        xsrc = self.x_in if L == 0 else self.xs
        if even:
            spans = [(0, 16), (1536, 16)]
            fm = [(0, 1024, 0), (1536, 2560, 1024)]
            nfull, half = 16, False
        else:
            spans = [(0, 20), (1536, 9)]
            fm = [(0, 1280, 0), (1536, 2112, 1280)]
            nfull, half = 14, True
        with ExitStack() as st:
            W, r_W = self.sb(st, [128, 8, F], BF16, "W")
            gB, r_g = self.sb(st, [128, D], F32, "gB")
            xt = [self.sb(st, [128, D], F32, "xt") for _ in range(2)]
            rc = [self.sb(st, [128, 512], F32, "rc") for _ in range(2)]
            junk, r_junk = self.sb(st, [128, D], BF16, "junk")
            h = [self.sb(st, [128, D], BF16, "h") for _ in range(2)]
            hT = [self.sb(st, [128, 8, 128], BF16, "hT") for _ in range(2)]
            proj = [self.sb(st, [128, F], F32, "proj") for _ in range(2)]
            tmp = [self.sb(st, [128, 160], F32, "rt") for _ in range(4)]
            pb = [self.sb(st, [128, 2048], BF16, "pb") for _ in range(2)]
            qst = [self.sb(st, [128, 16, 512], BF16, "qst") for _ in range(2)]
            vst = [self.sb(st, [128, 4, 1024], BF16, "vst") for _ in range(2)]
            wst = [self.sb(st, [128, 4, 8], F32, "wst") for _ in range(2)]
            stt = [self.sb(st, [128, 4], F32, "stt") for _ in range(2)]

            for c in range(8):
                P.dma("sp", W[:, c, :], wsrc[c * 128:(c + 1) * 128, :], r_W, writes=[r_W])
            P.dma("sp", gB[:], self.g_mix[L], r_g, writes=[r_g])
            ntile = cfg.NTOK // 128

            def load(ti):
                t0 = ti * 128
                x_, rx = xt[ti % 2]
                P.dma("sp", x_[:], xsrc[t0:t0 + 128, :], rx, writes=[rx])
                r_, rr = rc[ti % 2]
                p0 = t0 % cfg.S
                P.dma("sp", r_[:], self.c_rope[p0:p0 + 128, :], rr, writes=[rr])

            load(0)
            for ti in range(ntile):
                if ti + 1 < ntile:
                    load(ti + 1)
                g, j = ti // 4, ti % 4
                x_, rx = xt[ti % 2]
                r_, rr = rc[ti % 2]
                h_, rh = h[ti % 2]
                hT_, rhT = hT[ti % 2]
                pj, rpj = proj[ti % 2]
                pb_, rpb = pb[ti % 2]
                q_, rq = qst[g % 2]
                v_, rv = vst[g % 2]
                w_, rw = wst[g % 2]
                s_, rs = stt[ti % 2]
                self.rmsnorm(x_[:], rx, gB[:], r_g, h_[:], rh, s_, rs, junk[:], r_junk)
                self.transpose_h(h_, rh, hT_[:], rhT)
                nch = (F + 511) // 512
                for ch in range(nch):
                    f0 = ch * 512
                    fw = min(512, F - f0)
                    ps, rps = self.PS[ch % 6]
                    for c in range(8):
                        P.op("pe", lambda: nc.tensor.matmul(ps[:, 0:fw], hT_[:, c, :], W[:, c, f0:f0 + fw], start=(c == 0), stop=(c == 7)),
                             [rhT, r_W], [rps])
                    P.op("act", lambda: nc.scalar.copy(pj[:, f0:f0 + fw], ps[:, 0:fw]), [rps], [rpj])
                for (c0, nh) in spans:
                    v3 = pj[:, c0:c0 + 64 * nh].rearrange("p (h d) -> p h d", d=64)
                    x1, x2 = v3[:, :, 0:8], v3[:, :, 8:16]
                    cs = r_[:, 0:8 * nh].rearrange("p (h d) -> p h d", d=8)
                    sn = r_[:, 256:256 + 8 * nh].rearrange("p (h d) -> p h d", d=8)
                    tt = [t[0][:, 0:8 * nh].rearrange("p (h d) -> p h d", d=8) for t in tmp]
                    rt = [t[1] for t in tmp]
                    P.op("dve", lambda: nc.vector.tensor_tensor(tt[0], x1, cs, ALU.mult), [rpj, rr], [rt[0]])
                    P.op("dve", lambda: nc.vector.tensor_tensor(tt[1], x2, sn, ALU.mult), [rpj, rr], [rt[1]])
                    P.op("dve", lambda: nc.vector.tensor_tensor(tt[2], x2, cs, ALU.mult), [rpj, rr], [rt[2]])
                    P.op("dve", lambda: nc.vector.tensor_tensor(tt[3], x1, sn, ALU.mult), [rpj, rr], [rt[3]])
                    P.op("dve", lambda: nc.vector.tensor_tensor(x1, tt[0], tt[1], ALU.subtract), [rt[0], rt[1]], [rpj])
                    P.op("dve", lambda: nc.vector.tensor_tensor(x2, tt[2], tt[3], ALU.add), [rt[2], rt[3]], [rpj])
                for (a, b, d0) in fm:
                    P.op("act", lambda: nc.scalar.copy(pb_[:, d0:d0 + (b - a)], pj[:, a:b]), [rpj], [rpb])
                for blk in range(nfull):
                    P.op("pe", lambda: nc.tensor.transpose(self.PT[:, blk * 128:(blk + 1) * 128], pb_[:, blk * 128:(blk + 1) * 128], self.ident[:]),
                         [rpb, self.r_ident], [self.rPT[0], self.rPT[1]])
                if half:
                    P.op("pe", lambda: nc.tensor.transpose(self.PT[0:64, nfull * 128:(nfull + 1) * 128], pb_[:, nfull * 128:nfull * 128 + 64], self.ident[:]),
                         [rpb, self.r_ident], [self.rPT[0], self.rPT[1]])
                P.op("dve", lambda: nc.vector.tensor_copy(q_[:, 0:nfull, j * 128:(j + 1) * 128],
                                                          self.PT[:, 0:nfull * 128].rearrange("p (b t) -> p b t", b=nfull)),
                     [self.rPT[0], self.rPT[1]], [rq])
                if half:
                    P.op("dve", lambda: nc.vector.tensor_copy(q_[0:64, nfull, j * 128:(j + 1) * 128], self.PT[0:64, nfull * 128:(nfull + 1) * 128]),
                         [self.rPT[0], self.rPT[1]], [rq])
                if even:
                    P.op("pool", lambda: nc.gpsimd.tensor_copy(v_[:, j, 0:512], pj[:, 1024:1536]), [rpj], [rv])
                    P.op("pool", lambda: nc.gpsimd.tensor_copy(v_[:, j, 512:1024], pj[:, 2560:3072]), [rpj], [rv])
                else:
                    P.op("pool", lambda: nc.gpsimd.tensor_copy(v_[:, j, 0:256], pj[:, 1280:1536]), [rpj], [rv])
                    P.op("pool", lambda: nc.gpsimd.tensor_copy(w_[:, j, :], pj[:, 2112:2120]), [rpj], [rw])
                if j == 3:
                    t0 = g * 512
                    P.dma("sp", self.qkT[0:nfull * 128, t0:t0 + 512].rearrange("(b p) t -> p b t", p=128), q_[:, 0:nfull, :], rq, reads=[rq])
                    if half:
                        P.dma("sp", self.qkT[nfull * 128:nfull * 128 + 64, t0:t0 + 512], q_[0:64, nfull, :], rq, reads=[rq])
                    vw = 1024 if even else 256
                    P.dma("sp", self.vv[t0:t0 + 512, 0:vw].rearrange("(j p) f -> p j f", p=128), v_[:, :, 0:vw], rv, reads=[rv])
                    if not even:
                        P.dma("sp", self.wv[t0:t0 + 512, :].rearrange("(j p) f -> p j f", p=128), w_[:], rw, reads=[rw])

    def pipe_begin(self, la):
        self._pq = []
        self._la = int(os.environ.get('KLA', la))

    def pipe_push(self, front, back):
        front()
        self._pq.append(back)
        if len(self._pq) > self._la:
            self._pq.pop(0)()

    def pipe_flush(self):
        while self._pq:
            self._pq.pop(0)()

    def attn_unit(self, kT_ap, qT_ap, c0, psS, pT, mask_ap, mask_eng, pv_list, post=None, add_mask=None):
        nc, P = self.nc, self.P
        (ps, rps), (p_, rp) = psS, pT
        (kap, rk), (qap, rq) = kT_ap, qT_ap

        def front():
            P.op("pe", lambda: nc.tensor.matmul(ps[:, c0:512], kap, qap, start=True, stop=(add_mask is None)), [rk, rq], [rps])
            if add_mask is not None:
                (amap, ram) = add_mask
                P.op("pe", lambda: nc.tensor.matmul(ps[:, c0:512], self.ident[:], amap, start=False, stop=True), [ram, self.r_ident], [rps])
            P.op("act", lambda: nc.scalar.activation(p_[:, c0:512], ps[:, c0:512], AF.Exp, scale=0.125), [rps], [rp])
            if mask_ap is not None:
                (dst, map_, rm) = mask_ap
                if mask_eng == "pool":
                    P.op("pool", lambda: nc.gpsimd.tensor_tensor(dst, dst, map_, ALU.mult), [rp, rm], [rp])
                else:
                    P.op("dve", lambda: nc.vector.tensor_tensor(dst, dst, map_, ALU.mult), [rp, rm], [rp])

        def back():
            for (oap, ro, lap, rl, st_, sp_) in pv_list:
                P.op("pe", lambda: nc.tensor.matmul(oap, lap, p_[:, c0:512], start=st_, stop=sp_), [rp, rl], [ro])
            if post is not None:
                post()

        self.pipe_push(front, back)

    def finalize_pair(self, psE, psO, L_, R_, Rs_, ao, r_ao):
        nc, P = self.nc, self.P
        (pe_, rpe), (po_, rpo) = psE, psO
        (L, rL), (R, rR), (Rs, rRs) = L_, R_, Rs_
        P.op("dve", lambda: nc.vector.tensor_copy(L[0:64, :], po_[0:64, :]), [rpo], [rL])
        P.op("dve", lambda: nc.vector.tensor_copy(L[64:128, :], pe_[64:128, :]), [rpe], [rL])
        P.op("dve", lambda: nc.vector.reciprocal(R[:], L[:]), [rL], [rR])
        P.op("dve", lambda: nc.vector.tensor_copy(Rs[0:64, :], R[64:128, :]), [rR], [rRs])
        P.op("dve", lambda: nc.vector.tensor_copy(Rs[64:128, :], R[0:64, :]), [rR], [rRs])
        P.op("dve", lambda: nc.vector.tensor_tensor(ao[0:64, :], pe_[0:64, :], Rs[0:64, :], ALU.mult), [rpe, rRs], [r_ao])
        P.op("dve", lambda: nc.vector.tensor_tensor(ao[64:128, :], po_[64:128, :], Rs[64:128, :], ALU.mult), [rpo, rRs], [r_ao])

    def phase_even(self, L):
        nc, P, cfg = self.nc, self.P, self.cfg
        e = L // 2
        S, KT, NQB = cfg.S, cfg.KT, cfg.NQB
        lam_init = 0.8 - 0.6 * math.exp(-0.3 * L)
        with ExitStack() as st:
            maskA, r_mA = self.sb(st, [128, 2944], BF16, "maskA")
            qT = [self.sb(st, [128, 2, S], BF16, "qT") for _ in range(2)]
            kT = [self.sb(st, [128, S], BF16, "kT") for _ in range(2)]
            Vr = [self.sb(st, [128, KT, 128], BF16, "Vr") for _ in range(2)]
            Vx = [self.sb(st, [128, KT, 2, 192], BF16, "Vx") for _ in range(2)]
            pT = [self.sb(st, [128, 512], BF16, "pT") for _ in range(6)]
            Lb = self.sb(st, [128, 512], F32, "Lb")
            Rb = self.sb(st, [128, 512], F32, "Rb")
            Rsb = self.sb(st, [128, 512], F32, "Rsb")
            y0 = self.sb(st, [128, 512], F32, "y0")
            y1 = self.sb(st, [128, 512], F32, "y1")
            ao = [self.sb(st, [128, 512], BF16, "ao") for _ in range(4)]
            lam_t, r_lam = self.sb(st, [128, 256], F32, "lam")
            lst, r_lst = self.sb(st, [128, 8], F32, "lst")
            ljunk, r_lj = self.sb(st, [128, 64], F32, "ljunk")
            with ExitStack() as st2:
                mstg, r_ms = self.sb(st2, [128, 2944], F32, "mstg")
                P.dma("sp", mstg[:], self.c_maska[:, :], r_ms, writes=[r_ms])
                P.op("dve", lambda: nc.vector.tensor_copy(maskA[:], mstg[:]), [r_ms], [r_mA])
                P.barrier()
            for b in range(2):
                P.op("pool", lambda: nc.gpsimd.memset(Vx[b][0][:], 1.0), [], [Vx[b][1]])
                P.op("pool", lambda: nc.gpsimd.memset(qT[b][0][:], 0.0), [], [qT[b][1]])
            P.dma("sp", lam_t[:], self.lamv[e], r_lam, writes=[r_lam])
            P.dma("sp", lst[:, 6:7], self.subln[e], r_lst, writes=[r_lst])
            P.op("dve", lambda: nc.vector.tensor_tensor(ljunk[:], lam_t[:, 0:64], lam_t[:, 64:128], ALU.mult), [r_lam], [r_lj])
            P.op("dve", lambda: nc.vector.reduce_sum(lst[:, 0:1], ljunk[:], AX.X), [r_lj], [r_lst])
            P.op("dve", lambda: nc.vector.tensor_tensor(ljunk[:], lam_t[:, 128:192], lam_t[:, 192:256], ALU.mult), [r_lam, r_lst], [r_lj])
            P.op("dve", lambda: nc.vector.reduce_sum(lst[:, 1:2], ljunk[:], AX.X), [r_lj], [r_lst])
            P.op("act", lambda: nc.scalar.activation(lst[:, 2:4], lst[:, 0:2], AF.Exp), [r_lst], [r_lst])
            P.op("dve", lambda: nc.vector.scalar_tensor_tensor(lst[:, 4:5], lst[:, 3:4], -lam_init, lst[:, 2:3], ALU.add, ALU.subtract),
                 [r_lst], [r_lst])
            P.op("dve", lambda: nc.vector.tensor_scalar(lst[:, 5:6], lst[:, 6:7], 1.0 - lam_init, None, ALU.mult), [r_lst], [r_lst])

            it = 0
            pti = 0
            aoi = 0
            gi = 0
            y0b = [y0, self.sb(st, [128, 512], F32, "y0b")]
            self.pipe_begin(3)

            def load_group(gidx):
                if gidx >= cfg.NSEQ * 8:
                    return
                s_, r8 = gidx // 8, gidx % 8
                T0_ = s_ * S
                b_ = gidx % 2
                q__, rq_ = qT[b_]
                k__, rk_ = kT[b_]
                vr_, rvr_ = Vr[b_]
                vx_, rvx_ = Vx[b_]
                if r8 < 4:
                    hp_ = r8
                    for e_ in range(2):
                        P.dma("sp", q__[e_ * 64:(e_ + 1) * 64, e_, :], self.qkT[hp_ * 128 + e_ * 64:hp_ * 128 + (e_ + 1) * 64, T0_:T0_ + S], rq_, writes=[rq_])
                    P.dma("sp", k__[:], self.qkT[512 + hp_ * 128:512 + (hp_ + 1) * 128, T0_:T0_ + S], rk_, writes=[rk_])
                    P.dma("sp", vr_[:], self.vv[T0_:T0_ + S, hp_ * 128:(hp_ + 1) * 128].rearrange("(k p) f -> p k f", p=128), rvr_, writes=[rvr_])
                    for eh_ in range(2):
                        P.op("pool", lambda: nc.gpsimd.tensor_copy(vx_[:, :, eh_, 64:128], vr_[:, :, eh_ * 64:(eh_ + 1) * 64]), [rvr_], [rvx_])
                else:
                    hb_ = r8 - 4
                    for e_ in range(2):
                        P.dma("sp", q__[e_ * 64:(e_ + 1) * 64, e_, :], self.qkT[1024 + hb_ * 128 + e_ * 64:1024 + hb_ * 128 + (e_ + 1) * 64, T0_:T0_ + S], rq_, writes=[rq_])
                    P.dma("sp", k__[:], self.qkT[1536 + hb_ * 128:1536 + (hb_ + 1) * 128, T0_:T0_ + S], rk_, writes=[rk_])
                    P.dma("sp", vr_[:], self.vv[T0_:T0_ + S, 512 + hb_ * 128:512 + (hb_ + 1) * 128].rearrange("(k p) f -> p k f", p=128), rvr_, writes=[rvr_])
            for s in range(cfg.NSEQ):
                T0 = s * S
                for hp in range(4):
                    b = it % 2
                    it += 1
                    q_, rq = qT[b]
                    k_, rk = kT[b]
                    vr, rvr = Vr[b]
                    vx, rvx = Vx[b]
                    if it == 1:
                        load_group(0)
                    self.pipe_flush()
                    load_group(it)
                    for qb in range(NQB):
                        Q0 = qb * 512
                        kts = list(range(max(0, 4 * qb - 16), 4 * qb + 4))
                        accE = self.PS[2 + 2 * (gi % 2)]
                        accO = self.PS[3 + 2 * (gi % 2)]
                        gi += 1
                        a_, ra = ao[aoi % 4]
                        aoi += 1

                        def fin(accE=accE, accO=accO, a_=a_, ra=ra, hp=hp, Q0=Q0, T0=T0):
                            self.finalize_pair(accE, accO, Lb, Rb, Rsb, a_, ra)
                            P.dma("sp", self.aoT[hp * 128:(hp + 1) * 128, T0 + Q0:T0 + Q0 + 512], a_[:], ra, reads=[ra])

                        for eh in range(2):
                            po = accE if eh == 0 else accO
                            pr = slice(eh * 64, eh * 64 + 64)
                            for kt in kts:
                                i = kt - 4 * qb
                                c0 = 128 * i if i > 0 else 0
                                j0 = Q0 - 128 * kt + 384
                                p_ = pT[pti % 6]
                                psS = self.PS[pti % 2]
                                meng = "pool" if (pti % 5 == 4 and os.environ.get("KPOOL", "1") == "1") else "dve"
                                pti += 1
                                lhs = vx[:, kt, eh, 64:192] if eh == 0 else vx[:, kt, eh, 0:128]
                                last = (eh == 1 and kt == kts[-1])
                                self.attn_unit((k_[:, kt * 128:(kt + 1) * 128], rk), (q_[:, eh, Q0 + c0:Q0 + 512], rq), c0, psS, p_,
                                               (p_[0][:, c0:512], maskA[:, j0 + c0:j0 + 512], r_mA), meng,
                                               [(po[0][:, c0:512], po[1], lhs, rvx, kt == kts[0], kt == kts[-1])],
                                               post=(fin if last else None))
                for hb in range(4):
                    b = it % 2
                    it += 1
                    q_, rq = qT[b]
                    k_, rk = kT[b]
                    vr, rvr = Vr[b]
                    self.pipe_flush()
                    load_group(it)
                    for qb in range(NQB):
                        Q0 = qb * 512
                        kts = list(range(0, 4 * qb + 4))
                        (Y0, rY0) = y0b[gi % 2]
                        gi += 1
                        a_, ra = ao[aoi % 4]
                        aoi += 1

                        def fin0(Y0=Y0, rY0=rY0):
                            (R, rR) = Rb
                            P.op("dve", lambda: nc.vector.reciprocal(R[:], self.PS[3][0][:]), [self.PS[3][1]], [rR])
                            P.op("dve", lambda: nc.vector.tensor_tensor(Y0[:], self.PS[2][0][:], R[:], ALU.mult), [self.PS[2][1], rR], [rY0])

                        def fin1(Y0=Y0, rY0=rY0, a_=a_, ra=ra, hb=hb, Q0=Q0, T0=T0):
                            (R, rR), (Rs, rRs), (L_, rL) = Rb, Rsb, Lb
                            (Y1, rY1) = y1
                            P.op("dve", lambda: nc.vector.reciprocal(Rs[:], self.PS[5][0][:]), [self.PS[5][1]], [rRs])
                            P.op("dve", lambda: nc.vector.tensor_tensor(Y1[:], self.PS[4][0][:], Rs[:], ALU.mult), [self.PS[4][1], rRs], [rY1])
                            P.op("dve", lambda: nc.vector.scalar_tensor_tensor(Y0[:], Y1[:], lst[:, 4:5], Y0[:], ALU.mult, ALU.add), [rY0, rY1, r_lst], [rY0])
                            P.op("act", lambda: nc.scalar.activation(Y1[:], Y0[:], AF.Square), [rY0], [rY1])
                            pn = self.PS[0]
                            P.op("pe", lambda: nc.tensor.matmul(pn[0][:], self.onesf[:], Y1[:], start=True, stop=True), [rY1, self.r_onesf], [pn[1]])
                            P.op("act", lambda: nc.scalar.activation(L_[:], pn[0][:], AF.Sqrt, bias=self.epst[:, 0:1], scale=1.0 / 128.0),
                                 [pn[1], self.r_eps], [rL])
                            P.op("dve", lambda: nc.vector.reciprocal(Rs[:], L_[:]), [rL], [rRs])
                            P.op("dve", lambda: nc.vector.scalar_tensor_tensor(a_[:], Y0[:], lst[:, 5:6], Rs[:], ALU.mult, ALU.mult), [rY0, rRs, r_lst], [ra])
                            P.dma("sp", self.aoT[512 + hb * 128:512 + (hb + 1) * 128, T0 + Q0:T0 + Q0 + 512], a_[:], ra, reads=[ra])

                        for cc in range(2):
                            po = self.PS[2 + 2 * cc]
                            pl = self.PS[3 + 2 * cc]
                            pr = slice(cc * 64, cc * 64 + 64)
                            for kt in kts:
                                i = kt - 4 * qb
                                c0 = 128 * i if i > 0 else 0
                                p_ = pT[pti % 6]
                                psS = self.PS[pti % 2]
                                pti += 1
                                m = None
                                if i >= 0:
                                    m = (p_[0][:, c0:c0 + 128], self.tri[:], self.r_tri)
                                post = None
                                if kt == kts[-1]:
                                    post = fin0 if cc == 0 else fin1
                                self.attn_unit((k_[:, kt * 128:(kt + 1) * 128], rk), (q_[:, cc, Q0 + c0:Q0 + 512], rq), c0, psS, p_, m, "dve",
                                               [(po[0][:, c0:512], po[1], vr[:, kt, :], rvr, kt == 0, kt == kts[-1]),
                                                (pl[0][:, c0:512], pl[1], self.onesb[:], self.r_onesb, kt == 0, kt == kts[-1])],
                                               post=post)
            self.pipe_flush()

    def phase_odd(self, L):
        nc, P, cfg = self.nc, self.P, self.cfg
        S, KT, NQB = cfg.S, cfg.KT, cfg.NQB
        NIT = 16
        with ExitStack() as st:
            kT2, r_k = self.sb(st, [128, 2, S], BF16, "kT2")
            kiT, r_ki = self.sb(st, [128, S], BF16, "kiT")
            Vx, r_vx = self.sb(st, [128, KT, 4, 192], BF16, "Vx")
            qT = [self.sb(st, [128, 16, 512], BF16, "qT") for _ in range(1)]
            qiT = [self.sb(st, [128, 4, 512], BF16, "qiT") for _ in range(1)]
            wi = [self.sb(st, [128, 4, 8], F32, "wi") for _ in range(2)]
            Ibs = [self.sb(st, [128, S], F32, "Ib") for _ in range(2)]
            rl = [self.sb(st, [128, 512], F32, "rl") for _ in range(3)]
            mqs = [self.sb(st, [128, S], BF16, "mq") for _ in range(2)]
            mq, r_mq = mqs[0]
            bss = [self.sb(st, [128, 16], F32, "bs") for _ in range(2)]
            hss = [self.sb(st, [128, 64], F32, "hs") for _ in range(2)]
            mT, r_mT = self.sb(st, [128, KT, 512], BF16, "mT")
            pT = [self.sb(st, [128, 512], BF16, "pT") for _ in range(4)]
            Lb = self.sb(st, [128, 512], F32, "Lb")
            Rb = self.sb(st, [128, 512], F32, "Rb")
            Rsb = self.sb(st, [128, 512], F32, "Rsb")
            ao = [self.sb(st, [128, 512], BF16, "ao") for _ in range(4)]
            P.op("pool", lambda: nc.gpsimd.memset(Vx[:], 1.0), [], [r_vx])
            P.op("pool", lambda: nc.gpsimd.memset(qT[0][0][:], 0.0), [], [qT[0][1]])
            pti = 0
            aoi = 0
            rli = 0
            gi = 0
            self.pipe_begin(2)
            for s in range(cfg.NSEQ):
                T0 = s * S
                for c2 in range(2):
                    P.dma("sp", kT2[:, c2, :], self.qkT[1024 + c2 * 128:1024 + (c2 + 1) * 128, T0:T0 + S], r_k, writes=[r_k])
                for hh in range(2):
                    P.dma("sp", kiT[hh * 64:(hh + 1) * 64, :], self.qkT[1792:1856, T0:T0 + S], r_ki, writes=[r_ki])
                KH = KT // 2
                for vh in range(2):
                    vstg = mq[:, :].rearrange("p (k f) -> p k f", f=256)
                    P.dma("sp", vstg, self.vv[T0 + vh * KH * 128:T0 + (vh + 1) * KH * 128, 0:256].rearrange("(k p) f -> p k f", p=128), r_mq, writes=[r_mq])
                    for g in range(4):
                        P.op("pool", lambda: nc.gpsimd.tensor_copy(Vx[:, vh * KH:(vh + 1) * KH, g, 64:128], vstg[:, :, g * 64:(g + 1) * 64]), [r_mq], [r_vx])
                for qb in range(NQB):
                    Q0 = qb * 512
                    q_, rq = qT[0]
                    qi_, rqi = qiT[0]
                    w_, rw = wi[qb % 2]
                    for g4 in range(4):
                        ph = (g4 % 2) * 64
                        P.dma("sp", q_[ph:ph + 64, g4 * 4:(g4 + 1) * 4, :],
                              self.qkT[g4 * 256:(g4 + 1) * 256, T0 + Q0:T0 + Q0 + 512].rearrange("(h p) t -> p h t", p=64), rq, writes=[rq])
                    P.dma("sp", qi_[:], self.qkT[1280:1792, T0 + Q0:T0 + Q0 + 512].rearrange("(c p) t -> p c t", p=128), rqi, writes=[rqi])
                    P.dma("sp", w_[:], self.wv[T0 + Q0:T0 + Q0 + 512, :].rearrange("(j p) f -> p j f", p=128), rw, writes=[rw])
                    def accumulate(j, Ib_, r_I_):
                        nonlocal rli
                        qt = 4 * qb + j
                        n = (qt + 1) * 128
                        for kb in range((n + 511) // 512):
                            k0 = kb * 512
                            kw = min(512, n - k0)
                            for hi in range(8):
                                pr = slice((hi % 2) * 64, (hi % 2) * 64 + 64)
                                ps, rps = self.PS[rli % 2]
                                r_, rr = rl[rli % 3]
                                rli += 1
                                P.op("pe", lambda: nc.tensor.matmul(ps[:, 0:kw], qi_[pr, hi // 2, j * 128:(j + 1) * 128], kiT[pr, k0:k0 + kw], start=True, stop=True),
                                     [rqi, r_ki], [rps])
                                P.op("act", lambda: nc.scalar.activation(r_[:, 0:kw], ps[:, 0:kw], AF.Relu), [rps], [rr])
                                if hi == 0:
                                    P.op("dve", lambda: nc.vector.tensor_scalar(Ib_[:, k0:k0 + kw], r_[:, 0:kw], w_[:, j, 0:1], None, ALU.mult), [rr, rw], [r_I_])
                                else:
                                    P.op("dve", lambda: nc.vector.scalar_tensor_tensor(Ib_[:, k0:k0 + kw], r_[:, 0:kw], w_[:, j, hi:hi + 1], Ib_[:, k0:k0 + kw], ALU.mult, ALU.add),
                                         [rr, rw, r_I_], [r_I_])

                    def setup(j, Ib_, r_I_, bs_, r_bs_, hs_, r_hs_, on_act):
                        qt = 4 * qb + j
                        n = (qt + 1) * 128
                        P.op("dve", lambda: nc.vector.tensor_reduce(bs_[:, 1:2], Ib_[:, 0:n], AX.X, ALU.max), [r_I_], [r_bs_])
                        P.op("dve", lambda: nc.vector.tensor_reduce(bs_[:, 0:1], Ib_[:, 0:n], AX.X, ALU.min), [r_I_], [r_bs_])
                        P.op("dve", lambda: nc.vector.tensor_scalar(bs_[:, 0:1], bs_[:, 0:1], -1.0, None, ALU.add), [r_bs_], [r_bs_])
                        P.op("dve", lambda: nc.vector.tensor_tensor(Ib_[:, qt * 128:n], Ib_[:, qt * 128:n], self.negm[:], ALU.add), [r_I_, self.r_negm], [r_I_])
                        if qt >= 2:
                            P.op("dve", lambda: nc.vector.tensor_scalar(bs_[:, 2:3], bs_[:, 1:2], bs_[:, 0:1], 0.5, ALU.subtract, ALU.mult), [r_bs_], [r_bs_])
                            P.op("dve", lambda: nc.vector.tensor_scalar(hs_[:, 0:NIT + 1], self.pw[:, 0:NIT + 1], bs_[:, 2:3], None, ALU.mult), [r_bs_, self.r_pw], [r_hs_])
                            if on_act:
                                P.op("dve", lambda: nc.vector.tensor_scalar(hs_[:, 32:32 + NIT + 1], self.pw[:, 0:NIT + 1], bs_[:, 2:3], -1.0, ALU.mult, ALU.mult), [r_bs_, self.r_pw], [r_hs_])
                                P.op("dve", lambda: nc.vector.tensor_scalar(bs_[:, 3:4], bs_[:, 0:1], bs_[:, 2:3], -1.0, ALU.add, ALU.mult), [r_bs_], [r_bs_])
                            else:
                                P.op("dve", lambda: nc.vector.tensor_tensor(bs_[:, 3:4], bs_[:, 0:1], bs_[:, 2:3], ALU.add), [r_bs_], [r_bs_])

                    def bis_step_dve(j, it_, Ib_, r_I_, mq_, r_mq_, bs_, r_bs_, hs_, r_hs_):
                        n = (4 * qb + j + 1) * 128
                        P.op("dve", lambda: nc.vector.tensor_scalar(mq_[:, 0:n], Ib_[:, 0:n], bs_[:, 3:4], None, ALU.is_ge, ALU.add, accum_out=bs_[:, 4:5]),
                             [r_I_, r_bs_], [r_mq_, r_bs_])
                        P.op("dve", lambda: nc.vector.tensor_scalar(bs_[:, 5:6], bs_[:, 4:5], float(TOPK), 0.5, ALU.is_ge, ALU.subtract), [r_bs_], [r_bs_])
                        P.op("dve", lambda: nc.vector.scalar_tensor_tensor(bs_[:, 3:4], bs_[:, 5:6], hs_[:, it_:it_ + 1], bs_[:, 3:4], ALU.mult, ALU.add), [r_bs_, r_hs_], [r_bs_])

                    def bis_step_act(j, it_, Ib_, r_I_, mq_, r_mq_, bs_, r_bs_, hs_, r_hs_):
                        n = (4 * qb + j + 1) * 128
                        P.op("act", lambda: nc.scalar.activation(mq_[:, 0:n], Ib_[:, 0:n], AF.Sign, bias=bs_[:, 3:4], scale=1.0, accum_out=bs_[:, 4:5]),
                             [r_I_, r_bs_], [r_mq_, r_bs_])
                        P.op("pool", lambda: nc.gpsimd.tensor_scalar(bs_[:, 5:6], bs_[:, 4:5], float(2 * TOPK - n), 0.5, ALU.is_ge, ALU.subtract), [r_bs_], [r_bs_])
                        P.op("pool", lambda: nc.gpsimd.tensor_scalar(bs_[:, 3:4], bs_[:, 5:6], hs_[:, 32 + it_:32 + it_ + 1], bs_[:, 3:4], ALU.mult, ALU.add), [r_bs_, r_hs_], [r_bs_])

                    def finish(j, Ib_, r_I_, mq_, r_mq_, bs_, r_bs_, hs_, r_hs_, on_act):
                        qt = 4 * qb + j
                        n = (qt + 1) * 128
                        if qt >= 2:
                            if on_act:
                                P.op("dve", lambda: nc.vector.tensor_scalar(bs_[:, 0:1], bs_[:, 3:4], -1.0, hs_[:, NIT:NIT + 1], ALU.mult, ALU.subtract), [r_bs_, r_hs_], [r_bs_])
                            else:
                                P.op("dve", lambda: nc.vector.tensor_tensor(bs_[:, 0:1], bs_[:, 3:4], hs_[:, NIT:NIT + 1], ALU.subtract), [r_bs_, r_hs_], [r_bs_])
                        P.op("dve", lambda: nc.vector.tensor_scalar(mq_[:, 0:n], Ib_[:, 0:n], bs_[:, 0:1], NEGM, ALU.is_lt, ALU.mult), [r_I_, r_bs_], [r_mq_])
                        for k1 in range(0, qt + 1, 16):
                            nb = min(16, qt + 1 - k1)
                            for bb in range(nb):
                                kt = k1 + bb
                                P.op("pe", lambda: nc.tensor.transpose(self.PT[:, bb * 128:(bb + 1) * 128], mq_[:, kt * 128:(kt + 1) * 128], self.ident[:]),
                                     [r_mq_, self.r_ident], [self.rPT[0], self.rPT[1]])
                            P.op("act", lambda: nc.scalar.copy(mT[:, k1:k1 + nb, j * 128:(j + 1) * 128],
                                                               self.PT[:, 0:nb * 128].rearrange("p (b t) -> p b t", b=nb)),
                                 [self.rPT[0], self.rPT[1]], [r_mT])

                    for jp in range(2):
                        tiles = []
                        for t in range(2):
                            j = 2 * jp + t
                            on_act = (t == 0) and os.environ.get("KACTBIS", "1") == "1"
                            (Ib_, r_I_), (mq_, r_mq_), (bs_, r_bs_), (hs_, r_hs_) = Ibs[t], mqs[t], bss[t], hss[t]
                            accumulate(j, Ib_, r_I_)
                            setup(j, Ib_, r_I_, bs_, r_bs_, hs_, r_hs_, on_act)
                            tiles.append((j, Ib_, r_I_, mq_, r_mq_, bs_, r_bs_, hs_, r_hs_, on_act))
                        for it_ in range(NIT):
                            for (j, Ib_, r_I_, mq_, r_mq_, bs_, r_bs_, hs_, r_hs_, on_act) in tiles:
                                if 4 * qb + j >= 2:
                                    if on_act:
                                        bis_step_act(j, it_, Ib_, r_I_, mq_, r_mq_, bs_, r_bs_, hs_, r_hs_)
                                    else:
                                        bis_step_dve(j, it_, Ib_, r_I_, mq_, r_mq_, bs_, r_bs_, hs_, r_hs_)
                        for (j, Ib_, r_I_, mq_, r_mq_, bs_, r_bs_, hs_, r_hs_, on_act) in tiles:
                            finish(j, Ib_, r_I_, mq_, r_mq_, bs_, r_bs_, hs_, r_hs_, on_act)
                    kts = list(range(0, 4 * qb + 4))
                    for hp in range(8):
                        accE = self.PS[2 + 2 * (gi % 2)]
                        accO = self.PS[3 + 2 * (gi % 2)]
                        gi += 1
                        a_, ra = ao[aoi % 4]
                        aoi += 1

                        def fin(accE=accE, accO=accO, a_=a_, ra=ra, hp=hp, Q0=Q0, T0=T0):
                            self.finalize_pair(accE, accO, Lb, Rb, Rsb, a_, ra)
                            P.dma("sp", self.aoT[hp * 128:(hp + 1) * 128, T0 + Q0:T0 + Q0 + 512], a_[:], ra, reads=[ra])

                        for eh in range(2):
                            hd = 2 * hp + eh
                            g = hd // 4
                            po = accE if eh == 0 else accO
                            pr = slice(eh * 64, eh * 64 + 64)
                            for kt in kts:
                                i = kt - 4 * qb
                                c0 = 128 * i if i > 0 else 0
                                p_ = pT[pti % 4]
                                psS = self.PS[pti % 2]
                                meng = "pool" if (pti % 3 == 2) else "dve"
                                pti += 1
                                lhs = Vx[:, kt, g, 64:192] if eh == 0 else Vx[:, kt, g, 0:128]
                                last = (eh == 1 and kt == kts[-1])
                                self.attn_unit((kT2[:, g // 2, kt * 128:(kt + 1) * 128], r_k), (q_[:, hd, c0:512], rq), c0, psS, p_,
                                               None, meng,
                                               [(po[0][:, c0:512], po[1], lhs, r_vx, kt == 0, kt == kts[-1])],
                                               post=(fin if last else None), add_mask=(mT[:, kt, c0:512], r_mT))
                    self.pipe_flush()

    def phase3(self, L):
        nc, P, cfg = self.nc, self.P, self.cfg
        even = (L % 2 == 0)
        e = L // 2
        wo = self.b_oe[e] if even else self.b_oo[e]
        xsrc = self.x_in if L == 0 else self.xs
        last = (L == cfg.DEPTH - 1)
        dst = self.y_out if last else self.xs
        NG = cfg.NTOK // 512
        with ExitStack() as st:
            Wd, r_Wd = self.sb(st, [128, 32, D], BF16, "Wd")
            gB, r_g = self.sb(st, [128, D], F32, "gB")
            gF, r_gF = self.sb(st, [128, D], F32, "gF") if last else (None, None)
            NS = 3
            slots = [self.sb(st, [128, 4096], BF16, "ws") for _ in range(NS)]
            aoS = [self.sb(st, [128, 8, 512], BF16, "aoS") for _ in range(1)]
            xt = [self.sb(st, [128, 4, D], F32, "xt") for _ in range(2)]
            h2, r_h2 = self.sb(st, [128, D], BF16, "h2")
            junk, r_junk = self.sb(st, [128, D], BF16, "junk")
            h2T, r_h2T = self.sb(st, [128, 8, 512], BF16, "h2T")
            aT, r_aT = self.sb(st, [128, 32, 512], BF16, "aT")
            sq = [self.sb(st, [128, 512], F32, "sq") for _ in range(2)]
            stt = [self.sb(st, [128, 4], F32, "stt") for _ in range(2)]
            for c8 in range(4):
                P.dma("sp", Wd[:, c8 * 8:(c8 + 1) * 8, :], self.b_dn[L][c8 * 1024:(c8 + 1) * 1024, :].rearrange("(c p) f -> p c f", p=128), r_Wd, writes=[r_Wd])
            P.dma("sp", gB[:], self.g_ffn[L], r_g, writes=[r_g])
            if last:
                P.dma("sp", gF[:], self.g_fin[:, :], r_gF, writes=[r_gF])
            si = [0]
            sti = [0]
            h2x, r_h2x = self.sb(st, [128, 4, D], BF16, "h2x")
            h2Tb = [(h2T, r_h2T), self.sb(st, [128, 8, 512], BF16, "h2Tb")]

            def load_a(g):
                t0 = g * 512
                a_, ra = aoS[0]
                P.dma("sp", a_[:], self.aoT[:, t0:t0 + 512].rearrange("(c p) t -> p c t", p=128), ra, writes=[ra])

            def load_x(g):
                t0 = g * 512
                x_, rx = xt[g % 2]
                P.dma("sp", x_[:], xsrc[t0:t0 + 512, :].rearrange("(j p) f -> p j f", p=128), rx, writes=[rx])

            def F1(g):
                a_, ra = aoS[0]
                x_, rx = xt[g % 2]
                wslots = []
                for hc in range(2):
                    w_, rw = slots[si[0] % NS]
                    si[0] += 1
                    P.dma("sp", w_[:].rearrange("p (c f) -> p c f", c=4), wo[hc * 512:(hc + 1) * 512, :].rearrange("(c p) f -> p c f", p=128), rw, writes=[rw])
                    wslots.append((w_, rw))
                for j in range(4):
                    for half in range(2):
                        ps, rps = self.PS[half]
                        for c in range(8):
                            w_, rw = wslots[c // 4]
                            wv_ = w_[:].rearrange("p (c f) -> p c f", c=4)
                            P.op("pe", lambda: nc.tensor.matmul(ps[:], a_[:, c, j * 128:(j + 1) * 128], wv_[:, c % 4, half * 512:(half + 1) * 512], start=(c == 0), stop=(c == 7)),
                                 [ra, rw], [rps])
                        P.op("dve", lambda: nc.vector.tensor_tensor(x_[:, j, half * 512:(half + 1) * 512], ps[:], x_[:, j, half * 512:(half + 1) * 512], ALU.add),
                             [rps, rx], [rx])
                    s_, rs = stt[sti[0] % 2]
                    sti[0] += 1
                    self.rmsnorm(x_[:, j, :], rx, gB[:], r_g, h2x[:, j, :], r_h2x, s_, rs, junk[:], r_junk)

            def F2(g):
                hT_, rhT = h2Tb[g % 2]
                for j in range(4):
                    for c in range(8):
                        P.op("pe", lambda: nc.tensor.transpose(self.PT[:, c * 128:(c + 1) * 128], h2x[:, j, c * 128:(c + 1) * 128], self.ident[:]),
                             [r_h2x, self.r_ident], [self.rPT[0]])
                    P.op("act", lambda: nc.scalar.copy(hT_[:, :, j * 128:(j + 1) * 128], self.PT[:, 0:1024].rearrange("p (c t) -> p c t", c=8)),
                         [self.rPT[0]], [rhT])

            def U(g):
                hT_, rhT = h2Tb[g % 2]
                for qc in range(8):
                    w_, rw = slots[si[0] % NS]
                    si[0] += 1
                    wv_ = w_[:].rearrange("p (c f) -> p c f", c=8)
                    P.dma("sp", wv_, self.b_up[L][:, qc * 512:(qc + 1) * 512].rearrange("(c p) f -> p c f", p=128), rw, writes=[rw])
                    for f4 in range(4):
                        ffc = qc * 4 + f4
                        ps, rps = self.PS[2 + ffc % 2]
                        s_, rsq = sq[ffc % 2]
                        for c in range(8):
                            P.op("pe", lambda: nc.tensor.matmul(ps[:], wv_[:, c, f4 * 128:(f4 + 1) * 128], hT_[:, c, :], start=(c == 0), stop=(c == 7)),
                                 [rw, rhT], [rps])
                        P.op("act", lambda: nc.scalar.activation(s_[:], ps[:], AF.Square), [rps], [rsq])
                        P.op("dve", lambda: nc.vector.scalar_tensor_tensor(aT[:, ffc, :], ps[:], 0.0, s_[:], ALU.is_gt, ALU.mult), [rps, rsq], [r_aT])

            def Dn(g, jh_list):
                x_, rx = xt[g % 2]
                for (j, half) in jh_list:
                    ps, rps = self.PS[4 + half]
                    for ffc in range(32):
                        P.op("pe", lambda: nc.tensor.matmul(ps[:], aT[:, ffc, j * 128:(j + 1) * 128], Wd[:, ffc, half * 512:(half + 1) * 512], start=(ffc == 0), stop=(ffc == 31)),
                             [r_aT, r_Wd], [rps])
                    P.op("dve", lambda: nc.vector.tensor_tensor(x_[:, j, half * 512:(half + 1) * 512], ps[:], x_[:, j, half * 512:(half + 1) * 512], ALU.add),
                         [rps, rx], [rx])
                    if last and half == 1:
                        s_, rs = stt[sti[0] % 2]
                        sti[0] += 1
                        P.op("act", lambda: nc.scalar.activation(junk[:], x_[:, j, :], AF.Square, accum_out=s_[:, 0:1]), [rx], [r_junk, rs])
                        P.op("act", lambda: nc.scalar.activation(s_[:, 1:2], s_[:, 0:1], AF.Sqrt, bias=self.epst[:, 0:1], scale=1.0 / D), [rs, self.r_eps], [rs])
                        P.op("dve", lambda: nc.vector.reciprocal(s_[:, 2:3], s_[:, 1:2]), [rs], [rs])
                        P.op("dve", lambda: nc.vector.scalar_tensor_tensor(x_[:, j, :], x_[:, j, :], s_[:, 2:3], gF[:], ALU.mult, ALU.mult), [rx, rs, r_gF], [rx])

            jh = [(j, half) for j in range(4) for half in range(2)]
            load_a(0)
            load_x(0)
            F1(0)
            F2(0)
            for g in range(NG):
                t0 = g * 512
                x_, rx = xt[g % 2]
                if g + 1 < NG:
                    load_x(g + 1)
                    load_a(g + 1)
                U(g)
                if g + 1 < NG:
                    F1(g + 1)
                Dn(g, jh[0:2])
                if g + 1 < NG:
                    F2(g + 1)
                Dn(g, jh[2:8])
                P.dma("sp", dst[t0:t0 + 512, :].rearrange("(j p) f -> p j f", p=128), x_[:], rx, reads=[rx])


def _in_maps(cfg, x, norm_mix, norm_ffn, w_in_even, w_out_even, lambda_q1, lambda_k1, lambda_q2, lambda_k2,
             diff_subln, w_in_odd, w_out_odd, w_ffn_up, w_ffn_down, norm_final):
    f = lambda a: np.ascontiguousarray(np.asarray(a, dtype=np.float32))
    NE, NO = cfg.NE, max(cfg.NO, 1)
    bc = lambda a: np.ascontiguousarray(np.broadcast_to(f(a)[:, None, :], (a.shape[0], 128, a.shape[1])))
    lam = np.concatenate([f(lambda_q1)[:NE], f(lambda_k1)[:NE], f(lambda_q2)[:NE], f(lambda_k2)[:NE]], axis=1)
    w_io = f(w_in_odd)[:NO] if cfg.NO > 0 else np.zeros((1, D, 2120), np.float32)
    w_oo = f(w_out_odd)[:NO] if cfg.NO > 0 else np.zeros((1, D, D), np.float32)
    shared = {
        "g_mix": bc(np.asarray(norm_mix)[:cfg.DEPTH]),
        "g_ffn": bc(np.asarray(norm_ffn)[:cfg.DEPTH]),
        "g_fin": np.ascontiguousarray(np.broadcast_to(f(norm_final)[None, :], (128, D))),
        "lamv": np.ascontiguousarray(np.broadcast_to(lam[:, None, :], (NE, 128, 256))),
        "subln": np.ascontiguousarray(f(diff_subln)[:NE][:, :, None]),
        "w_ie": f(w_in_even)[:NE], "w_oe": f(w_out_even)[:NE],
        "w_io": w_io, "w_oo": w_oo,
        "w_up": f(w_ffn_up)[:cfg.DEPTH], "w_dn": f(w_ffn_down)[:cfg.DEPTH],
    }
    shared.update(_consts(cfg))
    xf = f(x).reshape(-1, D)
    maps = []
    for c in range(cfg.NCORES):
        m = dict(shared)
        m["x"] = np.ascontiguousarray(xf[c * cfg.NTOK:(c + 1) * cfg.NTOK])
        maps.append(m)
    return maps


def run_cfg(cfg, **inputs):
    nc = Builder(cfg).build()
    maps = _in_maps(cfg, **inputs)
    res = run_bass_kernel_spmd(nc, maps, core_ids=list(range(cfg.NCORES)))
    out = np.concatenate([np.asarray(r["y"]) for r in res.results], axis=0)
    if cfg.debug:
        return out, res.results
    return out


def kernel(**inputs):
    cfg = Cfg(S=4096, NSEQ=2, DEPTH=4, NCORES=8)
    out = run_cfg(cfg, **inputs)
    return out.reshape(16, 4096, D).astype(np.float32)
```

```python
import math
import os
from contextlib import ExitStack

import numpy as np
import concourse.bass as bass
import concourse.mybir as mybir
from concourse.bass_utils import run_bass_kernel_spmd

F32 = mybir.dt.float32
BF16 = mybir.dt.bfloat16
AF = mybir.ActivationFunctionType
ALU = mybir.AluOpType
AX = mybir.AxisListType

D = 1024
DFF = 4096
EPS = 1e-6
TOPK = 256
NEG = -1.0e30
NEGM = -30000.0
EPOCH = 10000


class Cfg:
    def __init__(self, S=4096, NSEQ=2, DEPTH=4, NCORES=8, debug=False):
        self.debug = debug
        self.S = S
        self.NSEQ = NSEQ
        self.DEPTH = DEPTH
        self.NCORES = NCORES
        self.NTOK = S * NSEQ
        self.KT = S // 128
        self.NQB = S // 512
        self.NE = (DEPTH + 1) // 2
        self.NO = DEPTH // 2


class Res:
    __slots__ = ("name", "w", "rs", "sem")

    def __init__(self, name):
        self.name = name
        self.w = None
        self.rs = {}
        self.sem = None


class DSem:
    __slots__ = ("h", "val")

    def __init__(self, h):
        self.h = h
        self.val = 0


class Prog:
    CE = ("pe", "act", "dve", "pool")

    def __init__(self, nc, stack):
        self.nc = nc
        self.stack = stack
        self.eng = {"pe": nc.tensor, "act": nc.scalar, "dve": nc.vector, "pool": nc.gpsimd, "sp": nc.sync}
        self.cnt = {e: 0 for e in self.CE}
        self.known = {e: {} for e in self.eng}
        self.snaps = {e: [] for e in self.CE}
        self.dirty = {e: True for e in self.CE}
        self.semh = {}
        self.nsem = 0
        self.nwait = 0
        self.nins = 0
        self.dfree = []
        self.dall = []
        self.downers = []

    def _new_sem(self, name):
        self.nsem += 1
        return self.stack.enter_context(self.nc.semaphore(name))

    def _esem(self, e, idx):
        key = (e, idx)
        if key not in self.semh:
            self.semh[key] = self._new_sem("s_%s_%d" % (e, idx))
        return self.semh[key]

    def _need(self, e, ev, raw):
        if ev is None:
            return
        key, val = ev
        kn = self.known[e]
        if isinstance(key, str):
            if key == e and e == "pe":
                return
            if kn.get(key, 0) >= val:
                return
            idx, v = (val - 1) // EPOCH, (val - 1) % EPOCH + 1
            self.eng[e].wait_ge(self._esem(key, idx), v)
            kn[key] = val
            sn = self.snaps[key]
            lo, hi = 0, len(sn)
            while lo < hi:
                mid = (lo + hi) // 2
                if sn[mid][0] <= val:
                    lo = mid + 1
                else:
                    hi = mid
            if lo > 0:
                for ce, kv in zip(self.CE, sn[lo - 1][1]):
                    if ce != e and kn.get(ce, 0) < kv:
                        kn[ce] = kv
        else:
            if kn.get(key, 0) >= val:
                return
            self.eng[e].wait_ge(key.h, val)
            kn[key] = val
        if e in self.dirty:
            self.dirty[e] = True
        self.nwait += 1

    def _deps(self, e, reads, writes):
        for r in reads:
            self._need(e, r.w, True)
        for w in writes:
            self._need(e, w.w, False)
            for k, v in w.rs.items():
                self._need(e, (k, v), False)

    def _commit(self, ev, reads, writes):
        k, v = ev
        for r in reads:
            if r.rs.get(k, 0) < v:
                r.rs[k] = v
        for w in writes:
            w.w = ev
            w.rs = {}

    def op(self, e, fn, reads=(), writes=()):
        self._deps(e, reads, writes)
        ins = fn()
        self.cnt[e] += 1
        n = self.cnt[e]
        idx = (n - 1) // EPOCH
        ins.then_inc(self._esem(e, idx), 1)
        if self.dirty[e]:
            kn = self.known[e]
            self.snaps[e].append((n, tuple(kn.get(ce, 0) for ce in self.CE)))
            self.dirty[e] = False
        self._commit((e, n), reads, writes)
        self.nins += 1
        return ins

    def dma(self, q, out, in_, owner, reads=(), writes=(), **kw):
        if owner.sem is None:
            if self.dfree:
                owner.sem = self.dfree.pop(0)
            else:
                owner.sem = DSem(self._new_sem("d%d" % len(self.dall)))
                self.dall.append(owner.sem)
            self.downers.append(owner)
        ds = owner.sem
        if ds.val > 0:
            self._need(q, (ds, ds.val), True)
        self._deps(q, reads, writes)
        ins = self.eng[q].dma_start(out=out, in_=in_, **kw)
        ds.val += 16
        ins.then_inc(ds.h, 16)
        self._commit((ds, ds.val), reads, writes)
        self.nins += 1
        return ins

    def barrier(self, release=True):
        evs = [(e, n) for e, n in self.cnt.items() if n > 0]
        for ds in self.dall:
            if ds.val > 0:
                evs.append((ds, ds.val))
        for e in self.eng:
            for ev in evs:
                self._need(e, ev, True)
        if release:
            for o in self.downers:
                self.dfree.append(o.sem)
                o.sem = None
            self.downers = []


def _rope_table(S):
    pos = np.arange(S, dtype=np.float32)
    inv = np.power(np.float32(500000.0), -np.arange(0, 16, 2, dtype=np.float32) / np.float32(16)).astype(np.float32)
    ang = (pos[:, None] * inv[None, :]).astype(np.float32)
    cos = np.cos(ang.astype(np.float64)).astype(np.float32)
    sin = np.sin(ang.astype(np.float64)).astype(np.float32)
    return np.concatenate([np.tile(cos, (1, 32)), np.tile(sin, (1, 32))], axis=1).astype(np.float32)


def _mask_a():
    k = np.arange(128)[:, None]
    j = np.arange(2944)[None, :]
    d = j - 384 - k
    m = ((d >= 0) & (d <= 128)).astype(np.float32)
    m += ((d >= 0) & (d <= 512) & (d % 4 == 0)).astype(np.float32)
    m += ((d >= 0) & (d <= 2048) & (d % 16 == 0)).astype(np.float32)
    return m


def _consts(cfg):
    k = np.arange(128)[:, None]
    u = np.arange(128)[None, :]
    c = {}
    c["c_rope"] = _rope_table(cfg.S)
    c["c_maska"] = _mask_a()
    small = np.zeros((128, 416), np.float32)
    small[:, 0:128] = np.eye(128, dtype=np.float32)
    small[:, 128:256] = (u >= k).astype(np.float32)
    small[:, 256:384] = np.where(u > k, NEG, 0.0)
    small[:, 384:416] = (0.5 ** np.arange(32, dtype=np.float64)).astype(np.float32)[None, :]
    c["c_small"] = small
    return c


class Builder:
    def __init__(self, cfg):
        self.cfg = cfg
        self.nc = bass.Bass("TRN2", target_bir_lowering=False)
        self.uid = 0

    def sb(self, st, shape, dt, name=None):
        self.uid += 1
        nm = "%s_%d" % (name or "t", self.uid)
        t = st.enter_context(self.nc.sbuf_tensor(nm, list(shape), dt))
        return t, Res(nm)

    def dram(self, name, shape, dt, kind=None):
        if kind is None:
            return self.nc.dram_tensor(name, list(shape), dt).ap()
        return self.nc.dram_tensor(name, list(shape), dt, kind=kind).ap()

    def build(self):
        cfg, nc = self.cfg, self.nc
        NT = cfg.NTOK
        NE, NO, DP = cfg.NE, max(cfg.NO, 1), cfg.DEPTH
        I = "ExternalInput"
        self.x_in = self.dram("x", [NT, D], F32, I)
        self.g_mix = self.dram("g_mix", [DP, 128, D], F32, I)
        self.g_ffn = self.dram("g_ffn", [DP, 128, D], F32, I)
        self.g_fin = self.dram("g_fin", [128, D], F32, I)
        self.lamv = self.dram("lamv", [NE, 128, 256], F32, I)
        self.subln = self.dram("subln", [NE, 128, 1], F32, I)
        self.w_ie = self.dram("w_ie", [NE, D, 3072], F32, I)
        self.w_oe = self.dram("w_oe", [NE, D, D], F32, I)
        self.w_io = self.dram("w_io", [NO, D, 2120], F32, I)
        self.w_oo = self.dram("w_oo", [NO, D, D], F32, I)
        self.w_up = self.dram("w_up", [DP, D, DFF], F32, I)
        self.w_dn = self.dram("w_dn", [DP, DFF, D], F32, I)
        self.c_rope = self.dram("c_rope", [cfg.S, 512], F32, I)
        self.c_maska = self.dram("c_maska", [128, 2944], F32, I)
        self.c_small = self.dram("c_small", [128, 416], F32, I)
        self.y_out = self.dram("y", [NT, D], F32, "ExternalOutput")
        self.b_ie = self.dram("b_ie", [NE, D, 3072], BF16)
        self.b_oe = self.dram("b_oe", [NE, D, D], BF16)
        self.b_io = self.dram("b_io", [NO, D, 2120], BF16)
        self.b_oo = self.dram("b_oo", [NO, D, D], BF16)
        self.b_up = self.dram("b_up", [DP, D, DFF], BF16)
        self.b_dn = self.dram("b_dn", [DP, DFF, D], BF16)
        dk = "ExternalOutput" if cfg.debug else None
        self.xs = self.dram("xs", [NT, D], F32, dk)
        self.qkT = self.dram("qkT", [2048, NT], BF16, dk)
        self.vv = self.dram("vv", [NT, 1024], BF16, dk)
        self.wv = self.dram("wv", [NT, 8], F32, dk)
        self.aoT = self.dram("aoT", [1024, NT], BF16, dk)

        with ExitStack() as st:
            P = self.P = Prog(nc, st)
            self.PS = []
            for i in range(6):
                t = st.enter_context(nc.psum_tensor("ps%d" % i, [128, 512], F32))
                self.PS.append((t, Res("ps%d" % i)))
            self.PT = st.enter_context(nc.psum_tensor("pt", [128, 2048], BF16))
            self.rPT = [Res("pta"), Res("ptb")]
            st.enter_context(nc.Block())
            self.ident, self.r_ident = self.sb(st, [128, 128], BF16, "ident")
            self.tri, self.r_tri = self.sb(st, [128, 128], BF16, "tri")
            self.negm, self.r_negm = self.sb(st, [128, 128], F32, "negm")
            self.onesb, self.r_onesb = self.sb(st, [128, 128], BF16, "onesb")
            self.onesf, self.r_onesf = self.sb(st, [128, 128], F32, "onesf")
            self.epst, self.r_eps = self.sb(st, [128, 1], F32, "eps")
            self.pw, self.r_pw = self.sb(st, [128, 32], F32, "pw")
            self.setup_consts()
            self.convert_weights()
            P.barrier()
            for L in range(cfg.DEPTH):
                self.phase1(L)
                P.barrier()
                if L % 2 == 0:
                    self.phase_even(L)
                else:
                    self.phase_odd(L)
                P.barrier()
                self.phase3(L)
                P.barrier()
            print("program: nins=%d nwait=%d nsem=%d" % (P.nins, P.nwait, P.nsem))
        return nc

    def setup_consts(self):
        nc, P = self.nc, self.P
        with ExitStack() as st:
            sm, r_sm = self.sb(st, [128, 416], F32, "csm")
            P.dma("sp", sm[:], self.c_small[:, :], r_sm, writes=[r_sm])
            P.op("dve", lambda: nc.vector.tensor_copy(self.ident[:], sm[:, 0:128]), [r_sm], [self.r_ident])
            P.op("dve", lambda: nc.vector.tensor_copy(self.tri[:], sm[:, 128:256]), [r_sm], [self.r_tri])
            P.op("dve", lambda: nc.vector.tensor_copy(self.negm[:], sm[:, 256:384]), [r_sm], [self.r_negm])
            P.op("dve", lambda: nc.vector.tensor_copy(self.pw[:], sm[:, 384:416]), [r_sm], [self.r_pw])
            P.op("pool", lambda: nc.gpsimd.memset(self.onesb[:], 1.0), [], [self.r_onesb])
            P.op("pool", lambda: nc.gpsimd.memset(self.onesf[:], 1.0), [], [self.r_onesf])
            P.op("pool", lambda: nc.gpsimd.memset(self.epst[:], EPS), [], [self.r_eps])
            P.barrier()

    def convert_weights(self):
        nc, P, cfg = self.nc, self.P, self.cfg
        jobs = []
        for e in range(cfg.NE):
            jobs.append((self.w_ie[e], self.b_ie[e], D, 3072))
            jobs.append((self.w_oe[e], self.b_oe[e], D, D))
        for o in range(cfg.NO):
            jobs.append((self.w_io[o], self.b_io[o], D, 2120))
            jobs.append((self.w_oo[o], self.b_oo[o], D, D))
        for L in range(cfg.DEPTH):
            jobs.append((self.w_up[L], self.b_up[L], D, DFF))
            jobs.append((self.w_dn[L], self.b_dn[L], DFF, D))
        with ExitStack() as st:
            NB = 6
            stg = [self.sb(st, [128, 2048], F32, "wst") for _ in range(NB)]
            obf = [self.sb(st, [128, 2048], BF16, "wob") for _ in range(NB)]
            tiles = []
            for (src, dst, R, C) in jobs:
                for r0 in range(0, R, 128):
                    for c0 in range(0, C, 2048):
                        tiles.append((src, dst, r0, c0, min(2048, C - c0)))

            def ld(i):
                (src, dst, r0, c0, w) = tiles[i]
                s_, rs = stg[i % NB]
                P.dma("sp", s_[:, 0:w], src[r0:r0 + 128, c0:c0 + w], rs, writes=[rs])

            LD = 3
            for i in range(min(LD, len(tiles))):
                ld(i)
            for i in range(len(tiles)):
                if i + LD < len(tiles):
                    ld(i + LD)
                (src, dst, r0, c0, w) = tiles[i]
                s_, rs = stg[i % NB]
                o, ro = obf[i % NB]
                eng = ("dve", "pool", "act")[i % 3]
                if eng == "dve":
                    P.op("dve", lambda: nc.vector.tensor_copy(o[:, 0:w], s_[:, 0:w]), [rs], [ro])
                elif eng == "pool":
                    P.op("pool", lambda: nc.gpsimd.tensor_copy(o[:, 0:w], s_[:, 0:w]), [rs], [ro])
                else:
                    P.op("act", lambda: nc.scalar.copy(o[:, 0:w], s_[:, 0:w]), [rs], [ro])
                P.dma("sp", dst[r0:r0 + 128, c0:c0 + w], o[:, 0:w], ro, reads=[ro])
            P.barrier()

    def rmsnorm(self, xt, r_x, gB, r_g, h, r_h, stt, r_st, junk, r_junk):
        nc, P = self.nc, self.P
        P.op("act", lambda: nc.scalar.activation(junk, xt, AF.Square, accum_out=stt[:, 0:1]), [r_x], [r_junk, r_st])
        P.op("act", lambda: nc.scalar.activation(stt[:, 1:2], stt[:, 0:1], AF.Sqrt, bias=self.epst[:, 0:1], scale=1.0 / D),
             [r_st, self.r_eps], [r_st])
        P.op("dve", lambda: nc.vector.reciprocal(stt[:, 2:3], stt[:, 1:2]), [r_st], [r_st])
        P.op("dve", lambda: nc.vector.scalar_tensor_tensor(h, xt, stt[:, 2:3], gB, ALU.mult, ALU.mult), [r_x, r_st, r_g], [r_h])

    def transpose_h(self, h, r_h, hT_dst, r_hT):
        nc, P = self.nc, self.P
        for c in range(8):
            P.op("pe", lambda: nc.tensor.transpose(self.PT[:, c * 128:(c + 1) * 128], h[:, c * 128:(c + 1) * 128], self.ident[:]),
                 [r_h, self.r_ident], [self.rPT[0]])
        P.op("act", lambda: nc.scalar.copy(hT_dst, self.PT[:, 0:1024].rearrange("p (c t) -> p c t", c=8)), [self.rPT[0]], [r_hT])

    def phase1(self, L):
        nc, P, cfg = self.nc, self.P, self.cfg
        even = (L % 2 == 0)
        e = L // 2
        F = 3072 if even else 2120
        wsrc = self.b_ie[e] if even else self.b_io[e]
        xsrc = self.x_in if L == 0 else self.xs
        if even:
            spans = [(0, 16), (1536, 16)]
            fm = [(0, 1024, 0), (1536, 2560, 1024)]
            nfull, half = 16, False
        else:
            spans = [(0, 20), (1536, 9)]
            fm = [(0, 1280, 0), (1536, 2112, 1280)]
            nfull, half = 14, True
        with ExitStack() as st:
            W, r_W = self.sb(st, [128, 8, F], BF16, "W")
            gB, r_g = self.sb(st, [128, D], F32, "gB")
            xt = [self.sb(st, [128, D], F32, "xt") for _ in range(2)]
            rc = [self.sb(st, [128, 512], F32, "rc") for _ in range(2)]
            junk, r_junk = self.sb(st, [128, D], BF16, "junk")
            h = [self.sb(st, [128, D], BF16, "h") for _ in range(2)]
            hT = [self.sb(st, [128, 8, 128], BF16, "hT") for _ in range(2)]
            proj = [self.sb(st, [128, F], F32, "proj") for _ in range(2)]
            tmp = [self.sb(st, [128, 160], F32, "rt") for _ in range(4)]
            pb = [self.sb(st, [128, 2048], BF16, "pb") for _ in range(2)]
            qst = [self.sb(st, [128, 16, 512], BF16, "qst") for _ in range(2)]
            vst = [self.sb(st, [128, 4, 1024], BF16, "vst") for _ in range(2)]
            wst = [self.sb(st, [128, 4, 8], F32, "wst") for _ in range(2)]
            stt = [self.sb(st, [128, 4], F32, "stt") for _ in range(2)]

            for c in range(8):
                P.dma("sp", W[:, c, :], wsrc[c * 128:(c + 1) * 128, :], r_W, writes=[r_W])
            P.dma("sp", gB[:], self.g_mix[L], r_g, writes=[r_g])
            ntile = cfg.NTOK // 128

            def load(ti):
                t0 = ti * 128
                x_, rx = xt[ti % 2]
                P.dma("sp", x_[:], xsrc[t0:t0 + 128, :], rx, writes=[rx])
                r_, rr = rc[ti % 2]
                p0 = t0 % cfg.S
                P.dma("sp", r_[:], self.c_rope[p0:p0 + 128, :], rr, writes=[rr])

            def prenorm(ti):
                x_, rx = xt[ti % 2]
                h_, rh = h[ti % 2]
                hT_, rhT = hT[ti % 2]
                s_, rs = stt[ti % 2]
                self.rmsnorm(x_[:], rx, gB[:], r_g, h_[:], rh, s_, rs, junk[:], r_junk)
                self.transpose_h(h_, rh, hT_[:], rhT)

            load(0)
            if ntile > 1:
                load(1)
            prenorm(0)
            for ti in range(ntile):
                g, j = ti // 4, ti % 4
                x_, rx = xt[ti % 2]
                r_, rr = rc[ti % 2]
                h_, rh = h[ti % 2]
                hT_, rhT = hT[ti % 2]
                pj, rpj = proj[ti % 2]
                pb_, rpb = pb[ti % 2]
                q_, rq = qst[g % 2]
                v_, rv = vst[g % 2]
                w_, rw = wst[g % 2]
                nch = (F + 511) // 512
                for ch in range(nch):
                    f0 = ch * 512
                    fw = min(512, F - f0)
                    ps, rps = self.PS[ch % 6]
                    for c in range(8):
                        P.op("pe", lambda: nc.tensor.matmul(ps[:, 0:fw], hT_[:, c, :], W[:, c, f0:f0 + fw], start=(c == 0), stop=(c == 7)),
                             [rhT, r_W], [rps])
                    P.op("act", lambda: nc.scalar.copy(pj[:, f0:f0 + fw], ps[:, 0:fw]), [rps], [rpj])
                if ti + 1 < ntile:
                    prenorm(ti + 1)
                for (c0, nh) in spans:
                    v3 = pj[:, c0:c0 + 64 * nh].rearrange("p (h d) -> p h d", d=64)
                    x1, x2 = v3[:, :, 0:8], v3[:, :, 8:16]
                    cs = r_[:, 0:8 * nh].rearrange("p (h d) -> p h d", d=8)
                    sn = r_[:, 256:256 + 8 * nh].rearrange("p (h d) -> p h d", d=8)
                    tt = [t[0][:, 0:8 * nh].rearrange("p (h d) -> p h d", d=8) for t in tmp]
                    rt = [t[1] for t in tmp]
                    P.op("dve", lambda: nc.vector.tensor_tensor(tt[0], x1, cs, ALU.mult), [rpj, rr], [rt[0]])
                    P.op("dve", lambda: nc.vector.tensor_tensor(tt[1], x2, sn, ALU.mult), [rpj, rr], [rt[1]])
                    P.op("dve", lambda: nc.vector.tensor_tensor(tt[2], x2, cs, ALU.mult), [rpj, rr], [rt[2]])
                    P.op("dve", lambda: nc.vector.tensor_tensor(tt[3], x1, sn, ALU.mult), [rpj, rr], [rt[3]])
                    P.op("dve", lambda: nc.vector.tensor_tensor(x1, tt[0], tt[1], ALU.subtract), [rt[0], rt[1]], [rpj])
                    P.op("dve", lambda: nc.vector.tensor_tensor(x2, tt[2], tt[3], ALU.add), [rt[2], rt[3]], [rpj])
                if ti + 2 < ntile:
                    load(ti + 2)
                for (a, b, d0) in fm:
                    P.op("act", lambda: nc.scalar.copy(pb_[:, d0:d0 + (b - a)], pj[:, a:b]), [rpj], [rpb])
                for blk in range(nfull):
                    P.op("pe", lambda: nc.tensor.transpose(self.PT[:, blk * 128:(blk + 1) * 128], pb_[:, blk * 128:(blk + 1) * 128], self.ident[:]),
                         [rpb, self.r_ident], [self.rPT[0], self.rPT[1]])
                if half:
                    P.op("pe", lambda: nc.tensor.transpose(self.PT[0:64, nfull * 128:(nfull + 1) * 128], pb_[:, nfull * 128:nfull * 128 + 64], self.ident[:]),
                         [rpb, self.r_ident], [self.rPT[0], self.rPT[1]])
                P.op("dve", lambda: nc.vector.tensor_copy(q_[:, 0:nfull, j * 128:(j + 1) * 128],
                                                          self.PT[:, 0:nfull * 128].rearrange("p (b t) -> p b t", b=nfull)),
                     [self.rPT[0], self.rPT[1]], [rq])
                if half:
                    P.op("dve", lambda: nc.vector.tensor_copy(q_[0:64, nfull, j * 128:(j + 1) * 128], self.PT[0:64, nfull * 128:(nfull + 1) * 128]),
                         [self.rPT[0], self.rPT[1]], [rq])
                if even:
                    P.op("pool", lambda: nc.gpsimd.tensor_copy(v_[:, j, 0:512], pj[:, 1024:1536]), [rpj], [rv])
                    P.op("pool", lambda: nc.gpsimd.tensor_copy(v_[:, j, 512:1024], pj[:, 2560:3072]), [rpj], [rv])
                else:
                    P.op("pool", lambda: nc.gpsimd.tensor_copy(v_[:, j, 0:256], pj[:, 1280:1536]), [rpj], [rv])
                    P.op("pool", lambda: nc.gpsimd.tensor_copy(w_[:, j, :], pj[:, 2112:2120]), [rpj], [rw])
                if j == 3:
                    t0 = g * 512
                    P.dma("sp", self.qkT[0:nfull * 128, t0:t0 + 512].rearrange("(b p) t -> p b t", p=128), q_[:, 0:nfull, :], rq, reads=[rq])
                    if half:
                        P.dma("sp", self.qkT[nfull * 128:nfull * 128 + 64, t0:t0 + 512], q_[0:64, nfull, :], rq, reads=[rq])
                    vw = 1024 if even else 256
                    P.dma("sp", self.vv[t0:t0 + 512, 0:vw].rearrange("(j p) f -> p j f", p=128), v_[:, :, 0:vw], rv, reads=[rv])
                    if not even:
                        P.dma("sp", self.wv[t0:t0 + 512, :].rearrange("(j p) f -> p j f", p=128), w_[:], rw, reads=[rw])

    def pipe_begin(self, la):
        self._pq = []
        self._la = int(os.environ.get('KLA', la))

    def pipe_push(self, front, back):
        front()
        self._pq.append(back)
        if len(self._pq) > self._la:
            self._pq.pop(0)()

    def pipe_flush(self):
        while self._pq:
            self._pq.pop(0)()

    def attn_unit(self, kT_ap, qT_ap, c0, psS, pT, mask_ap, mask_eng, pv_list, post=None, add_mask=None):
        nc, P = self.nc, self.P
        (ps, rps), (p_, rp) = psS, pT
        (kap, rk), (qap, rq) = kT_ap, qT_ap

        def front():
            P.op("pe", lambda: nc.tensor.matmul(ps[:, c0:512], kap, qap, start=True, stop=(add_mask is None)), [rk, rq], [rps])
            if add_mask is not None:
                (amap, ram) = add_mask
                P.op("pe", lambda: nc.tensor.matmul(ps[:, c0:512], self.ident[:], amap, start=False, stop=True), [ram, self.r_ident], [rps])
            P.op("act", lambda: nc.scalar.activation(p_[:, c0:512], ps[:, c0:512], AF.Exp, scale=0.125), [rps], [rp])
            if mask_ap is not None:
                (dst, map_, rm) = mask_ap
                if mask_eng == "pool":
                    P.op("pool", lambda: nc.gpsimd.tensor_tensor(dst, dst, map_, ALU.mult), [rp, rm], [rp])
                else:
                    P.op("dve", lambda: nc.vector.tensor_tensor(dst, dst, map_, ALU.mult), [rp, rm], [rp])

        def back():
            for (oap, ro, lap, rl, st_, sp_) in pv_list:
                P.op("pe", lambda: nc.tensor.matmul(oap, lap, p_[:, c0:512], start=st_, stop=sp_), [rp, rl], [ro])
            if post is not None:
                post()

        self.pipe_push(front, back)

    def finalize_pair(self, psE, psO, L_, R_, Rs_, ao, r_ao):
        nc, P = self.nc, self.P
        (pe_, rpe), (po_, rpo) = psE, psO
        (L, rL), (R, rR), (Rs, rRs) = L_, R_, Rs_
        P.op("dve", lambda: nc.vector.tensor_copy(L[0:64, :], po_[0:64, :]), [rpo], [rL])
        P.op("dve", lambda: nc.vector.tensor_copy(L[64:128, :], pe_[64:128, :]), [rpe], [rL])
        P.op("dve", lambda: nc.vector.reciprocal(R[:], L[:]), [rL], [rR])
        P.op("dve", lambda: nc.vector.tensor_copy(Rs[0:64, :], R[64:128, :]), [rR], [rRs])
        P.op("dve", lambda: nc.vector.tensor_copy(Rs[64:128, :], R[0:64, :]), [rR], [rRs])
        P.op("dve", lambda: nc.vector.tensor_tensor(ao[0:64, :], pe_[0:64, :], Rs[0:64, :], ALU.mult), [rpe, rRs], [r_ao])
        P.op("dve", lambda: nc.vector.tensor_tensor(ao[64:128, :], po_[64:128, :], Rs[64:128, :], ALU.mult), [rpo, rRs], [r_ao])

    def phase_even(self, L):
        nc, P, cfg = self.nc, self.P, self.cfg
        e = L // 2
        S, KT, NQB = cfg.S, cfg.KT, cfg.NQB
        lam_init = 0.8 - 0.6 * math.exp(-0.3 * L)
        with ExitStack() as st:
            maskA, r_mA = self.sb(st, [128, 2944], BF16, "maskA")
            qT = [self.sb(st, [128, 2, S], BF16, "qT") for _ in range(2)]
            kT = [self.sb(st, [128, S], BF16, "kT") for _ in range(2)]
            Vr = [self.sb(st, [128, KT, 128], BF16, "Vr") for _ in range(2)]
            Vx = [self.sb(st, [128, KT, 2, 192], BF16, "Vx") for _ in range(2)]
            pT = [self.sb(st, [128, 512], BF16, "pT") for _ in range(6)]
            Lb = self.sb(st, [128, 512], F32, "Lb")
            Rb = self.sb(st, [128, 512], F32, "Rb")
            Rsb = self.sb(st, [128, 512], F32, "Rsb")
            y0 = self.sb(st, [128, 512], F32, "y0")
            y1 = self.sb(st, [128, 512], F32, "y1")
            ao = [self.sb(st, [128, 512], BF16, "ao") for _ in range(4)]
            lam_t, r_lam = self.sb(st, [128, 256], F32, "lam")
            lst, r_lst = self.sb(st, [128, 8], F32, "lst")
            ljunk, r_lj = self.sb(st, [128, 64], F32, "ljunk")
            with ExitStack() as st2:
                mstg, r_ms = self.sb(st2, [128, 2944], F32, "mstg")
                P.dma("sp", mstg[:], self.c_maska[:, :], r_ms, writes=[r_ms])
                P.op("dve", lambda: nc.vector.tensor_copy(maskA[:], mstg[:]), [r_ms], [r_mA])
                P.barrier()
            for b in range(2):
                P.op("pool", lambda: nc.gpsimd.memset(Vx[b][0][:], 1.0), [], [Vx[b][1]])
                P.op("pool", lambda: nc.gpsimd.memset(qT[b][0][:], 0.0), [], [qT[b][1]])
            P.dma("sp", lam_t[:], self.lamv[e], r_lam, writes=[r_lam])
            P.dma("sp", lst[:, 6:7], self.subln[e], r_lst, writes=[r_lst])
            P.op("dve", lambda: nc.vector.tensor_tensor(ljunk[:], lam_t[:, 0:64], lam_t[:, 64:128], ALU.mult), [r_lam], [r_lj])
            P.op("dve", lambda: nc.vector.reduce_sum(lst[:, 0:1], ljunk[:], AX.X), [r_lj], [r_lst])
            P.op("dve", lambda: nc.vector.tensor_tensor(ljunk[:], lam_t[:, 128:192], lam_t[:, 192:256], ALU.mult), [r_lam, r_lst], [r_lj])
            P.op("dve", lambda: nc.vector.reduce_sum(lst[:, 1:2], ljunk[:], AX.X), [r_lj], [r_lst])
            P.op("act", lambda: nc.scalar.activation(lst[:, 2:4], lst[:, 0:2], AF.Exp), [r_lst], [r_lst])
            P.op("dve", lambda: nc.vector.scalar_tensor_tensor(lst[:, 4:5], lst[:, 3:4], -lam_init, lst[:, 2:3], ALU.add, ALU.subtract),
                 [r_lst], [r_lst])
            P.op("dve", lambda: nc.vector.tensor_scalar(lst[:, 5:6], lst[:, 6:7], 1.0 - lam_init, None, ALU.mult), [r_lst], [r_lst])

            it = 0
            pti = 0
            aoi = 0
            gi = 0
            y0b = [y0, self.sb(st, [128, 512], F32, "y0b")]
            self.pipe_begin(3)

            def load_group(gidx):
                if gidx >= cfg.NSEQ * 8:
                    return
                s_, r8 = gidx // 8, gidx % 8
                T0_ = s_ * S
                b_ = gidx % 2
                q__, rq_ = qT[b_]
                k__, rk_ = kT[b_]
                vr_, rvr_ = Vr[b_]
                vx_, rvx_ = Vx[b_]
                if r8 < 4:
                    hp_ = r8
                    for e_ in range(2):
                        P.dma("sp", q__[e_ * 64:(e_ + 1) * 64, e_, :], self.qkT[hp_ * 128 + e_ * 64:hp_ * 128 + (e_ + 1) * 64, T0_:T0_ + S], rq_, writes=[rq_])
                    P.dma("sp", k__[:], self.qkT[512 + hp_ * 128:512 + (hp_ + 1) * 128, T0_:T0_ + S], rk_, writes=[rk_])
                    P.dma("sp", vr_[:], self.vv[T0_:T0_ + S, hp_ * 128:(hp_ + 1) * 128].rearrange("(k p) f -> p k f", p=128), rvr_, writes=[rvr_])
                    for eh_ in range(2):
                        P.op("pool", lambda: nc.gpsimd.tensor_copy(vx_[:, :, eh_, 64:128], vr_[:, :, eh_ * 64:(eh_ + 1) * 64]), [rvr_], [rvx_])
                else:
                    hb_ = r8 - 4
                    for e_ in range(2):
                        P.dma("sp", q__[e_ * 64:(e_ + 1) * 64, e_, :], self.qkT[1024 + hb_ * 128 + e_ * 64:1024 + hb_ * 128 + (e_ + 1) * 64, T0_:T0_ + S], rq_, writes=[rq_])
                    P.dma("sp", k__[:], self.qkT[1536 + hb_ * 128:1536 + (hb_ + 1) * 128, T0_:T0_ + S], rk_, writes=[rk_])
                    P.dma("sp", vr_[:], self.vv[T0_:T0_ + S, 512 + hb_ * 128:512 + (hb_ + 1) * 128].rearrange("(k p) f -> p k f", p=128), rvr_, writes=[rvr_])
            for s in range(cfg.NSEQ):
                T0 = s * S
                for hp in range(4):
                    b = it % 2
                    it += 1
                    q_, rq = qT[b]
                    k_, rk = kT[b]
                    vr, rvr = Vr[b]
                    vx, rvx = Vx[b]
                    if it == 1:
                        load_group(0)
                    self.pipe_flush()
                    load_group(it)
                    for qb in range(NQB):
                        Q0 = qb * 512
                        kts = list(range(max(0, 4 * qb - 16), 4 * qb + 4))
                        accE = self.PS[2 + 2 * (gi % 2)]
                        accO = self.PS[3 + 2 * (gi % 2)]
                        gi += 1
                        a_, ra = ao[aoi % 4]
                        aoi += 1

                        def fin(accE=accE, accO=accO, a_=a_, ra=ra, hp=hp, Q0=Q0, T0=T0):
                            self.finalize_pair(accE, accO, Lb, Rb, Rsb, a_, ra)
                            P.dma("sp", self.aoT[hp * 128:(hp + 1) * 128, T0 + Q0:T0 + Q0 + 512], a_[:], ra, reads=[ra])

                        for eh in range(2):
                            po = accE if eh == 0 else accO
                            pr = slice(eh * 64, eh * 64 + 64)
                            for kt in kts:
                                i = kt - 4 * qb
                                c0 = 128 * i if i > 0 else 0
                                j0 = Q0 - 128 * kt + 384
                                p_ = pT[pti % 6]
                                psS = self.PS[pti % 2]
                                meng = "pool" if (pti % 3 == 2) else "dve"
                                pti += 1
                                lhs = vx[:, kt, eh, 64:192] if eh == 0 else vx[:, kt, eh, 0:128]
                                last = (eh == 1 and kt == kts[-1])
                                self.attn_unit((k_[:, kt * 128:(kt + 1) * 128], rk), (q_[:, eh, Q0 + c0:Q0 + 512], rq), c0, psS, p_,
                                               (p_[0][:, c0:512], maskA[:, j0 + c0:j0 + 512], r_mA), meng,
                                               [(po[0][:, c0:512], po[1], lhs, rvx, kt == kts[0], kt == kts[-1])],
                                               post=(fin if last else None))
                for hb in range(4):
                    b = it % 2
                    it += 1
                    q_, rq = qT[b]
                    k_, rk = kT[b]
                    vr, rvr = Vr[b]
                    self.pipe_flush()
                    load_group(it)
                    for qb in range(NQB):
                        Q0 = qb * 512
                        kts = list(range(0, 4 * qb + 4))
                        (Y0, rY0) = y0b[gi % 2]
                        gi += 1
                        a_, ra = ao[aoi % 4]
                        aoi += 1

                        def fin0(Y0=Y0, rY0=rY0):
                            (R, rR) = Rb
                            P.op("dve", lambda: nc.vector.reciprocal(R[:], self.PS[3][0][:]), [self.PS[3][1]], [rR])
                            P.op("dve", lambda: nc.vector.tensor_tensor(Y0[:], self.PS[2][0][:], R[:], ALU.mult), [self.PS[2][1], rR], [rY0])

                        def fin1(Y0=Y0, rY0=rY0, a_=a_, ra=ra, hb=hb, Q0=Q0, T0=T0):
                            (R, rR), (Rs, rRs), (L_, rL) = Rb, Rsb, Lb
                            (Y1, rY1) = y1
                            P.op("dve", lambda: nc.vector.reciprocal(Rs[:], self.PS[5][0][:]), [self.PS[5][1]], [rRs])
                            P.op("dve", lambda: nc.vector.tensor_tensor(Y1[:], self.PS[4][0][:], Rs[:], ALU.mult), [self.PS[4][1], rRs], [rY1])
                            P.op("dve", lambda: nc.vector.scalar_tensor_tensor(Y0[:], Y1[:], lst[:, 4:5], Y0[:], ALU.mult, ALU.add), [rY0, rY1, r_lst], [rY0])
                            P.op("act", lambda: nc.scalar.activation(Y1[:], Y0[:], AF.Square), [rY0], [rY1])
                            pn = self.PS[0]
                            P.op("pe", lambda: nc.tensor.matmul(pn[0][:], self.onesf[:], Y1[:], start=True, stop=True), [rY1, self.r_onesf], [pn[1]])
                            P.op("act", lambda: nc.scalar.activation(L_[:], pn[0][:], AF.Sqrt, bias=self.epst[:, 0:1], scale=1.0 / 128.0),
                                 [pn[1], self.r_eps], [rL])
                            P.op("dve", lambda: nc.vector.reciprocal(Rs[:], L_[:]), [rL], [rRs])
                            P.op("dve", lambda: nc.vector.scalar_tensor_tensor(a_[:], Y0[:], lst[:, 5:6], Rs[:], ALU.mult, ALU.mult), [rY0, rRs, r_lst], [ra])
                            P.dma("sp", self.aoT[512 + hb * 128:512 + (hb + 1) * 128, T0 + Q0:T0 + Q0 + 512], a_[:], ra, reads=[ra])

                        for cc in range(2):
                            po = self.PS[2 + 2 * cc]
                            pl = self.PS[3 + 2 * cc]
                            pr = slice(cc * 64, cc * 64 + 64)
                            for kt in kts:
                                i = kt - 4 * qb
                                c0 = 128 * i if i > 0 else 0
                                p_ = pT[pti % 6]
                                psS = self.PS[pti % 2]
                                pti += 1
                                m = None
                                if i >= 0:
                                    m = (p_[0][:, c0:c0 + 128], self.tri[:], self.r_tri)
                                post = None
                                if kt == kts[-1]:
                                    post = fin0 if cc == 0 else fin1
                                self.attn_unit((k_[:, kt * 128:(kt + 1) * 128], rk), (q_[:, cc, Q0 + c0:Q0 + 512], rq), c0, psS, p_, m, "dve",
                                               [(po[0][:, c0:512], po[1], vr[:, kt, :], rvr, kt == 0, kt == kts[-1]),
                                                (pl[0][:, c0:512], pl[1], self.onesb[:], self.r_onesb, kt == 0, kt == kts[-1])],
                                               post=post)
            self.pipe_flush()

    def phase_odd(self, L):
        nc, P, cfg = self.nc, self.P, self.cfg
        S, KT, NQB = cfg.S, cfg.KT, cfg.NQB
        NIT = 16
        with ExitStack() as st:
            kT2, r_k = self.sb(st, [128, 2, S], BF16, "kT2")
            kiT, r_ki = self.sb(st, [128, S], BF16, "kiT")
            Vx, r_vx = self.sb(st, [128, KT, 4, 192], BF16, "Vx")
            qT = [self.sb(st, [128, 16, 512], BF16, "qT") for _ in range(1)]
            qiT = [self.sb(st, [128, 4, 2, 512], BF16, "qiT") for _ in range(1)]
            wi = [self.sb(st, [128, 4, 8], F32, "wi") for _ in range(2)]
            Ibs = [self.sb(st, [128, S], F32, "Ib") for _ in range(2)]
            rl = [self.sb(st, [128, 512], F32, "rl") for _ in range(3)]
            mqs = [self.sb(st, [128, S], BF16, "mq") for _ in range(2)]
            mq, r_mq = mqs[0]
            bss = [self.sb(st, [128, 16], F32, "bs") for _ in range(2)]
            hss = [self.sb(st, [128, 64], F32, "hs") for _ in range(2)]
            mT, r_mT = self.sb(st, [128, KT, 512], BF16, "mT")
            pT = [self.sb(st, [128, 512], BF16, "pT") for _ in range(4)]
            Lb = self.sb(st, [128, 512], F32, "Lb")
            Rb = self.sb(st, [128, 512], F32, "Rb")
            Rsb = self.sb(st, [128, 512], F32, "Rsb")
            ao = [self.sb(st, [128, 512], BF16, "ao") for _ in range(4)]
            P.op("pool", lambda: nc.gpsimd.memset(Vx[:], 1.0), [], [r_vx])
            P.op("pool", lambda: nc.gpsimd.memset(qT[0][0][:], 0.0), [], [qT[0][1]])
            P.op("pool", lambda: nc.gpsimd.memset(qiT[0][0][:], 0.0), [], [qiT[0][1]])
            pti = 0
            aoi = 0
            rli = 0
            gi = 0
            self.pipe_begin(2)
            for s in range(cfg.NSEQ):
                T0 = s * S
                for c2 in range(2):
                    P.dma("sp", kT2[:, c2, :], self.qkT[1024 + c2 * 128:1024 + (c2 + 1) * 128, T0:T0 + S], r_k, writes=[r_k])
                for hh in range(2):
                    P.dma("sp", kiT[hh * 64:(hh + 1) * 64, :], self.qkT[1792:1856, T0:T0 + S], r_ki, writes=[r_ki])
                KH = KT // 2
                for vh in range(2):
                    vstg = mq[:, :].rearrange("p (k f) -> p k f", f=256)
                    P.dma("sp", vstg, self.vv[T0 + vh * KH * 128:T0 + (vh + 1) * KH * 128, 0:256].rearrange("(k p) f -> p k f", p=128), r_mq, writes=[r_mq])
                    for g in range(4):
                        P.op("pool", lambda: nc.gpsimd.tensor_copy(Vx[:, vh * KH:(vh + 1) * KH, g, 64:128], vstg[:, :, g * 64:(g + 1) * 64]), [r_mq], [r_vx])
                for qb in range(NQB):
                    Q0 = qb * 512
                    q_, rq = qT[0]
                    qi_, rqi = qiT[0]
                    w_, rw = wi[qb % 2]
                    for g4 in range(4):
                        ph = (g4 % 2) * 64
                        P.dma("sp", q_[ph:ph + 64, g4 * 4:(g4 + 1) * 4, :],
                              self.qkT[g4 * 256:(g4 + 1) * 256, T0 + Q0:T0 + Q0 + 512].rearrange("(h p) t -> p h t", p=64), rq, writes=[rq])
                    qsrc = self.qkT[1280:1792, T0 + Q0:T0 + Q0 + 512].rearrange("(c two p) t -> two p c t", two=2, p=64)
                    for e_ in range(2):
                        P.dma("sp", qi_[e_ * 64:(e_ + 1) * 64, :, e_, :], qsrc[e_], rqi, writes=[rqi])
                    P.dma("sp", w_[:], self.wv[T0 + Q0:T0 + Q0 + 512, :].rearrange("(j p) f -> p j f", p=128), rw, writes=[rw])
                    def accumulate(j, Ib_, r_I_):
                        nonlocal rli
                        qt = 4 * qb + j
                        n = (qt + 1) * 128
                        for kb in range((n + 511) // 512):
                            k0 = kb * 512
                            kw = min(512, n - k0)
                            for hi in range(8):
                                pr = slice((hi % 2) * 64, (hi % 2) * 64 + 64)
                                ps, rps = self.PS[rli % 2]
                                r_, rr = rl[rli % 3]
                                rli += 1
                                P.op("pe", lambda: nc.tensor.matmul(ps[:, 0:kw], qi_[:, hi // 2, hi % 2, j * 128:(j + 1) * 128], kiT[:, k0:k0 + kw], start=True, stop=True),
                                     [rqi, r_ki], [rps])
                                P.op("act", lambda: nc.scalar.activation(r_[:, 0:kw], ps[:, 0:kw], AF.Relu), [rps], [rr])
                                if hi == 0:
                                    P.op("dve", lambda: nc.vector.tensor_scalar(Ib_[:, k0:k0 + kw], r_[:, 0:kw], w_[:, j, 0:1], None, ALU.mult), [rr, rw], [r_I_])
                                else:
                                    P.op("dve", lambda: nc.vector.scalar_tensor_tensor(Ib_[:, k0:k0 + kw], r_[:, 0:kw], w_[:, j, hi:hi + 1], Ib_[:, k0:k0 + kw], ALU.mult, ALU.add),
                                         [rr, rw, r_I_], [r_I_])

                    def setup(j, Ib_, r_I_, bs_, r_bs_, hs_, r_hs_, on_act):
                        qt = 4 * qb + j
                        n = (qt + 1) * 128
                        P.op("dve", lambda: nc.vector.tensor_reduce(bs_[:, 1:2], Ib_[:, 0:n], AX.X, ALU.max), [r_I_], [r_bs_])
                        P.op("dve", lambda: nc.vector.tensor_reduce(bs_[:, 0:1], Ib_[:, 0:n], AX.X, ALU.min), [r_I_], [r_bs_])
                        P.op("dve", lambda: nc.vector.tensor_scalar(bs_[:, 0:1], bs_[:, 0:1], -1.0, None, ALU.add), [r_bs_], [r_bs_])
                        P.op("dve", lambda: nc.vector.tensor_tensor(Ib_[:, qt * 128:n], Ib_[:, qt * 128:n], self.negm[:], ALU.add), [r_I_, self.r_negm], [r_I_])
                        if qt >= 2:
                            P.op("dve", lambda: nc.vector.tensor_scalar(bs_[:, 2:3], bs_[:, 1:2], bs_[:, 0:1], 0.5, ALU.subtract, ALU.mult), [r_bs_], [r_bs_])
                            P.op("dve", lambda: nc.vector.tensor_scalar(hs_[:, 0:NIT + 1], self.pw[:, 0:NIT + 1], bs_[:, 2:3], None, ALU.mult), [r_bs_, self.r_pw], [r_hs_])
                            if on_act:
                                P.op("dve", lambda: nc.vector.tensor_scalar(hs_[:, 32:32 + NIT + 1], self.pw[:, 0:NIT + 1], bs_[:, 2:3], -1.0, ALU.mult, ALU.mult), [r_bs_, self.r_pw], [r_hs_])
                                P.op("dve", lambda: nc.vector.tensor_scalar(bs_[:, 3:4], bs_[:, 0:1], bs_[:, 2:3], -1.0, ALU.add, ALU.mult), [r_bs_], [r_bs_])
                            else:
                                P.op("dve", lambda: nc.vector.tensor_tensor(bs_[:, 3:4], bs_[:, 0:1], bs_[:, 2:3], ALU.add), [r_bs_], [r_bs_])

                    def bis_step_dve(j, it_, Ib_, r_I_, mq_, r_mq_, bs_, r_bs_, hs_, r_hs_):
                        n = (4 * qb + j + 1) * 128
                        P.op("dve", lambda: nc.vector.tensor_scalar(mq_[:, 0:n], Ib_[:, 0:n], bs_[:, 3:4], None, ALU.is_ge, ALU.add, accum_out=bs_[:, 4:5]),
                             [r_I_, r_bs_], [r_mq_, r_bs_])
                        P.op("dve", lambda: nc.vector.tensor_scalar(bs_[:, 5:6], bs_[:, 4:5], float(TOPK), 0.5, ALU.is_ge, ALU.subtract), [r_bs_], [r_bs_])
                        P.op("dve", lambda: nc.vector.scalar_tensor_tensor(bs_[:, 3:4], bs_[:, 5:6], hs_[:, it_:it_ + 1], bs_[:, 3:4], ALU.mult, ALU.add), [r_bs_, r_hs_], [r_bs_])

                    def bis_step_act(j, it_, Ib_, r_I_, mq_, r_mq_, bs_, r_bs_, hs_, r_hs_):
                        n = (4 * qb + j + 1) * 128
                        P.op("act", lambda: nc.scalar.activation(mq_[:, 0:n], Ib_[:, 0:n], AF.Sign, bias=bs_[:, 3:4], scale=1.0, accum_out=bs_[:, 4:5]),
                             [r_I_, r_bs_], [r_mq_, r_bs_])
                        P.op("pool", lambda: nc.gpsimd.tensor_scalar(bs_[:, 5:6], bs_[:, 4:5], float(2 * TOPK - n), 0.5, ALU.is_ge, ALU.subtract), [r_bs_], [r_bs_])
                        P.op("pool", lambda: nc.gpsimd.tensor_scalar(bs_[:, 3:4], bs_[:, 5:6], hs_[:, 32 + it_:32 + it_ + 1], bs_[:, 3:4], ALU.mult, ALU.add), [r_bs_, r_hs_], [r_bs_])

                    def finish(j, Ib_, r_I_, mq_, r_mq_, bs_, r_bs_, hs_, r_hs_, on_act):
                        qt = 4 * qb + j
                        n = (qt + 1) * 128
                        if qt >= 2:
                            if on_act:
                                P.op("dve", lambda: nc.vector.tensor_scalar(bs_[:, 0:1], bs_[:, 3:4], -1.0, hs_[:, NIT:NIT + 1], ALU.mult, ALU.subtract), [r_bs_, r_hs_], [r_bs_])
                            else:
                                P.op("dve", lambda: nc.vector.tensor_tensor(bs_[:, 0:1], bs_[:, 3:4], hs_[:, NIT:NIT + 1], ALU.subtract), [r_bs_, r_hs_], [r_bs_])
                        P.op("dve", lambda: nc.vector.tensor_scalar(mq_[:, 0:n], Ib_[:, 0:n], bs_[:, 0:1], NEGM, ALU.is_lt, ALU.mult), [r_I_, r_bs_], [r_mq_])
                        for k1 in range(0, qt + 1, 16):
                            nb = min(16, qt + 1 - k1)
                            for bb in range(nb):
                                kt = k1 + bb
                                P.op("pe", lambda: nc.tensor.transpose(self.PT[:, bb * 128:(bb + 1) * 128], mq_[:, kt * 128:(kt + 1) * 128], self.ident[:]),
                                     [r_mq_, self.r_ident], [self.rPT[0], self.rPT[1]])
                            P.op("act", lambda: nc.scalar.copy(mT[:, k1:k1 + nb, j * 128:(j + 1) * 128],
                                                               self.PT[:, 0:nb * 128].rearrange("p (b t) -> p b t", b=nb)),
                                 [self.rPT[0], self.rPT[1]], [r_mT])

                    for jp in range(2):
                        tiles = []
                        for t in range(2):
                            j = 2 * jp + t
                            on_act = (t == 0) and os.environ.get("KACTBIS", "1") == "1"
                            (Ib_, r_I_), (mq_, r_mq_), (bs_, r_bs_), (hs_, r_hs_) = Ibs[t], mqs[t], bss[t], hss[t]
                            accumulate(j, Ib_, r_I_)
                            setup(j, Ib_, r_I_, bs_, r_bs_, hs_, r_hs_, on_act)
                            tiles.append((j, Ib_, r_I_, mq_, r_mq_, bs_, r_bs_, hs_, r_hs_, on_act))
                        for it_ in range(NIT):
                            for (j, Ib_, r_I_, mq_, r_mq_, bs_, r_bs_, hs_, r_hs_, on_act) in tiles:
                                if 4 * qb + j >= 2:
                                    if on_act:
                                        bis_step_act(j, it_, Ib_, r_I_, mq_, r_mq_, bs_, r_bs_, hs_, r_hs_)
                                    else:
                                        bis_step_dve(j, it_, Ib_, r_I_, mq_, r_mq_, bs_, r_bs_, hs_, r_hs_)
                        for (j, Ib_, r_I_, mq_, r_mq_, bs_, r_bs_, hs_, r_hs_, on_act) in tiles:
                            finish(j, Ib_, r_I_, mq_, r_mq_, bs_, r_bs_, hs_, r_hs_, on_act)
                    kts = list(range(0, 4 * qb + 4))
                    for hp in range(8):
                        accE = self.PS[2 + 2 * (gi % 2)]
                        accO = self.PS[3 + 2 * (gi % 2)]
                        gi += 1
                        a_, ra = ao[aoi % 4]
                        aoi += 1

                        def fin(accE=accE, accO=accO, a_=a_, ra=ra, hp=hp, Q0=Q0, T0=T0):
                            self.finalize_pair(accE, accO, Lb, Rb, Rsb, a_, ra)
                            P.dma("sp", self.aoT[hp * 128:(hp + 1) * 128, T0 + Q0:T0 + Q0 + 512], a_[:], ra, reads=[ra])

                        for eh in range(2):
                            hd = 2 * hp + eh
                            g = hd // 4
                            po = accE if eh == 0 else accO
                            pr = slice(eh * 64, eh * 64 + 64)
                            for kt in kts:
                                i = kt - 4 * qb
                                c0 = 128 * i if i > 0 else 0
                                p_ = pT[pti % 4]
                                psS = self.PS[pti % 2]
                                meng = "pool" if (pti % 3 == 2) else "dve"
                                pti += 1
                                lhs = Vx[:, kt, g, 64:192] if eh == 0 else Vx[:, kt, g, 0:128]
                                last = (eh == 1 and kt == kts[-1])
                                self.attn_unit((kT2[:, g // 2, kt * 128:(kt + 1) * 128], r_k), (q_[:, hd, c0:512], rq), c0, psS, p_,
                                               None, meng,
                                               [(po[0][:, c0:512], po[1], lhs, r_vx, kt == 0, kt == kts[-1])],
                                               post=(fin if last else None), add_mask=(mT[:, kt, c0:512], r_mT))
                    self.pipe_flush()

    def phase3(self, L):
        nc, P, cfg = self.nc, self.P, self.cfg
        even = (L % 2 == 0)
        e = L // 2
        wo = self.b_oe[e] if even else self.b_oo[e]
        xsrc = self.x_in if L == 0 else self.xs
        last = (L == cfg.DEPTH - 1)
        dst = self.y_out if last else self.xs
        NG = cfg.NTOK // 512
        with ExitStack() as st:
            Wd, r_Wd = self.sb(st, [128, 32, D], BF16, "Wd")
            gB, r_g = self.sb(st, [128, D], F32, "gB")
            gF, r_gF = self.sb(st, [128, D], F32, "gF") if last else (None, None)
            NS = 3
            slots = [self.sb(st, [128, 4096], BF16, "ws") for _ in range(NS)]
            aoS = [self.sb(st, [128, 8, 512], BF16, "aoS") for _ in range(1)]
            xt = [self.sb(st, [128, 4, D], F32, "xt") for _ in range(2)]
            h2, r_h2 = self.sb(st, [128, D], BF16, "h2")
            junk, r_junk = self.sb(st, [128, D], BF16, "junk")
            h2T, r_h2T = self.sb(st, [128, 8, 512], BF16, "h2T")
            aT, r_aT = self.sb(st, [128, 32, 512], BF16, "aT")
            sq = [self.sb(st, [128, 512], F32, "sq") for _ in range(2)]
            stt = [self.sb(st, [128, 4], F32, "stt") for _ in range(2)]
            for c8 in range(4):
                P.dma("sp", Wd[:, c8 * 8:(c8 + 1) * 8, :], self.b_dn[L][c8 * 1024:(c8 + 1) * 1024, :].rearrange("(c p) f -> p c f", p=128), r_Wd, writes=[r_Wd])
            P.dma("sp", gB[:], self.g_ffn[L], r_g, writes=[r_g])
            if last:
                P.dma("sp", gF[:], self.g_fin[:, :], r_gF, writes=[r_gF])
            si = [0]
            sti = [0]
            h2x, r_h2x = self.sb(st, [128, 4, D], BF16, "h2x")
            h2Tb = [(h2T, r_h2T), self.sb(st, [128, 8, 512], BF16, "h2Tb")]

            def load_a(g):
                t0 = g * 512
                a_, ra = aoS[0]
                P.dma("sp", a_[:], self.aoT[:, t0:t0 + 512].rearrange("(c p) t -> p c t", p=128), ra, writes=[ra])

            def load_x(g):
                t0 = g * 512
                x_, rx = xt[g % 2]
                P.dma("sp", x_[:], xsrc[t0:t0 + 512, :].rearrange("(j p) f -> p j f", p=128), rx, writes=[rx])

            def F1(g):
                a_, ra = aoS[0]
                x_, rx = xt[g % 2]
                wslots = []
                for hc in range(2):
                    w_, rw = slots[si[0] % NS]
                    si[0] += 1
                    P.dma("sp", w_[:].rearrange("p (c f) -> p c f", c=4), wo[hc * 512:(hc + 1) * 512, :].rearrange("(c p) f -> p c f", p=128), rw, writes=[rw])
                    wslots.append((w_, rw))
                for j in range(4):
                    for half in range(2):
                        ps, rps = self.PS[half]
                        for c in range(8):
                            w_, rw = wslots[c // 4]
                            wv_ = w_[:].rearrange("p (c f) -> p c f", c=4)
                            P.op("pe", lambda: nc.tensor.matmul(ps[:], a_[:, c, j * 128:(j + 1) * 128], wv_[:, c % 4, half * 512:(half + 1) * 512], start=(c == 0), stop=(c == 7)),
                                 [ra, rw], [rps])
                        P.op("dve", lambda: nc.vector.tensor_tensor(x_[:, j, half * 512:(half + 1) * 512], ps[:], x_[:, j, half * 512:(half + 1) * 512], ALU.add),
                             [rps, rx], [rx])
                    s_, rs = stt[sti[0] % 2]
                    sti[0] += 1
                    self.rmsnorm(x_[:, j, :], rx, gB[:], r_g, h2x[:, j, :], r_h2x, s_, rs, junk[:], r_junk)

            def F2(g):
                hT_, rhT = h2Tb[g % 2]
                for j in range(4):
                    for c in range(8):
                        P.op("pe", lambda: nc.tensor.transpose(self.PT[:, c * 128:(c + 1) * 128], h2x[:, j, c * 128:(c + 1) * 128], self.ident[:]),
                             [r_h2x, self.r_ident], [self.rPT[0]])
                    P.op("act", lambda: nc.scalar.copy(hT_[:, :, j * 128:(j + 1) * 128], self.PT[:, 0:1024].rearrange("p (c t) -> p c t", c=8)),
                         [self.rPT[0]], [rhT])

            def U(g):
                hT_, rhT = h2Tb[g % 2]
                for qc in range(8):
                    w_, rw = slots[si[0] % NS]
                    si[0] += 1
                    wv_ = w_[:].rearrange("p (c f) -> p c f", c=8)
                    P.dma("sp", wv_, self.b_up[L][:, qc * 512:(qc + 1) * 512].rearrange("(c p) f -> p c f", p=128), rw, writes=[rw])
                    for f4 in range(4):
                        ffc = qc * 4 + f4
                        ps, rps = self.PS[2 + ffc % 2]
                        s_, rsq = sq[ffc % 2]
                        for c in range(8):
                            P.op("pe", lambda: nc.tensor.matmul(ps[:], wv_[:, c, f4 * 128:(f4 + 1) * 128], hT_[:, c, :], start=(c == 0), stop=(c == 7)),
                                 [rw, rhT], [rps])
                        P.op("act", lambda: nc.scalar.activation(s_[:], ps[:], AF.Square), [rps], [rsq])
                        P.op("dve", lambda: nc.vector.scalar_tensor_tensor(aT[:, ffc, :], ps[:], 0.0, s_[:], ALU.is_gt, ALU.mult), [rps, rsq], [r_aT])

            def Dn(g, jh_list):
                x_, rx = xt[g % 2]
                for (j, half) in jh_list:
                    ps, rps = self.PS[4 + half]
                    for ffc in range(32):
                        P.op("pe", lambda: nc.tensor.matmul(ps[:], aT[:, ffc, j * 128:(j + 1) * 128], Wd[:, ffc, half * 512:(half + 1) * 512], start=(ffc == 0), stop=(ffc == 31)),
                             [r_aT, r_Wd], [rps])
                    P.op("dve", lambda: nc.vector.tensor_tensor(x_[:, j, half * 512:(half + 1) * 512], ps[:], x_[:, j, half * 512:(half + 1) * 512], ALU.add),
                         [rps, rx], [rx])
                    if last and half == 1:
                        s_, rs = stt[sti[0] % 2]
                        sti[0] += 1
                        P.op("act", lambda: nc.scalar.activation(junk[:], x_[:, j, :], AF.Square, accum_out=s_[:, 0:1]), [rx], [r_junk, rs])
                        P.op("act", lambda: nc.scalar.activation(s_[:, 1:2], s_[:, 0:1], AF.Sqrt, bias=self.epst[:, 0:1], scale=1.0 / D), [rs, self.r_eps], [rs])
                        P.op("dve", lambda: nc.vector.reciprocal(s_[:, 2:3], s_[:, 1:2]), [rs], [rs])
                        P.op("dve", lambda: nc.vector.scalar_tensor_tensor(x_[:, j, :], x_[:, j, :], s_[:, 2:3], gF[:], ALU.mult, ALU.mult), [rx, rs, r_gF], [rx])

            jh = [(j, half) for j in range(4) for half in range(2)]
            load_a(0)
            load_x(0)
            F1(0)
            F2(0)
            for g in range(NG):
                t0 = g * 512
                x_, rx = xt[g % 2]
                if g + 1 < NG:
                    load_x(g + 1)
                    load_a(g + 1)
                U(g)
                if g + 1 < NG:
                    F1(g + 1)
                Dn(g, jh[0:2])
                if g + 1 < NG:
                    F2(g + 1)
                Dn(g, jh[2:8])
                P.dma("sp", dst[t0:t0 + 512, :].rearrange("(j p) f -> p j f", p=128), x_[:], rx, reads=[rx])


def _in_maps(cfg, x, norm_mix, norm_ffn, w_in_even, w_out_even, lambda_q1, lambda_k1, lambda_q2, lambda_k2,
             diff_subln, w_in_odd, w_out_odd, w_ffn_up, w_ffn_down, norm_final):
    f = lambda a: np.ascontiguousarray(np.asarray(a, dtype=np.float32))
    NE, NO = cfg.NE, max(cfg.NO, 1)
    bc = lambda a: np.ascontiguousarray(np.broadcast_to(f(a)[:, None, :], (a.shape[0], 128, a.shape[1])))
    lam = np.concatenate([f(lambda_q1)[:NE], f(lambda_k1)[:NE], f(lambda_q2)[:NE], f(lambda_k2)[:NE]], axis=1)
    w_io = f(w_in_odd)[:NO] if cfg.NO > 0 else np.zeros((1, D, 2120), np.float32)
    w_oo = f(w_out_odd)[:NO] if cfg.NO > 0 else np.zeros((1, D, D), np.float32)
    shared = {
        "g_mix": bc(np.asarray(norm_mix)[:cfg.DEPTH]),
        "g_ffn": bc(np.asarray(norm_ffn)[:cfg.DEPTH]),
        "g_fin": np.ascontiguousarray(np.broadcast_to(f(norm_final)[None, :], (128, D))),
        "lamv": np.ascontiguousarray(np.broadcast_to(lam[:, None, :], (NE, 128, 256))),
        "subln": np.ascontiguousarray(f(diff_subln)[:NE][:, :, None]),
        "w_ie": f(w_in_even)[:NE], "w_oe": f(w_out_even)[:NE],
        "w_io": w_io, "w_oo": w_oo,
        "w_up": f(w_ffn_up)[:cfg.DEPTH], "w_dn": f(w_ffn_down)[:cfg.DEPTH],
    }
    shared.update(_consts(cfg))
    xf = f(x).reshape(-1, D)
    maps = []
    for c in range(cfg.NCORES):
        m = dict(shared)
        m["x"] = np.ascontiguousarray(xf[c * cfg.NTOK:(c + 1) * cfg.NTOK])
        maps.append(m)
    return maps


def run_cfg(cfg, **inputs):
    nc = Builder(cfg).build()
    maps = _in_maps(cfg, **inputs)
    res = run_bass_kernel_spmd(nc, maps, core_ids=list(range(cfg.NCORES)))
    out = np.concatenate([np.asarray(r["y"]) for r in res.results], axis=0)
    if cfg.debug:
        return out, res.results
    return out


def kernel(**inputs):
    cfg = Cfg(S=4096, NSEQ=2, DEPTH=4, NCORES=8)
    out = run_cfg(cfg, **inputs)
    return out.reshape(16, 4096, D).astype(np.float32)
```

```python
import math
import os
from contextlib import ExitStack

import numpy as np
import concourse.bass as bass
import concourse.mybir as mybir
from concourse.bass_utils import run_bass_kernel_spmd

F32 = mybir.dt.float32
BF16 = mybir.dt.bfloat16
AF = mybir.ActivationFunctionType
ALU = mybir.AluOpType
AX = mybir.AxisListType

D = 1024
DFF = 4096
EPS = 1e-6
TOPK = 256
NEG = -1.0e30
NEGM = -30000.0
EPOCH = 10000


class Cfg:
    def __init__(self, S=4096, NSEQ=2, DEPTH=4, NCORES=8, debug=False):
        self.debug = debug
        self.S = S
        self.NSEQ = NSEQ
        self.DEPTH = DEPTH
        self.NCORES = NCORES
        self.NTOK = S * NSEQ
        self.KT = S // 128
        self.NQB = S // 512
        self.NE = (DEPTH + 1) // 2
        self.NO = DEPTH // 2


class Res:
    __slots__ = ("name", "w", "rs", "sem")

    def __init__(self, name):
        self.name = name
        self.w = None
        self.rs = {}
        self.sem = None


class DSem:
    __slots__ = ("h", "val")

    def __init__(self, h):
        self.h = h
        self.val = 0


class Prog:
    CE = ("pe", "act", "dve", "pool")

    def __init__(self, nc, stack):
        self.nc = nc
        self.stack = stack
        self.eng = {"pe": nc.tensor, "act": nc.scalar, "dve": nc.vector, "pool": nc.gpsimd, "sp": nc.sync}
        self.cnt = {e: 0 for e in self.CE}
        self.known = {e: {} for e in self.eng}
        self.snaps = {e: [] for e in self.CE}
        self.dirty = {e: True for e in self.CE}
        self.semh = {}
        self.nsem = 0
        self.nwait = 0
        self.nins = 0
        self.dfree = []
        self.dall = []
        self.downers = []

    def _new_sem(self, name):
        self.nsem += 1
        return self.stack.enter_context(self.nc.semaphore(name))

    def _esem(self, e, idx):
        key = (e, idx)
        if key not in self.semh:
            self.semh[key] = self._new_sem("s_%s_%d" % (e, idx))
        return self.semh[key]

    def _need(self, e, ev, raw):
        if ev is None:
            return
        key, val = ev
        kn = self.known[e]
        if isinstance(key, str):
            if key == e and e == "pe":
                return
            if kn.get(key, 0) >= val:
                return
            idx, v = (val - 1) // EPOCH, (val - 1) % EPOCH + 1
            self.eng[e].wait_ge(self._esem(key, idx), v)
            kn[key] = val
            sn = self.snaps[key]
            lo, hi = 0, len(sn)
            while lo < hi:
                mid = (lo + hi) // 2
                if sn[mid][0] <= val:
                    lo = mid + 1
                else:
                    hi = mid
            if lo > 0:
                for ce, kv in zip(self.CE, sn[lo - 1][1]):
                    if ce != e and kn.get(ce, 0) < kv:
                        kn[ce] = kv
        else:
            if kn.get(key, 0) >= val:
                return
            self.eng[e].wait_ge(key.h, val)
            kn[key] = val
        if e in self.dirty:
            self.dirty[e] = True
        self.nwait += 1

    def _deps(self, e, reads, writes):
        for r in reads:
            self._need(e, r.w, True)
        for w in writes:
            self._need(e, w.w, False)
            for k, v in w.rs.items():
                self._need(e, (k, v), False)

    def _commit(self, ev, reads, writes):
        k, v = ev
        for r in reads:
            if r.rs.get(k, 0) < v:
                r.rs[k] = v
        for w in writes:
            w.w = ev
            w.rs = {}

    def op(self, e, fn, reads=(), writes=()):
        self._deps(e, reads, writes)
        ins = fn()
        self.cnt[e] += 1
        n = self.cnt[e]
        idx = (n - 1) // EPOCH
        ins.then_inc(self._esem(e, idx), 1)
        if self.dirty[e]:
            kn = self.known[e]
            self.snaps[e].append((n, tuple(kn.get(ce, 0) for ce in self.CE)))
            self.dirty[e] = False
        self._commit((e, n), reads, writes)
        self.nins += 1
        return ins

    def dma(self, q, out, in_, owner, reads=(), writes=(), **kw):
        if owner.sem is None:
            if self.dfree:
                owner.sem = self.dfree.pop(0)
            else:
                owner.sem = DSem(self._new_sem("d%d" % len(self.dall)))
                self.dall.append(owner.sem)
            self.downers.append(owner)
        ds = owner.sem
        if ds.val > 0:
            self._need(q, (ds, ds.val), True)
        self._deps(q, reads, writes)
        ins = self.eng[q].dma_start(out=out, in_=in_, **kw)
        ds.val += 16
        ins.then_inc(ds.h, 16)
        self._commit((ds, ds.val), reads, writes)
        self.nins += 1
        return ins

    def barrier(self, release=True):
        evs = [(e, n) for e, n in self.cnt.items() if n > 0]
        for ds in self.dall:
            if ds.val > 0:
                evs.append((ds, ds.val))
        for e in self.eng:
            for ev in evs:
                self._need(e, ev, True)
        if release:
            for o in self.downers:
                self.dfree.append(o.sem)
                o.sem = None
            self.downers = []


def _rope_table(S):
    pos = np.arange(S, dtype=np.float32)
    inv = np.power(np.float32(500000.0), -np.arange(0, 16, 2, dtype=np.float32) / np.float32(16)).astype(np.float32)
    ang = (pos[:, None] * inv[None, :]).astype(np.float32)
    cos = np.cos(ang.astype(np.float64)).astype(np.float32)
    sin = np.sin(ang.astype(np.float64)).astype(np.float32)
    return np.concatenate([np.tile(cos, (1, 32)), np.tile(sin, (1, 32))], axis=1).astype(np.float32)


def _mask_a():
    k = np.arange(128)[:, None]
    j = np.arange(2944)[None, :]
    d = j - 384 - k
    m = ((d >= 0) & (d <= 128)).astype(np.float32)
    m += ((d >= 0) & (d <= 512) & (d % 4 == 0)).astype(np.float32)
    m += ((d >= 0) & (d <= 2048) & (d % 16 == 0)).astype(np.float32)
    return m


def _consts(cfg):
    k = np.arange(128)[:, None]
    u = np.arange(128)[None, :]
    c = {}
    c["c_rope"] = _rope_table(cfg.S)
    c["c_maska"] = _mask_a()
    small = np.zeros((128, 416), np.float32)
    small[:, 0:128] = np.eye(128, dtype=np.float32)
    small[:, 128:256] = (u >= k).astype(np.float32)
    small[:, 256:384] = np.where(u > k, NEG, 0.0)
    small[:, 384:416] = (0.5 ** np.arange(32, dtype=np.float64)).astype(np.float32)[None, :]
    c["c_small"] = small
    return c


class Builder:
    def __init__(self, cfg):
        self.cfg = cfg
        self.nc = bass.Bass("TRN2", target_bir_lowering=False)
        self.uid = 0

    def sb(self, st, shape, dt, name=None):
        self.uid += 1
        nm = "%s_%d" % (name or "t", self.uid)
        t = st.enter_context(self.nc.sbuf_tensor(nm, list(shape), dt))
        return t, Res(nm)

    def dram(self, name, shape, dt, kind=None):
        if kind is None:
            return self.nc.dram_tensor(name, list(shape), dt).ap()
        return self.nc.dram_tensor(name, list(shape), dt, kind=kind).ap()

    def build(self):
        cfg, nc = self.cfg, self.nc
        NT = cfg.NTOK
        NE, NO, DP = cfg.NE, max(cfg.NO, 1), cfg.DEPTH
        I = "ExternalInput"
        self.x_in = self.dram("x", [NT, D], F32, I)
        self.g_mix = self.dram("g_mix", [DP, 128, D], F32, I)
        self.g_ffn = self.dram("g_ffn", [DP, 128, D], F32, I)
        self.g_fin = self.dram("g_fin", [128, D], F32, I)
        self.lamv = self.dram("lamv", [NE, 128, 256], F32, I)
        self.subln = self.dram("subln", [NE, 128, 1], F32, I)
        self.w_ie = self.dram("w_ie", [NE, D, 3072], F32, I)
        self.w_oe = self.dram("w_oe", [NE, D, D], F32, I)
        self.w_io = self.dram("w_io", [NO, D, 2120], F32, I)
        self.w_oo = self.dram("w_oo", [NO, D, D], F32, I)
        self.w_up = self.dram("w_up", [DP, D, DFF], F32, I)
        self.w_dn = self.dram("w_dn", [DP, DFF, D], F32, I)
        self.c_rope = self.dram("c_rope", [cfg.S, 512], F32, I)
        self.c_maska = self.dram("c_maska", [128, 2944], F32, I)
        self.c_small = self.dram("c_small", [128, 416], F32, I)
        self.y_out = self.dram("y", [NT, D], F32, "ExternalOutput")
        self.b_ie = self.dram("b_ie", [NE, D, 3072], BF16)
        self.b_oe = self.dram("b_oe", [NE, D, D], BF16)
        self.b_io = self.dram("b_io", [NO, D, 2120], BF16)
        self.b_oo = self.dram("b_oo", [NO, D, D], BF16)
        self.b_up = self.dram("b_up", [DP, D, DFF], BF16)
        self.b_dn = self.dram("b_dn", [DP, DFF, D], BF16)
        dk = "ExternalOutput" if cfg.debug else None
        self.xs = self.dram("xs", [NT, D], F32, dk)
        self.qkT = self.dram("qkT", [2048, NT], BF16, dk)
        self.vv = self.dram("vv", [NT, 1024], BF16, dk)
        self.wv = self.dram("wv", [NT, 8], F32, dk)
        self.aoT = self.dram("aoT", [1024, NT], BF16, dk)

        with ExitStack() as st:
            P = self.P = Prog(nc, st)
            self.PS = []
            for i in range(6):
                t = st.enter_context(nc.psum_tensor("ps%d" % i, [128, 512], F32))
                self.PS.append((t, Res("ps%d" % i)))
            self.PT = st.enter_context(nc.psum_tensor("pt", [128, 2048], BF16))
            self.rPT = [Res("pta"), Res("ptb")]
            st.enter_context(nc.Block())
            self.ident, self.r_ident = self.sb(st, [128, 128], BF16, "ident")
            self.tri, self.r_tri = self.sb(st, [128, 128], BF16, "tri")
            self.negm, self.r_negm = self.sb(st, [128, 128], F32, "negm")
            self.onesb, self.r_onesb = self.sb(st, [128, 128], BF16, "onesb")
            self.onesf, self.r_onesf = self.sb(st, [128, 128], F32, "onesf")
            self.epst, self.r_eps = self.sb(st, [128, 1], F32, "eps")
            self.pw, self.r_pw = self.sb(st, [128, 32], F32, "pw")
            self.setup_consts()
            self.convert_weights()
            P.barrier()
            for L in range(cfg.DEPTH):
                self.phase1(L)
                P.barrier()
                if L % 2 == 0:
                    self.phase_even(L)
                else:
                    self.phase_odd(L)
                P.barrier()
                self.phase3(L)
                P.barrier()
            print("program: nins=%d nwait=%d nsem=%d" % (P.nins, P.nwait, P.nsem))
        return nc

    def setup_consts(self):
        nc, P = self.nc, self.P
        with ExitStack() as st:
            sm, r_sm = self.sb(st, [128, 416], F32, "csm")
            P.dma("sp", sm[:], self.c_small[:, :], r_sm, writes=[r_sm])
            P.op("dve", lambda: nc.vector.tensor_copy(self.ident[:], sm[:, 0:128]), [r_sm], [self.r_ident])
            P.op("dve", lambda: nc.vector.tensor_copy(self.tri[:], sm[:, 128:256]), [r_sm], [self.r_tri])
            P.op("dve", lambda: nc.vector.tensor_copy(self.negm[:], sm[:, 256:384]), [r_sm], [self.r_negm])
            P.op("dve", lambda: nc.vector.tensor_copy(self.pw[:], sm[:, 384:416]), [r_sm], [self.r_pw])
            P.op("pool", lambda: nc.gpsimd.memset(self.onesb[:], 1.0), [], [self.r_onesb])
            P.op("pool", lambda: nc.gpsimd.memset(self.onesf[:], 1.0), [], [self.r_onesf])
            P.op("pool", lambda: nc.gpsimd.memset(self.epst[:], EPS), [], [self.r_eps])
            P.barrier()

    def convert_weights(self):
        nc, P, cfg = self.nc, self.P, self.cfg
        jobs = []
        for e in range(cfg.NE):
            jobs.append((self.w_ie[e], self.b_ie[e], D, 3072))
            jobs.append((self.w_oe[e], self.b_oe[e], D, D))
        for o in range(cfg.NO):
            jobs.append((self.w_io[o], self.b_io[o], D, 2120))
            jobs.append((self.w_oo[o], self.b_oo[o], D, D))
        for L in range(cfg.DEPTH):
            jobs.append((self.w_up[L], self.b_up[L], D, DFF))
            jobs.append((self.w_dn[L], self.b_dn[L], DFF, D))
        with ExitStack() as st:
            NB = 6
            stg = [self.sb(st, [128, 2048], F32, "wst") for _ in range(NB)]
            obf = [self.sb(st, [128, 2048], BF16, "wob") for _ in range(NB)]
            tiles = []
            for (src, dst, R, C) in jobs:
                for r0 in range(0, R, 128):
                    for c0 in range(0, C, 2048):
                        tiles.append((src, dst, r0, c0, min(2048, C - c0)))

            def ld(i):
                (src, dst, r0, c0, w) = tiles[i]
                s_, rs = stg[i % NB]
                P.dma("sp", s_[:, 0:w], src[r0:r0 + 128, c0:c0 + w], rs, writes=[rs])

            LD = 3
            for i in range(min(LD, len(tiles))):
                ld(i)
            for i in range(len(tiles)):
                if i + LD < len(tiles):
                    ld(i + LD)
                (src, dst, r0, c0, w) = tiles[i]
                s_, rs = stg[i % NB]
                o, ro = obf[i % NB]
                eng = ("dve", "pool", "act")[i % 3]
                if eng == "dve":
                    P.op("dve", lambda: nc.vector.tensor_copy(o[:, 0:w], s_[:, 0:w]), [rs], [ro])
                elif eng == "pool":
                    P.op("pool", lambda: nc.gpsimd.tensor_copy(o[:, 0:w], s_[:, 0:w]), [rs], [ro])
                else:
                    P.op("act", lambda: nc.scalar.copy(o[:, 0:w], s_[:, 0:w]), [rs], [ro])
                P.dma("sp", dst[r0:r0 + 128, c0:c0 + w], o[:, 0:w], ro, reads=[ro])
            P.barrier()

    def rmsnorm(self, xt, r_x, gB, r_g, h, r_h, stt, r_st, junk, r_junk):
        nc, P = self.nc, self.P
        P.op("act", lambda: nc.scalar.activation(junk, xt, AF.Square, accum_out=stt[:, 0:1]), [r_x], [r_junk, r_st])
        P.op("act", lambda: nc.scalar.activation(stt[:, 1:2], stt[:, 0:1], AF.Sqrt, bias=self.epst[:, 0:1], scale=1.0 / D),
             [r_st, self.r_eps], [r_st])
        P.op("dve", lambda: nc.vector.reciprocal(stt[:, 2:3], stt[:, 1:2]), [r_st], [r_st])
        P.op("dve", lambda: nc.vector.scalar_tensor_tensor(h, xt, stt[:, 2:3], gB, ALU.mult, ALU.mult), [r_x, r_st, r_g], [r_h])

    def transpose_h(self, h, r_h, hT_dst, r_hT):
        nc, P = self.nc, self.P
        for c in range(8):
            P.op("pe", lambda: nc.tensor.transpose(self.PT[:, c * 128:(c + 1) * 128], h[:, c * 128:(c + 1) * 128], self.ident[:]),
                 [r_h, self.r_ident], [self.rPT[0]])
        P.op("act", lambda: nc.scalar.copy(hT_dst, self.PT[:, 0:1024].rearrange("p (c t) -> p c t", c=8)), [self.rPT[0]], [r_hT])

    def phase1(self, L):
        nc, P, cfg = self.nc, self.P, self.cfg
        even = (L % 2 == 0)
        e = L // 2
        F = 3072 if even else 2120
        wsrc = self.b_ie[e] if even else self.b_io[e]
        xsrc = self.x_in if L == 0 else self.xs
        if even:
            spans = [(0, 16), (1536, 16)]
            fm = [(0, 1024, 0), (1536, 2560, 1024)]
            nfull, half = 16, False
        else:
            spans = [(0, 20), (1536, 9)]
            fm = [(0, 1280, 0), (1536, 2112, 1280)]
            nfull, half = 14, True
        with ExitStack() as st:
            W, r_W = self.sb(st, [128, 8, F], BF16, "W")
            gB, r_g = self.sb(st, [128, D], F32, "gB")
            xt = [self.sb(st, [128, D], F32, "xt") for _ in range(2)]
            rc = [self.sb(st, [128, 512], F32, "rc") for _ in range(2)]
            junk, r_junk = self.sb(st, [128, D], BF16, "junk")
            h = [self.sb(st, [128, D], BF16, "h") for _ in range(2)]
            hT = [self.sb(st, [128, 8, 128], BF16, "hT") for _ in range(2)]
            proj = [self.sb(st, [128, F], F32, "proj") for _ in range(2)]
            tmp = [self.sb(st, [128, 160], F32, "rt") for _ in range(4)]
            pb = [self.sb(st, [128, 2048], BF16, "pb") for _ in range(2)]
            qst = [self.sb(st, [128, 16, 512], BF16, "qst") for _ in range(2)]
            vst = [self.sb(st, [128, 4, 1024], BF16, "vst") for _ in range(2)]
            wst = [self.sb(st, [128, 4, 8], F32, "wst") for _ in range(2)]
            stt = [self.sb(st, [128, 4], F32, "stt") for _ in range(2)]

            for c in range(8):
                P.dma("sp", W[:, c, :], wsrc[c * 128:(c + 1) * 128, :], r_W, writes=[r_W])
            P.dma("sp", gB[:], self.g_mix[L], r_g, writes=[r_g])
            ntile = cfg.NTOK // 128

            def load(ti):
                t0 = ti * 128
                x_, rx = xt[ti % 2]
                P.dma("sp", x_[:], xsrc[t0:t0 + 128, :], rx, writes=[rx])
                r_, rr = rc[ti % 2]
                p0 = t0 % cfg.S
                P.dma("sp", r_[:], self.c_rope[p0:p0 + 128, :], rr, writes=[rr])

            def prenorm(ti):
                x_, rx = xt[ti % 2]
                h_, rh = h[ti % 2]
                hT_, rhT = hT[ti % 2]
                s_, rs = stt[ti % 2]
                self.rmsnorm(x_[:], rx, gB[:], r_g, h_[:], rh, s_, rs, junk[:], r_junk)
                self.transpose_h(h_, rh, hT_[:], rhT)

            load(0)
            if ntile > 1:
                load(1)
            prenorm(0)
            for ti in range(ntile):
                g, j = ti // 4, ti % 4
                x_, rx = xt[ti % 2]
                r_, rr = rc[ti % 2]
                h_, rh = h[ti % 2]
                hT_, rhT = hT[ti % 2]
                pj, rpj = proj[ti % 2]
                pb_, rpb = pb[ti % 2]
                q_, rq = qst[g % 2]
                v_, rv = vst[g % 2]
                w_, rw = wst[g % 2]
                nch = (F + 511) // 512
                for ch in range(nch):
                    f0 = ch * 512
                    fw = min(512, F - f0)
                    ps, rps = self.PS[ch % 6]
                    for c in range(8):
                        P.op("pe", lambda: nc.tensor.matmul(ps[:, 0:fw], hT_[:, c, :], W[:, c, f0:f0 + fw], start=(c == 0), stop=(c == 7)),
                             [rhT, r_W], [rps])
                    P.op("act", lambda: nc.scalar.copy(pj[:, f0:f0 + fw], ps[:, 0:fw]), [rps], [rpj])
                if ti + 1 < ntile:
                    prenorm(ti + 1)
                for (c0, nh) in spans:
                    v3 = pj[:, c0:c0 + 64 * nh].rearrange("p (h d) -> p h d", d=64)
                    x1, x2 = v3[:, :, 0:8], v3[:, :, 8:16]
                    cs = r_[:, 0:8 * nh].rearrange("p (h d) -> p h d", d=8)
                    sn = r_[:, 256:256 + 8 * nh].rearrange("p (h d) -> p h d", d=8)
                    tt = [t[0][:, 0:8 * nh].rearrange("p (h d) -> p h d", d=8) for t in tmp]
                    rt = [t[1] for t in tmp]
                    P.op("dve", lambda: nc.vector.tensor_tensor(tt[0], x1, cs, ALU.mult), [rpj, rr], [rt[0]])
                    P.op("dve", lambda: nc.vector.tensor_tensor(tt[1], x2, sn, ALU.mult), [rpj, rr], [rt[1]])
                    P.op("dve", lambda: nc.vector.tensor_tensor(tt[2], x2, cs, ALU.mult), [rpj, rr], [rt[2]])
                    P.op("dve", lambda: nc.vector.tensor_tensor(tt[3], x1, sn, ALU.mult), [rpj, rr], [rt[3]])
                    P.op("dve", lambda: nc.vector.tensor_tensor(x1, tt[0], tt[1], ALU.subtract), [rt[0], rt[1]], [rpj])
                    P.op("dve", lambda: nc.vector.tensor_tensor(x2, tt[2], tt[3], ALU.add), [rt[2], rt[3]], [rpj])
                if ti + 2 < ntile:
                    load(ti + 2)
                for (a, b, d0) in fm:
                    P.op("act", lambda: nc.scalar.copy(pb_[:, d0:d0 + (b - a)], pj[:, a:b]), [rpj], [rpb])
                for blk in range(nfull):
                    P.op("pe", lambda: nc.tensor.transpose(self.PT[:, blk * 128:(blk + 1) * 128], pb_[:, blk * 128:(blk + 1) * 128], self.ident[:]),
                         [rpb, self.r_ident], [self.rPT[0], self.rPT[1]])
                if half:
                    P.op("pe", lambda: nc.tensor.transpose(self.PT[0:64, nfull * 128:(nfull + 1) * 128], pb_[:, nfull * 128:nfull * 128 + 64], self.ident[:]),
                         [rpb, self.r_ident], [self.rPT[0], self.rPT[1]])
                P.op("dve", lambda: nc.vector.tensor_copy(q_[:, 0:nfull, j * 128:(j + 1) * 128],
                                                          self.PT[:, 0:nfull * 128].rearrange("p (b t) -> p b t", b=nfull)),
                     [self.rPT[0], self.rPT[1]], [rq])
                if half:
                    P.op("dve", lambda: nc.vector.tensor_copy(q_[0:64, nfull, j * 128:(j + 1) * 128], self.PT[0:64, nfull * 128:(nfull + 1) * 128]),
                         [self.rPT[0], self.rPT[1]], [rq])
                if even:
                    P.op("pool", lambda: nc.gpsimd.tensor_copy(v_[:, j, 0:512], pj[:, 1024:1536]), [rpj], [rv])
                    P.op("pool", lambda: nc.gpsimd.tensor_copy(v_[:, j, 512:1024], pj[:, 2560:3072]), [rpj], [rv])
                else:
                    P.op("pool", lambda: nc.gpsimd.tensor_copy(v_[:, j, 0:256], pj[:, 1280:1536]), [rpj], [rv])
                    P.op("pool", lambda: nc.gpsimd.tensor_copy(w_[:, j, :], pj[:, 2112:2120]), [rpj], [rw])
                if j == 3:
                    t0 = g * 512
                    P.dma("sp", self.qkT[0:nfull * 128, t0:t0 + 512].rearrange("(b p) t -> p b t", p=128), q_[:, 0:nfull, :], rq, reads=[rq])
                    if half:
                        P.dma("sp", self.qkT[nfull * 128:nfull * 128 + 64, t0:t0 + 512], q_[0:64, nfull, :], rq, reads=[rq])
                    vw = 1024 if even else 256
                    P.dma("sp", self.vv[t0:t0 + 512, 0:vw].rearrange("(j p) f -> p j f", p=128), v_[:, :, 0:vw], rv, reads=[rv])
                    if not even:
                        P.dma("sp", self.wv[t0:t0 + 512, :].rearrange("(j p) f -> p j f", p=128), w_[:], rw, reads=[rw])

    def pipe_begin(self, la):
        self._pq = []
        self._la = int(os.environ.get('KLA', la))

    def pipe_push(self, front, back):
        front()
        self._pq.append(back)
        if len(self._pq) > self._la:
            self._pq.pop(0)()

    def pipe_flush(self):
        while self._pq:
            self._pq.pop(0)()

    def attn_unit(self, kT_ap, qT_ap, c0, psS, pT, mask_ap, mask_eng, pv_list, post=None, add_mask=None):
        nc, P = self.nc, self.P
        (ps, rps), (p_, rp) = psS, pT
        (kap, rk), (qap, rq) = kT_ap, qT_ap

        def front():
            P.op("pe", lambda: nc.tensor.matmul(ps[:, c0:512], kap, qap, start=True, stop=(add_mask is None)), [rk, rq], [rps])
            if add_mask is not None:
                (amap, ram) = add_mask
                P.op("pe", lambda: nc.tensor.matmul(ps[:, c0:512], self.ident[:], amap, start=False, stop=True), [ram, self.r_ident], [rps])
            P.op("act", lambda: nc.scalar.activation(p_[:, c0:512], ps[:, c0:512], AF.Exp, scale=0.125), [rps], [rp])
            if mask_ap is not None:
                (dst, map_, rm) = mask_ap
                if mask_eng == "pool":
                    P.op("pool", lambda: nc.gpsimd.tensor_tensor(dst, dst, map_, ALU.mult), [rp, rm], [rp])
                else:
                    P.op("dve", lambda: nc.vector.tensor_tensor(dst, dst, map_, ALU.mult), [rp, rm], [rp])

        def back():
            for (oap, ro, lap, rl, st_, sp_) in pv_list:
                P.op("pe", lambda: nc.tensor.matmul(oap, lap, p_[:, c0:512], start=st_, stop=sp_), [rp, rl], [ro])
            if post is not None:
                post()

        self.pipe_push(front, back)

    def finalize_pair(self, psE, psO, L_, R_, Rs_, ao, r_ao):
        nc, P = self.nc, self.P
        (pe_, rpe), (po_, rpo) = psE, psO
        (L, rL), (R, rR), (Rs, rRs) = L_, R_, Rs_
        P.op("dve", lambda: nc.vector.tensor_copy(L[0:64, :], po_[0:64, :]), [rpo], [rL])
        P.op("dve", lambda: nc.vector.tensor_copy(L[64:128, :], pe_[64:128, :]), [rpe], [rL])
        P.op("dve", lambda: nc.vector.reciprocal(R[:], L[:]), [rL], [rR])
        P.op("dve", lambda: nc.vector.tensor_copy(Rs[0:64, :], R[64:128, :]), [rR], [rRs])
        P.op("dve", lambda: nc.vector.tensor_copy(Rs[64:128, :], R[0:64, :]), [rR], [rRs])
        P.op("dve", lambda: nc.vector.tensor_tensor(ao[0:64, :], pe_[0:64, :], Rs[0:64, :], ALU.mult), [rpe, rRs], [r_ao])
        P.op("dve", lambda: nc.vector.tensor_tensor(ao[64:128, :], po_[64:128, :], Rs[64:128, :], ALU.mult), [rpo, rRs], [r_ao])

    def phase_even(self, L):
        nc, P, cfg = self.nc, self.P, self.cfg
        e = L // 2
        S, KT, NQB = cfg.S, cfg.KT, cfg.NQB
        lam_init = 0.8 - 0.6 * math.exp(-0.3 * L)
        with ExitStack() as st:
            maskA, r_mA = self.sb(st, [128, 2944], BF16, "maskA")
            qT = [self.sb(st, [128, 2, S], BF16, "qT") for _ in range(2)]
            kT = [self.sb(st, [128, S], BF16, "kT") for _ in range(2)]
            Vr = [self.sb(st, [128, KT, 128], BF16, "Vr") for _ in range(2)]
            Vx = [self.sb(st, [128, KT, 2, 192], BF16, "Vx") for _ in range(2)]
            pT = [self.sb(st, [128, 512], BF16, "pT") for _ in range(6)]
            Lb = self.sb(st, [128, 512], F32, "Lb")
            Rb = self.sb(st, [128, 512], F32, "Rb")
            Rsb = self.sb(st, [128, 512], F32, "Rsb")
            y0 = self.sb(st, [128, 512], F32, "y0")
            y1 = self.sb(st, [128, 512], F32, "y1")
            ao = [self.sb(st, [128, 512], BF16, "ao") for _ in range(4)]
            lam_t, r_lam = self.sb(st, [128, 256], F32, "lam")
            lst, r_lst = self.sb(st, [128, 8], F32, "lst")
            ljunk, r_lj = self.sb(st, [128, 64], F32, "ljunk")
            with ExitStack() as st2:
                mstg, r_ms = self.sb(st2, [128, 2944], F32, "mstg")
                P.dma("sp", mstg[:], self.c_maska[:, :], r_ms, writes=[r_ms])
                P.op("dve", lambda: nc.vector.tensor_copy(maskA[:], mstg[:]), [r_ms], [r_mA])
                P.barrier()
            for b in range(2):
                P.op("pool", lambda: nc.gpsimd.memset(Vx[b][0][:], 1.0), [], [Vx[b][1]])
                P.op("pool", lambda: nc.gpsimd.memset(qT[b][0][:], 0.0), [], [qT[b][1]])
            P.dma("sp", lam_t[:], self.lamv[e], r_lam, writes=[r_lam])
            P.dma("sp", lst[:, 6:7], self.subln[e], r_lst, writes=[r_lst])
            P.op("dve", lambda: nc.vector.tensor_tensor(ljunk[:], lam_t[:, 0:64], lam_t[:, 64:128], ALU.mult), [r_lam], [r_lj])
            P.op("dve", lambda: nc.vector.reduce_sum(lst[:, 0:1], ljunk[:], AX.X), [r_lj], [r_lst])
            P.op("dve", lambda: nc.vector.tensor_tensor(ljunk[:], lam_t[:, 128:192], lam_t[:, 192:256], ALU.mult), [r_lam, r_lst], [r_lj])
            P.op("dve", lambda: nc.vector.reduce_sum(lst[:, 1:2], ljunk[:], AX.X), [r_lj], [r_lst])
            P.op("act", lambda: nc.scalar.activation(lst[:, 2:4], lst[:, 0:2], AF.Exp), [r_lst], [r_lst])
            P.op("dve", lambda: nc.vector.scalar_tensor_tensor(lst[:, 4:5], lst[:, 3:4], -lam_init, lst[:, 2:3], ALU.add, ALU.subtract),
                 [r_lst], [r_lst])
            P.op("dve", lambda: nc.vector.tensor_scalar(lst[:, 5:6], lst[:, 6:7], 1.0 - lam_init, None, ALU.mult), [r_lst], [r_lst])

            it = 0
            pti = 0
            aoi = 0
            gi = 0
            y0b = [y0, self.sb(st, [128, 512], F32, "y0b")]
            self.pipe_begin(3)

            def load_group(gidx):
                if gidx >= cfg.NSEQ * 8:
                    return
                s_, r8 = gidx // 8, gidx % 8
                T0_ = s_ * S
                b_ = gidx % 2
                q__, rq_ = qT[b_]
                k__, rk_ = kT[b_]
                vr_, rvr_ = Vr[b_]
                vx_, rvx_ = Vx[b_]
                if r8 < 4:
                    hp_ = r8
                    for e_ in range(2):
                        P.dma("sp", q__[e_ * 64:(e_ + 1) * 64, e_, :], self.qkT[hp_ * 128 + e_ * 64:hp_ * 128 + (e_ + 1) * 64, T0_:T0_ + S], rq_, writes=[rq_])
                    P.dma("sp", k__[:], self.qkT[512 + hp_ * 128:512 + (hp_ + 1) * 128, T0_:T0_ + S], rk_, writes=[rk_])
                    P.dma("sp", vr_[:], self.vv[T0_:T0_ + S, hp_ * 128:(hp_ + 1) * 128].rearrange("(k p) f -> p k f", p=128), rvr_, writes=[rvr_])
                    for eh_ in range(2):
                        P.op("pool", lambda: nc.gpsimd.tensor_copy(vx_[:, :, eh_, 64:128], vr_[:, :, eh_ * 64:(eh_ + 1) * 64]), [rvr_], [rvx_])
                else:
                    hb_ = r8 - 4
                    for e_ in range(2):
                        P.dma("sp", q__[e_ * 64:(e_ + 1) * 64, e_, :], self.qkT[1024 + hb_ * 128 + e_ * 64:1024 + hb_ * 128 + (e_ + 1) * 64, T0_:T0_ + S], rq_, writes=[rq_])
                    P.dma("sp", k__[:], self.qkT[1536 + hb_ * 128:1536 + (hb_ + 1) * 128, T0_:T0_ + S], rk_, writes=[rk_])
                    P.dma("sp", vr_[:], self.vv[T0_:T0_ + S, 512 + hb_ * 128:512 + (hb_ + 1) * 128].rearrange("(k p) f -> p k f", p=128), rvr_, writes=[rvr_])
            for s in range(cfg.NSEQ):
                T0 = s * S
                for hp in range(4):
                    b = it % 2
                    it += 1
                    q_, rq = qT[b]
                    k_, rk = kT[b]
                    vr, rvr = Vr[b]
                    vx, rvx = Vx[b]
                    if it == 1:
                        load_group(0)
                    self.pipe_flush()
                    load_group(it)
                    for qb in range(NQB):
                        Q0 = qb * 512
                        kts = list(range(max(0, 4 * qb - 16), 4 * qb + 4))
                        accE = self.PS[2 + 2 * (gi % 2)]
                        accO = self.PS[3 + 2 * (gi % 2)]
                        gi += 1
                        a_, ra = ao[aoi % 4]
                        aoi += 1

                        def fin(accE=accE, accO=accO, a_=a_, ra=ra, hp=hp, Q0=Q0, T0=T0):
                            self.finalize_pair(accE, accO, Lb, Rb, Rsb, a_, ra)
                            P.dma("sp", self.aoT[hp * 128:(hp + 1) * 128, T0 + Q0:T0 + Q0 + 512], a_[:], ra, reads=[ra])

                        for eh in range(2):
                            po = accE if eh == 0 else accO
                            pr = slice(eh * 64, eh * 64 + 64)
                            for kt in kts:
                                i = kt - 4 * qb
                                c0 = 128 * i if i > 0 else 0
                                j0 = Q0 - 128 * kt + 384
                                p_ = pT[pti % 6]
                                psS = self.PS[pti % 2]
                                meng = "pool" if (pti % 3 == 2) else "dve"
                                pti += 1
                                lhs = vx[:, kt, eh, 64:192] if eh == 0 else vx[:, kt, eh, 0:128]
                                last = (eh == 1 and kt == kts[-1])
                                self.attn_unit((k_[:, kt * 128:(kt + 1) * 128], rk), (q_[:, eh, Q0 + c0:Q0 + 512], rq), c0, psS, p_,
                                               (p_[0][:, c0:512], maskA[:, j0 + c0:j0 + 512], r_mA), meng,
                                               [(po[0][:, c0:512], po[1], lhs, rvx, kt == kts[0], kt == kts[-1])],
                                               post=(fin if last else None))
                for hb in range(4):
                    b = it % 2
                    it += 1
                    q_, rq = qT[b]
                    k_, rk = kT[b]
                    vr, rvr = Vr[b]
                    self.pipe_flush()
                    load_group(it)
                    for qb in range(NQB):
                        Q0 = qb * 512
                        kts = list(range(0, 4 * qb + 4))
                        (Y0, rY0) = y0b[gi % 2]
                        gi += 1
                        a_, ra = ao[aoi % 4]
                        aoi += 1

                        def fin0(Y0=Y0, rY0=rY0):
                            (R, rR) = Rb
                            P.op("dve", lambda: nc.vector.reciprocal(R[:], self.PS[3][0][:]), [self.PS[3][1]], [rR])
                            P.op("dve", lambda: nc.vector.tensor_tensor(Y0[:], self.PS[2][0][:], R[:], ALU.mult), [self.PS[2][1], rR], [rY0])

                        def fin1(Y0=Y0, rY0=rY0, a_=a_, ra=ra, hb=hb, Q0=Q0, T0=T0):
                            (R, rR), (Rs, rRs), (L_, rL) = Rb, Rsb, Lb
                            (Y1, rY1) = y1
                            P.op("dve", lambda: nc.vector.reciprocal(Rs[:], self.PS[5][0][:]), [self.PS[5][1]], [rRs])
                            P.op("dve", lambda: nc.vector.tensor_tensor(Y1[:], self.PS[4][0][:], Rs[:], ALU.mult), [self.PS[4][1], rRs], [rY1])
                            P.op("dve", lambda: nc.vector.scalar_tensor_tensor(Y0[:], Y1[:], lst[:, 4:5], Y0[:], ALU.mult, ALU.add), [rY0, rY1, r_lst], [rY0])
                            P.op("act", lambda: nc.scalar.activation(Y1[:], Y0[:], AF.Square), [rY0], [rY1])
                            pn = self.PS[0]
                            P.op("pe", lambda: nc.tensor.matmul(pn[0][:], self.onesf[:], Y1[:], start=True, stop=True), [rY1, self.r_onesf], [pn[1]])
                            P.op("act", lambda: nc.scalar.activation(L_[:], pn[0][:], AF.Sqrt, bias=self.epst[:, 0:1], scale=1.0 / 128.0),
                                 [pn[1], self.r_eps], [rL])
                            P.op("dve", lambda: nc.vector.reciprocal(Rs[:], L_[:]), [rL], [rRs])
                            P.op("dve", lambda: nc.vector.scalar_tensor_tensor(a_[:], Y0[:], lst[:, 5:6], Rs[:], ALU.mult, ALU.mult), [rY0, rRs, r_lst], [ra])
                            P.dma("sp", self.aoT[512 + hb * 128:512 + (hb + 1) * 128, T0 + Q0:T0 + Q0 + 512], a_[:], ra, reads=[ra])

                        for cc in range(2):
                            po = self.PS[2 + 2 * cc]
                            pl = self.PS[3 + 2 * cc]
                            pr = slice(cc * 64, cc * 64 + 64)
                            for kt in kts:
                                i = kt - 4 * qb
                                c0 = 128 * i if i > 0 else 0
                                p_ = pT[pti % 6]
                                psS = self.PS[pti % 2]
                                pti += 1
                                m = None
                                if i >= 0:
                                    m = (p_[0][:, c0:c0 + 128], self.tri[:], self.r_tri)
                                post = None
                                if kt == kts[-1]:
                                    post = fin0 if cc == 0 else fin1
                                self.attn_unit((k_[:, kt * 128:(kt + 1) * 128], rk), (q_[:, cc, Q0 + c0:Q0 + 512], rq), c0, psS, p_, m, "dve",
                                               [(po[0][:, c0:512], po[1], vr[:, kt, :], rvr, kt == 0, kt == kts[-1]),
                                                (pl[0][:, c0:512], pl[1], self.onesb[:], self.r_onesb, kt == 0, kt == kts[-1])],
                                               post=post)
            self.pipe_flush()

    def phase_odd(self, L):
        nc, P, cfg = self.nc, self.P, self.cfg
        S, KT, NQB = cfg.S, cfg.KT, cfg.NQB
        NIT = 16
        with ExitStack() as st:
            kT2, r_k = self.sb(st, [128, 2, S], BF16, "kT2")
            kiT, r_ki = self.sb(st, [128, S], BF16, "kiT")
            Vx, r_vx = self.sb(st, [128, KT, 4, 192], BF16, "Vx")
            qT = [self.sb(st, [128, 16, 512], BF16, "qT") for _ in range(1)]
            qiT = [self.sb(st, [128, 4, 2, 512], BF16, "qiT") for _ in range(1)]
            wi = [self.sb(st, [128, 4, 8], F32, "wi") for _ in range(2)]
            Ibs = [self.sb(st, [128, S], F32, "Ib") for _ in range(2)]
            rl = [self.sb(st, [128, 512], F32, "rl") for _ in range(3)]
            mqs = [self.sb(st, [128, S], BF16, "mq") for _ in range(2)]
            mq, r_mq = mqs[0]
            bss = [self.sb(st, [128, 16], F32, "bs") for _ in range(2)]
            hss = [self.sb(st, [128, 64], F32, "hs") for _ in range(2)]
            mT, r_mT = self.sb(st, [128, KT, 512], BF16, "mT")
            pT = [self.sb(st, [128, 512], BF16, "pT") for _ in range(6)]
            Lb = self.sb(st, [128, 512], F32, "Lb")
            Rb = self.sb(st, [128, 512], F32, "Rb")
            Rsb = self.sb(st, [128, 512], F32, "Rsb")
            ao = [self.sb(st, [128, 512], BF16, "ao") for _ in range(4)]
            P.op("pool", lambda: nc.gpsimd.memset(Vx[:], 1.0), [], [r_vx])
            P.op("pool", lambda: nc.gpsimd.memset(qT[0][0][:], 0.0), [], [qT[0][1]])
            P.op("pool", lambda: nc.gpsimd.memset(qiT[0][0][:], 0.0), [], [qiT[0][1]])
            pti = 0
            aoi = 0
            rli = 0
            gi = 0
            self.pipe_begin(3)
            for s in range(cfg.NSEQ):
                T0 = s * S
                for c2 in range(2):
                    P.dma("sp", kT2[:, c2, :], self.qkT[1024 + c2 * 128:1024 + (c2 + 1) * 128, T0:T0 + S], r_k, writes=[r_k])
                for hh in range(2):
                    P.dma("sp", kiT[hh * 64:(hh + 1) * 64, :], self.qkT[1792:1856, T0:T0 + S], r_ki, writes=[r_ki])
                KH = KT // 2
                for vh in range(2):
                    vstg = mq[:, :].rearrange("p (k f) -> p k f", f=256)
                    P.dma("sp", vstg, self.vv[T0 + vh * KH * 128:T0 + (vh + 1) * KH * 128, 0:256].rearrange("(k p) f -> p k f", p=128), r_mq, writes=[r_mq])
                    for g in range(4):
                        P.op("pool", lambda: nc.gpsimd.tensor_copy(Vx[:, vh * KH:(vh + 1) * KH, g, 64:128], vstg[:, :, g * 64:(g + 1) * 64]), [r_mq], [r_vx])
                for qb in range(NQB):
                    Q0 = qb * 512
                    q_, rq = qT[0]
                    qi_, rqi = qiT[0]
                    w_, rw = wi[qb % 2]
                    for g4 in range(4):
                        ph = (g4 % 2) * 64
                        P.dma("sp", q_[ph:ph + 64, g4 * 4:(g4 + 1) * 4, :],
                              self.qkT[g4 * 256:(g4 + 1) * 256, T0 + Q0:T0 + Q0 + 512].rearrange("(h p) t -> p h t", p=64), rq, writes=[rq])
                    qsrc = self.qkT[1280:1792, T0 + Q0:T0 + Q0 + 512].rearrange("(c two p) t -> two p c t", two=2, p=64)
                    for e_ in range(2):
                        P.dma("sp", qi_[e_ * 64:(e_ + 1) * 64, :, e_, :], qsrc[e_], rqi, writes=[rqi])
                    P.dma("sp", w_[:], self.wv[T0 + Q0:T0 + Q0 + 512, :].rearrange("(j p) f -> p j f", p=128), rw, writes=[rw])
                    def accumulate(j, Ib_, r_I_):
                        nonlocal rli
                        qt = 4 * qb + j
                        n = (qt + 1) * 128
                        for kb in range((n + 511) // 512):
                            k0 = kb * 512
                            kw = min(512, n - k0)
                            for hi in range(8):
                                pr = slice((hi % 2) * 64, (hi % 2) * 64 + 64)
                                ps, rps = self.PS[rli % 2]
                                r_, rr = rl[rli % 3]
                                rli += 1
                                P.op("pe", lambda: nc.tensor.matmul(ps[:, 0:kw], qi_[:, hi // 2, hi % 2, j * 128:(j + 1) * 128], kiT[:, k0:k0 + kw], start=True, stop=True),
                                     [rqi, r_ki], [rps])
                                P.op("act", lambda: nc.scalar.activation(r_[:, 0:kw], ps[:, 0:kw], AF.Relu), [rps], [rr])
                                if hi == 0:
                                    P.op("dve", lambda: nc.vector.tensor_scalar(Ib_[:, k0:k0 + kw], r_[:, 0:kw], w_[:, j, 0:1], None, ALU.mult), [rr, rw], [r_I_])
                                else:
                                    P.op("dve", lambda: nc.vector.scalar_tensor_tensor(Ib_[:, k0:k0 + kw], r_[:, 0:kw], w_[:, j, hi:hi + 1], Ib_[:, k0:k0 + kw], ALU.mult, ALU.add),
                                         [rr, rw, r_I_], [r_I_])

                    def setup(j, Ib_, r_I_, bs_, r_bs_, hs_, r_hs_, on_act):
                        qt = 4 * qb + j
                        n = (qt + 1) * 128
                        P.op("dve", lambda: nc.vector.tensor_reduce(bs_[:, 1:2], Ib_[:, 0:n], AX.X, ALU.max), [r_I_], [r_bs_])
                        P.op("dve", lambda: nc.vector.tensor_reduce(bs_[:, 0:1], Ib_[:, 0:n], AX.X, ALU.min), [r_I_], [r_bs_])
                        P.op("dve", lambda: nc.vector.tensor_scalar(bs_[:, 0:1], bs_[:, 0:1], -1.0, None, ALU.add), [r_bs_], [r_bs_])
                        P.op("dve", lambda: nc.vector.tensor_tensor(Ib_[:, qt * 128:n], Ib_[:, qt * 128:n], self.negm[:], ALU.add), [r_I_, self.r_negm], [r_I_])
                        if qt >= 2:
                            P.op("dve", lambda: nc.vector.tensor_scalar(bs_[:, 2:3], bs_[:, 1:2], bs_[:, 0:1], 0.5, ALU.subtract, ALU.mult), [r_bs_], [r_bs_])
                            P.op("dve", lambda: nc.vector.tensor_scalar(hs_[:, 0:NIT + 1], self.pw[:, 0:NIT + 1], bs_[:, 2:3], None, ALU.mult), [r_bs_, self.r_pw], [r_hs_])
                            if on_act:
                                P.op("dve", lambda: nc.vector.tensor_scalar(hs_[:, 32:32 + NIT + 1], self.pw[:, 0:NIT + 1], bs_[:, 2:3], -1.0, ALU.mult, ALU.mult), [r_bs_, self.r_pw], [r_hs_])
                                P.op("dve", lambda: nc.vector.tensor_scalar(bs_[:, 3:4], bs_[:, 0:1], bs_[:, 2:3], -1.0, ALU.add, ALU.mult), [r_bs_], [r_bs_])
                            else:
                                P.op("dve", lambda: nc.vector.tensor_tensor(bs_[:, 3:4], bs_[:, 0:1], bs_[:, 2:3], ALU.add), [r_bs_], [r_bs_])

                    def bis_step_dve(j, it_, Ib_, r_I_, mq_, r_mq_, bs_, r_bs_, hs_, r_hs_):
                        n = (4 * qb + j + 1) * 128
                        P.op("dve", lambda: nc.vector.tensor_scalar(mq_[:, 0:n], Ib_[:, 0:n], bs_[:, 3:4], None, ALU.is_ge, ALU.add, accum_out=bs_[:, 4:5]),
                             [r_I_, r_bs_], [r_mq_, r_bs_])
                        P.op("dve", lambda: nc.vector.tensor_scalar(bs_[:, 5:6], bs_[:, 4:5], float(TOPK), 0.5, ALU.is_ge, ALU.subtract), [r_bs_], [r_bs_])
                        P.op("dve", lambda: nc.vector.scalar_tensor_tensor(bs_[:, 3:4], bs_[:, 5:6], hs_[:, it_:it_ + 1], bs_[:, 3:4], ALU.mult, ALU.add), [r_bs_, r_hs_], [r_bs_])

                    def bis_step_act(j, it_, Ib_, r_I_, mq_, r_mq_, bs_, r_bs_, hs_, r_hs_):
                        n = (4 * qb + j + 1) * 128
                        P.op("act", lambda: nc.scalar.activation(mq_[:, 0:n], Ib_[:, 0:n], AF.Sign, bias=bs_[:, 3:4], scale=1.0, accum_out=bs_[:, 4:5]),
                             [r_I_, r_bs_], [r_mq_, r_bs_])
                        P.op("pool", lambda: nc.gpsimd.tensor_scalar(bs_[:, 5:6], bs_[:, 4:5], float(2 * TOPK - n), 0.5, ALU.is_ge, ALU.subtract), [r_bs_], [r_bs_])
                        P.op("pool", lambda: nc.gpsimd.tensor_scalar(bs_[:, 3:4], bs_[:, 5:6], hs_[:, 32 + it_:32 + it_ + 1], bs_[:, 3:4], ALU.mult, ALU.add), [r_bs_, r_hs_], [r_bs_])

                    def finish(j, Ib_, r_I_, mq_, r_mq_, bs_, r_bs_, hs_, r_hs_, on_act):
                        qt = 4 * qb + j
                        n = (qt + 1) * 128
                        if qt >= 2:
                            if on_act:
                                P.op("dve", lambda: nc.vector.tensor_scalar(bs_[:, 0:1], bs_[:, 3:4], -1.0, hs_[:, NIT:NIT + 1], ALU.mult, ALU.subtract), [r_bs_, r_hs_], [r_bs_])
                            else:
                                P.op("dve", lambda: nc.vector.tensor_tensor(bs_[:, 0:1], bs_[:, 3:4], hs_[:, NIT:NIT + 1], ALU.subtract), [r_bs_, r_hs_], [r_bs_])
                        P.op("dve", lambda: nc.vector.tensor_scalar(mq_[:, 0:n], Ib_[:, 0:n], bs_[:, 0:1], None, ALU.is_ge), [r_I_, r_bs_], [r_mq_])
                        for k1 in range(0, qt + 1, 16):
                            nb = min(16, qt + 1 - k1)
                            for bb in range(nb):
                                kt = k1 + bb
                                P.op("pe", lambda: nc.tensor.transpose(self.PT[:, bb * 128:(bb + 1) * 128], mq_[:, kt * 128:(kt + 1) * 128], self.ident[:]),
                                     [r_mq_, self.r_ident], [self.rPT[0], self.rPT[1]])
                            P.op("act", lambda: nc.scalar.copy(mT[:, k1:k1 + nb, j * 128:(j + 1) * 128],
                                                               self.PT[:, 0:nb * 128].rearrange("p (b t) -> p b t", b=nb)),
                                 [self.rPT[0], self.rPT[1]], [r_mT])

                    for jp in range(2):
                        tiles = []
                        for t in range(2):
                            j = 2 * jp + t
                            on_act = (t == 0) and os.environ.get("KACTBIS", "1") == "1"
                            (Ib_, r_I_), (mq_, r_mq_), (bs_, r_bs_), (hs_, r_hs_) = Ibs[t], mqs[t], bss[t], hss[t]
                            accumulate(j, Ib_, r_I_)
                            setup(j, Ib_, r_I_, bs_, r_bs_, hs_, r_hs_, on_act)
                            tiles.append((j, Ib_, r_I_, mq_, r_mq_, bs_, r_bs_, hs_, r_hs_, on_act))
                        for it_ in range(NIT):
                            for (j, Ib_, r_I_, mq_, r_mq_, bs_, r_bs_, hs_, r_hs_, on_act) in tiles:
                                if 4 * qb + j >= 2:
                                    if on_act:
                                        bis_step_act(j, it_, Ib_, r_I_, mq_, r_mq_, bs_, r_bs_, hs_, r_hs_)
                                    else:
                                        bis_step_dve(j, it_, Ib_, r_I_, mq_, r_mq_, bs_, r_bs_, hs_, r_hs_)
                        for (j, Ib_, r_I_, mq_, r_mq_, bs_, r_bs_, hs_, r_hs_, on_act) in tiles:
                            finish(j, Ib_, r_I_, mq_, r_mq_, bs_, r_bs_, hs_, r_hs_, on_act)
                    kts = list(range(0, 4 * qb + 4))
                    for hp in range(8):
                        accE = self.PS[2 + 2 * (gi % 2)]
                        accO = self.PS[3 + 2 * (gi % 2)]
                        gi += 1
                        a_, ra = ao[aoi % 4]
                        aoi += 1

                        def fin(accE=accE, accO=accO, a_=a_, ra=ra, hp=hp, Q0=Q0, T0=T0):
                            self.finalize_pair(accE, accO, Lb, Rb, Rsb, a_, ra)
                            P.dma("sp", self.aoT[hp * 128:(hp + 1) * 128, T0 + Q0:T0 + Q0 + 512], a_[:], ra, reads=[ra])

                        for eh in range(2):
                            hd = 2 * hp + eh
                            g = hd // 4
                            po = accE if eh == 0 else accO
                            pr = slice(eh * 64, eh * 64 + 64)
                            for kt in kts:
                                i = kt - 4 * qb
                                c0 = 128 * i if i > 0 else 0
                                p_ = pT[pti % 6]
                                psS = self.PS[pti % 2]
                                meng = "pool" if (pti % 3 == 2) else "dve"
                                pti += 1
                                lhs = Vx[:, kt, g, 64:192] if eh == 0 else Vx[:, kt, g, 0:128]
                                last = (eh == 1 and kt == kts[-1])
                                self.attn_unit((kT2[:, g // 2, kt * 128:(kt + 1) * 128], r_k), (q_[:, hd, c0:512], rq), c0, psS, p_,
                                               (p_[0][:, c0:512], mT[:, kt, c0:512], r_mT), "dve",
                                               [(po[0][:, c0:512], po[1], lhs, r_vx, kt == 0, kt == kts[-1])],
                                               post=(fin if last else None))
                    self.pipe_flush()

    def phase3(self, L):
        nc, P, cfg = self.nc, self.P, self.cfg
        even = (L % 2 == 0)
        e = L // 2
        wo = self.b_oe[e] if even else self.b_oo[e]
        xsrc = self.x_in if L == 0 else self.xs
        last = (L == cfg.DEPTH - 1)
        dst = self.y_out if last else self.xs
        NG = cfg.NTOK // 512
        with ExitStack() as st:
            Wd, r_Wd = self.sb(st, [128, 32, D], BF16, "Wd")
            gB, r_g = self.sb(st, [128, D], F32, "gB")
            gF, r_gF = self.sb(st, [128, D], F32, "gF") if last else (None, None)
            NS = 3
            slots = [self.sb(st, [128, 4096], BF16, "ws") for _ in range(NS)]
            aoS = [self.sb(st, [128, 8, 512], BF16, "aoS") for _ in range(1)]
            xt = [self.sb(st, [128, 4, D], F32, "xt") for _ in range(2)]
            h2, r_h2 = self.sb(st, [128, D], BF16, "h2")
            junk, r_junk = self.sb(st, [128, D], BF16, "junk")
            h2T, r_h2T = self.sb(st, [128, 8, 512], BF16, "h2T")
            aT, r_aT = self.sb(st, [128, 32, 512], BF16, "aT")
            sq = [self.sb(st, [128, 512], F32, "sq") for _ in range(2)]
            stt = [self.sb(st, [128, 4], F32, "stt") for _ in range(2)]
            for c8 in range(4):
                P.dma("sp", Wd[:, c8 * 8:(c8 + 1) * 8, :], self.b_dn[L][c8 * 1024:(c8 + 1) * 1024, :].rearrange("(c p) f -> p c f", p=128), r_Wd, writes=[r_Wd])
            P.dma("sp", gB[:], self.g_ffn[L], r_g, writes=[r_g])
            if last:
                P.dma("sp", gF[:], self.g_fin[:, :], r_gF, writes=[r_gF])
            si = [0]
            sti = [0]
            h2x, r_h2x = self.sb(st, [128, 4, D], BF16, "h2x")
            h2Tb = [(h2T, r_h2T), self.sb(st, [128, 8, 512], BF16, "h2Tb")]

            def load_a(g):
                t0 = g * 512
                a_, ra = aoS[0]
                P.dma("sp", a_[:], self.aoT[:, t0:t0 + 512].rearrange("(c p) t -> p c t", p=128), ra, writes=[ra])

            def load_x(g):
                t0 = g * 512
                x_, rx = xt[g % 2]
                P.dma("sp", x_[:], xsrc[t0:t0 + 512, :].rearrange("(j p) f -> p j f", p=128), rx, writes=[rx])

            def F1(g):
                a_, ra = aoS[0]
                x_, rx = xt[g % 2]
                wslots = []
                for hc in range(2):
                    w_, rw = slots[si[0] % NS]
                    si[0] += 1
                    P.dma("sp", w_[:].rearrange("p (c f) -> p c f", c=4), wo[hc * 512:(hc + 1) * 512, :].rearrange("(c p) f -> p c f", p=128), rw, writes=[rw])
                    wslots.append((w_, rw))
                for j in range(4):
                    for half in range(2):
                        ps, rps = self.PS[half]
                        for c in range(8):
                            w_, rw = wslots[c // 4]
                            wv_ = w_[:].rearrange("p (c f) -> p c f", c=4)
                            P.op("pe", lambda: nc.tensor.matmul(ps[:], a_[:, c, j * 128:(j + 1) * 128], wv_[:, c % 4, half * 512:(half + 1) * 512], start=(c == 0), stop=(c == 7)),
                                 [ra, rw], [rps])
                        P.op("dve", lambda: nc.vector.tensor_tensor(x_[:, j, half * 512:(half + 1) * 512], ps[:], x_[:, j, half * 512:(half + 1) * 512], ALU.add),
                             [rps, rx], [rx])
                    s_, rs = stt[sti[0] % 2]
                    sti[0] += 1
                    self.rmsnorm(x_[:, j, :], rx, gB[:], r_g, h2x[:, j, :], r_h2x, s_, rs, junk[:], r_junk)

            def F2(g):
                hT_, rhT = h2Tb[g % 2]
                for j in range(4):
                    for c in range(8):
                        P.op("pe", lambda: nc.tensor.transpose(self.PT[:, c * 128:(c + 1) * 128], h2x[:, j, c * 128:(c + 1) * 128], self.ident[:]),
                             [r_h2x, self.r_ident], [self.rPT[0]])
                    P.op("act", lambda: nc.scalar.copy(hT_[:, :, j * 128:(j + 1) * 128], self.PT[:, 0:1024].rearrange("p (c t) -> p c t", c=8)),
                         [self.rPT[0]], [rhT])

            def U(g):
                hT_, rhT = h2Tb[g % 2]
                for qc in range(8):
                    w_, rw = slots[si[0] % NS]
                    si[0] += 1
                    wv_ = w_[:].rearrange("p (c f) -> p c f", c=8)
                    P.dma("sp", wv_, self.b_up[L][:, qc * 512:(qc + 1) * 512].rearrange("(c p) f -> p c f", p=128), rw, writes=[rw])
                    for f4 in range(4):
                        ffc = qc * 4 + f4
                        ps, rps = self.PS[2 + ffc % 2]
                        s_, rsq = sq[ffc % 2]
                        for c in range(8):
                            P.op("pe", lambda: nc.tensor.matmul(ps[:], wv_[:, c, f4 * 128:(f4 + 1) * 128], hT_[:, c, :], start=(c == 0), stop=(c == 7)),
                                 [rw, rhT], [rps])
                        P.op("act", lambda: nc.scalar.activation(s_[:], ps[:], AF.Square), [rps], [rsq])
                        P.op("dve", lambda: nc.vector.scalar_tensor_tensor(aT[:, ffc, :], ps[:], 0.0, s_[:], ALU.is_gt, ALU.mult), [rps, rsq], [r_aT])

            def Dn(g, jh_list):
                x_, rx = xt[g % 2]
                for (j, half) in jh_list:
                    ps, rps = self.PS[4 + half]
                    for ffc in range(32):
                        P.op("pe", lambda: nc.tensor.matmul(ps[:], aT[:, ffc, j * 128:(j + 1) * 128], Wd[:, ffc, half * 512:(half + 1) * 512], start=(ffc == 0), stop=(ffc == 31)),
                             [r_aT, r_Wd], [rps])
                    P.op("dve", lambda: nc.vector.tensor_tensor(x_[:, j, half * 512:(half + 1) * 512], ps[:], x_[:, j, half * 512:(half + 1) * 512], ALU.add),
                         [rps, rx], [rx])
                    if last and half == 1:
                        s_, rs = stt[sti[0] % 2]
                        sti[0] += 1
                        P.op("act", lambda: nc.scalar.activation(junk[:], x_[:, j, :], AF.Square, accum_out=s_[:, 0:1]), [rx], [r_junk, rs])
                        P.op("act", lambda: nc.scalar.activation(s_[:, 1:2], s_[:, 0:1], AF.Sqrt, bias=self.epst[:, 0:1], scale=1.0 / D), [rs, self.r_eps], [rs])
                        P.op("dve", lambda: nc.vector.reciprocal(s_[:, 2:3], s_[:, 1:2]), [rs], [rs])
                        P.op("dve", lambda: nc.vector.scalar_tensor_tensor(x_[:, j, :], x_[:, j, :], s_[:, 2:3], gF[:], ALU.mult, ALU.mult), [rx, rs, r_gF], [rx])

            jh = [(j, half) for j in range(4) for half in range(2)]
            load_a(0)
            load_x(0)
            F1(0)
            F2(0)
            for g in range(NG):
                t0 = g * 512
                x_, rx = xt[g % 2]
                if g + 1 < NG:
                    load_x(g + 1)
                    load_a(g + 1)
                U(g)
                if g + 1 < NG:
                    F1(g + 1)
                Dn(g, jh[0:2])
                if g + 1 < NG:
                    F2(g + 1)
                Dn(g, jh[2:8])
                P.dma("sp", dst[t0:t0 + 512, :].rearrange("(j p) f -> p j f", p=128), x_[:], rx, reads=[rx])


def _in_maps(cfg, x, norm_mix, norm_ffn, w_in_even, w_out_even, lambda_q1, lambda_k1, lambda_q2, lambda_k2,
             diff_subln, w_in_odd, w_out_odd, w_ffn_up, w_ffn_down, norm_final):
    f = lambda a: np.ascontiguousarray(np.asarray(a, dtype=np.float32))
    NE, NO = cfg.NE, max(cfg.NO, 1)
    bc = lambda a: np.ascontiguousarray(np.broadcast_to(f(a)[:, None, :], (a.shape[0], 128, a.shape[1])))
    lam = np.concatenate([f(lambda_q1)[:NE], f(lambda_k1)[:NE], f(lambda_q2)[:NE], f(lambda_k2)[:NE]], axis=1)
    w_io = f(w_in_odd)[:NO] if cfg.NO > 0 else np.zeros((1, D, 2120), np.float32)
    w_oo = f(w_out_odd)[:NO] if cfg.NO > 0 else np.zeros((1, D, D), np.float32)
    shared = {
        "g_mix": bc(np.asarray(norm_mix)[:cfg.DEPTH]),
        "g_ffn": bc(np.asarray(norm_ffn)[:cfg.DEPTH]),
        "g_fin": np.ascontiguousarray(np.broadcast_to(f(norm_final)[None, :], (128, D))),
        "lamv": np.ascontiguousarray(np.broadcast_to(lam[:, None, :], (NE, 128, 256))),
        "subln": np.ascontiguousarray(f(diff_subln)[:NE][:, :, None]),
        "w_ie": f(w_in_even)[:NE], "w_oe": f(w_out_even)[:NE],
        "w_io": w_io, "w_oo": w_oo,
        "w_up": f(w_ffn_up)[:cfg.DEPTH], "w_dn": f(w_ffn_down)[:cfg.DEPTH],
    }
    shared.update(_consts(cfg))
    xf = f(x).reshape(-1, D)
    maps = []
    for c in range(cfg.NCORES):
        m = dict(shared)
        m["x"] = np.ascontiguousarray(xf[c * cfg.NTOK:(c + 1) * cfg.NTOK])
        maps.append(m)
    return maps


def run_cfg(cfg, **inputs):
    nc = Builder(cfg).build()
    maps = _in_maps(cfg, **inputs)
    res = run_bass_kernel_spmd(nc, maps, core_ids=list(range(cfg.NCORES)))
    out = np.concatenate([np.asarray(r["y"]) for r in res.results], axis=0)
    if cfg.debug:
        return out, res.results
    return out


def kernel(**inputs):
    cfg = Cfg(S=4096, NSEQ=2, DEPTH=4, NCORES=8)
    out = run_cfg(cfg, **inputs)
    return out.reshape(16, 4096, D).astype(np.float32)
```
